# Optimizing a Trainium2 kernel written in Bass

```python
import math
import jax, jax.numpy as jnp
from jax import lax
import numpy as np

D_MODEL = 1024
BATCH = 16
SEQ = 4096
DEPTH = 1

HG_HEADS = 4
HG_DK = 128
HG_DV = 128
HG_WIDTH = HG_HEADS * HG_DK
HG_CHUNK = 64
DF_HEADS = 4
DF_DQK = 64
DF_DV = 2 * DF_DQK
DF_QK_WIDTH = DF_HEADS * DF_DQK
DF_V_WIDTH = DF_HEADS * DF_DV
Q_BLOCK = 128
ROPE_THETA = 500000.0
ROT_DIM = DF_DQK // 4
N_MEM = 256
MEM_HEADS = 4
MEM_DH = 128
MEM_WIDTH = MEM_HEADS * MEM_DH
N_BRANCH = 3
IN_WIDTH = 4 * HG_WIDTH + 4 * DF_QK_WIDTH + DF_V_WIDTH + MEM_WIDTH
IN_SPLITS = (512, 1024, 1536, 2048, 2304, 2560, 2816, 3072, 3584)
N_GROUPS = 4
EXPERTS_PER_GROUP = 8
N_EXPERTS = N_GROUPS * EXPERTS_PER_GROUP
TOP_K = 2
D_EXPERT = 512
MOE_BLOCK = 128
DEEPNORM_ALPHA = (2.0 * DEPTH) ** 0.25
DEEPNORM_BETA = (8.0 * DEPTH) ** -0.25
LN_EPS = 1e-5
RMS_EPS = 1e-6

kernel_name = 'hybrid_hgrn2_diffattn_memxattn_hmoe_deepnorm'


def _layer_norm(x, gain, bias):
    xf = x.astype(jnp.float32)
    mu = jnp.mean(xf, axis=-1, keepdims=True)
    var = jnp.mean(jnp.square(xf - mu), axis=-1, keepdims=True)
    y = (xf - mu) * lax.rsqrt(var + LN_EPS)
    return (y * gain.astype(jnp.float32) + bias.astype(jnp.float32)).astype(x.dtype)


def _rms_norm(x, gain):
    xf = x.astype(jnp.float32)
    y = xf * lax.rsqrt(jnp.mean(jnp.square(xf), axis=-1, keepdims=True) + RMS_EPS)
    return (y * gain.astype(jnp.float32)).astype(x.dtype)


def _rope_tables(positions):
    inv_freq = ROPE_THETA ** (-jnp.arange(0, ROT_DIM, 2, dtype=jnp.float32) / ROT_DIM)
    ang = positions.astype(jnp.float32)[..., None] * inv_freq
    return jnp.cos(ang)[:, :, None, :], jnp.sin(ang)[:, :, None, :]


def _apply_partial_rope(t, cos, sin):
    half = ROT_DIM // 2
    t1, t2, rest = t[..., :half], t[..., half:ROT_DIM], t[..., ROT_DIM:]
    c = cos.astype(t.dtype)
    s = sin.astype(t.dtype)
    return jnp.concatenate([t1 * c - t2 * s, t2 * c + t1 * s, rest], axis=-1)


def _hgrn2(q, f_pre, v_in, g, lb, norm_gain):
    b_sz, s_len, _ = q.shape
    n_chunks = s_len // HG_CHUNK
    f32 = jnp.float32
    forget = lb + (1.0 - lb) * jax.nn.sigmoid(f_pre.astype(f32))
    log_f = jnp.log(forget)
    k = 1.0 - forget
    qf = jax.nn.silu(q.astype(f32))
    v = v_in.astype(f32)

    def to_chunks(t):
        return t.reshape(b_sz, n_chunks, HG_CHUNK, HG_HEADS, -1).transpose(1, 0, 3, 2, 4)

    causal = jnp.tril(jnp.ones((HG_CHUNK, HG_CHUNK), dtype=bool))[:, :, None]

    def chunk_step(state, inp):
        qc, kc, vc, lfc = inp
        cum = jnp.cumsum(lfc, axis=2)
        o_inter = jnp.einsum('bhtk,bhkv->bhtv', qc * jnp.exp(cum), state)
        rel = cum[:, :, :, None, :] - cum[:, :, None, :, :]
        decay = jnp.where(causal, jnp.exp(jnp.where(causal, rel, 0.0)), 0.0)
        scores = jnp.einsum('bhtsk,bhsk->bhts', qc[:, :, :, None, :] * decay, kc)
        o_intra = jnp.einsum('bhts,bhsv->bhtv', scores, vc)
        last = cum[:, :, -1:, :]
        state = jnp.exp(last[:, :, 0, :])[..., None] * state + jnp.einsum(
            'bhsk,bhsv->bhkv', kc * jnp.exp(last - cum), vc)
        return state, o_inter + o_intra

    state0 = jnp.zeros((b_sz, HG_HEADS, HG_DK, HG_DV), f32)
    _, o = lax.scan(chunk_step, state0,
                    (to_chunks(qf), to_chunks(k), to_chunks(v), to_chunks(log_f)))
    o = o.transpose(1, 0, 3, 2, 4).reshape(b_sz, s_len, HG_HEADS, HG_DV)
    o = _rms_norm(o, norm_gain.reshape(HG_HEADS, HG_DV))
    gate = jax.nn.silu(g.astype(f32)).reshape(b_sz, s_len, HG_HEADS, HG_DV)
    return (o * gate).reshape(b_sz, s_len, HG_WIDTH).astype(q.dtype)


def _diff_attention(q1, q2, k1, k2, v, lam, lam_init, subln_gain):
    b_sz, s_len = q1.shape[0], q1.shape[1]
    q = jnp.stack([q1, q2], axis=1).transpose(0, 1, 3, 2, 4)
    k = jnp.stack([k1, k2], axis=1).transpose(0, 1, 3, 2, 4)
    vt = v.transpose(0, 2, 1, 3)
    scale = DF_DQK ** -0.5
    key_pos = jnp.arange(s_len)

    def query_block(i):
        start = i * Q_BLOCK
        qb = lax.dynamic_slice_in_dim(q, start, Q_BLOCK, axis=3)
        s = jnp.einsum('bmhqd,bmhkd->bmhqk', qb, k).astype(jnp.float32) * scale
        mask = (start + jnp.arange(Q_BLOCK))[:, None] >= key_pos[None, :]
        p = jax.nn.softmax(jnp.where(mask, s, -jnp.inf), axis=-1)
        w = p[:, 0] - lam * p[:, 1]
        return jnp.einsum('bhqk,bhkd->bhqd', w.astype(vt.dtype), vt)

    o = lax.map(query_block, jnp.arange(s_len // Q_BLOCK))
    o = o.transpose(1, 0, 3, 2, 4).reshape(b_sz, s_len, DF_HEADS, DF_DV)
    o = _rms_norm(o, subln_gain) * (1.0 - lam_init)
    return o.reshape(b_sz, s_len, DF_V_WIDTH)


def _memory_attention(q, k, v):
    s = jnp.einsum('bshd,bnhd->bhsn', q, k).astype(jnp.float32) * (MEM_DH ** -0.5)
    p = jax.nn.softmax(s, axis=-1)
    o = jnp.einsum('bhsn,bnhd->bshd', p.astype(v.dtype), v)
    return o.reshape(o.shape[0], o.shape[1], MEM_WIDTH)


def _token_mixer(x, cos, sin, mem, w_in, w_gates, lb, hg_gain, lam_q1, lam_k1, lam_q2, lam_k2,
                 lam_init, df_gain, w_mem_kv, wb_hg, wb_df, wb_mem, w_out):
    b_sz, s_len, _ = x.shape
    f32 = jnp.float32
    proj = x @ w_in
    hq, hf, hi, hg, dq1, dq2, dk1, dk2, dv, mq = jnp.split(proj, IN_SPLITS, axis=-1)

    y_hg = _hgrn2(hq, hf, hi, hg, lb, hg_gain)

    def heads(t, h):
        return t.reshape(t.shape[0], t.shape[1], h, -1)

    def rope(t):
        return _apply_partial_rope(heads(t, DF_HEADS), cos, sin)

    lam = (jnp.exp(jnp.sum(lam_q1.astype(f32) * lam_k1.astype(f32)))
           - jnp.exp(jnp.sum(lam_q2.astype(f32) * lam_k2.astype(f32))) + lam_init)
    y_df = _diff_attention(rope(dq1), rope(dq2), rope(dk1), rope(dk2), heads(dv, DF_HEADS),
                           lam, lam_init, df_gain)

    mk, mv = jnp.split(mem @ w_mem_kv, 2, axis=-1)
    y_mem = _memory_attention(heads(mq, MEM_HEADS), heads(mk, MEM_HEADS), heads(mv, MEM_HEADS))

    gates = jax.nn.sigmoid((x @ w_gates).astype(f32)).astype(x.dtype)
    gates = gates.reshape(b_sz, s_len, N_BRANCH, D_MODEL)
    merged = (gates[:, :, 0] * (y_hg @ wb_hg) + gates[:, :, 1] * (y_df @ wb_df)
              + gates[:, :, 2] * (y_mem @ wb_mem))
    return merged @ w_out


def _hier_moe(h, w_group_router, w_expert_router, w_gate, w_up, w_down):
    b_sz, s_len, d = h.shape
    n_tok = b_sz * s_len
    hf = h.reshape(n_tok, d)
    f32 = jnp.float32
    group_p = jax.nn.softmax((hf @ w_group_router).astype(f32), axis=-1)
    group_idx = jnp.argmax(group_p, axis=-1).astype(jnp.int32)
    group_w = jnp.max(group_p, axis=-1)
    exp_logits = (hf @ w_expert_router).astype(f32).reshape(n_tok, N_GROUPS, EXPERTS_PER_GROUP)
    in_group = jnp.take_along_axis(exp_logits, group_idx[:, None, None], axis=1)[:, 0]
    top_logit, top_local = lax.top_k(in_group, TOP_K)
    top_w = jax.nn.softmax(top_logit, axis=-1) * group_w[:, None]
    expert_id = (group_idx[:, None] * EXPERTS_PER_GROUP + top_local.astype(jnp.int32)).reshape(-1)

    n_assign = n_tok * TOP_K
    token_id = jnp.repeat(jnp.arange(n_tok, dtype=jnp.int32), TOP_K)
    order = jnp.argsort(expert_id)
    sorted_e = expert_id[order]
    counts = jnp.bincount(expert_id, length=N_EXPERTS).astype(jnp.int32)
    padded = (counts + MOE_BLOCK - 1) // MOE_BLOCK * MOE_BLOCK
    padded_end = jnp.cumsum(padded)
    padded_start = padded_end - padded
    seg_start = jnp.cumsum(counts) - counts
    dest = padded_start[sorted_e] + jnp.arange(n_assign, dtype=jnp.int32) - seg_start[sorted_e]
    n_slots = n_assign + N_EXPERTS * MOE_BLOCK
    n_blocks = n_slots // MOE_BLOCK
    slot_tok = jnp.zeros((n_slots,), jnp.int32).at[dest].set(token_id[order])
    slot_w = jnp.zeros((n_slots,), h.dtype).at[dest].set(top_w.reshape(-1)[order].astype(h.dtype))
    block_start = jnp.arange(n_blocks, dtype=jnp.int32) * MOE_BLOCK
    block_expert = jnp.minimum(jnp.searchsorted(padded_end, block_start, side='right'),
                               N_EXPERTS - 1).astype(jnp.int32)

    def expert_block(args):
        tok, w, e = args
        xb = hf[tok]
        act = jax.nn.silu(xb @ w_gate[e]) * (xb @ w_up[e])
        return (act @ w_down[e]) * w[:, None]

    y = lax.map(expert_block, (slot_tok.reshape(n_blocks, MOE_BLOCK),
                               slot_w.reshape(n_blocks, MOE_BLOCK), block_expert))
    out = jnp.zeros_like(hf).at[slot_tok].add(y.reshape(n_slots, d))
    return out.reshape(b_sz, s_len, d)


def _normal(k, shape, scale):
    return jax.random.normal(k, shape, jnp.float32) * scale


def setup_inputs(seed: int = 0) -> dict:
    key = jax.random.key(seed)
    ks = jax.random.split(key, 32)
    L = DEPTH
    beta = DEEPNORM_BETA
    d_inv = D_MODEL ** -0.5
    offset = jax.random.randint(ks[2], (BATCH, 1), 0, 1024, dtype=jnp.int32)
    positions = (offset + jnp.arange(SEQ, dtype=jnp.int32)[None, :]).astype(jnp.int32)
    return {
        'x': _normal(ks[0], (BATCH, SEQ, D_MODEL), 1.0),
        'mem': _normal(ks[1], (BATCH, N_MEM, D_MODEL), 1.0),
        'positions': positions,
        'w_in': _normal(ks[3], (L, D_MODEL, IN_WIDTH), d_inv),
        'w_gates': _normal(ks[4], (L, D_MODEL, N_BRANCH * D_MODEL), d_inv),
        'hgrn_lower_bounds': _normal(ks[5], (L + 1, HG_WIDTH), 0.1),
        'hgrn_norm_gain': 1.0 + _normal(ks[6], (L, HG_WIDTH), 0.02),
        'diff_lambda_q1': _normal(ks[7], (L, DF_DQK), 0.1),
        'diff_lambda_k1': _normal(ks[8], (L, DF_DQK), 0.1),
        'diff_lambda_q2': _normal(ks[9], (L, DF_DQK), 0.1),
        'diff_lambda_k2': _normal(ks[10], (L, DF_DQK), 0.1),
        'diff_subln_gain': 1.0 + _normal(ks[11], (L, DF_DV), 0.02),
        'w_mem_kv': _normal(ks[12], (L, D_MODEL, 2 * MEM_WIDTH), d_inv),
        'w_branch_hgrn': _normal(ks[13], (L, HG_WIDTH, D_MODEL), beta * HG_WIDTH ** -0.5),
        'w_branch_diff': _normal(ks[14], (L, DF_V_WIDTH, D_MODEL), beta * DF_V_WIDTH ** -0.5),
        'w_branch_mem': _normal(ks[15], (L, MEM_WIDTH, D_MODEL), beta * MEM_WIDTH ** -0.5),
        'w_out': _normal(ks[16], (L, D_MODEL, D_MODEL), beta * d_inv),
        'ln1_gain': 1.0 + _normal(ks[17], (L, D_MODEL), 0.02),
        'ln1_bias': _normal(ks[18], (L, D_MODEL), 0.02),
        'w_group_router': _normal(ks[19], (L, D_MODEL, N_GROUPS), d_inv),
        'w_expert_router': _normal(ks[20], (L, D_MODEL, N_EXPERTS), d_inv),
        'w_expert_gate': _normal(ks[21], (L, N_EXPERTS, D_MODEL, D_EXPERT), d_inv),
        'w_expert_up': _normal(ks[22], (L, N_EXPERTS, D_MODEL, D_EXPERT), d_inv),
        'w_expert_down': _normal(ks[23], (L, N_EXPERTS, D_EXPERT, D_MODEL), beta * D_EXPERT ** -0.5),
        'ln2_gain': 1.0 + _normal(ks[24], (L, D_MODEL), 0.02),
        'ln2_bias': _normal(ks[25], (L, D_MODEL), 0.02),
    }


def reference(x, mem, positions, w_in, w_gates, hgrn_lower_bounds, hgrn_norm_gain,
              diff_lambda_q1, diff_lambda_k1, diff_lambda_q2, diff_lambda_k2, diff_subln_gain,
              w_mem_kv, w_branch_hgrn, w_branch_diff, w_branch_mem, w_out, ln1_gain, ln1_bias,
              w_group_router, w_expert_router, w_expert_gate, w_expert_up, w_expert_down,
              ln2_gain, ln2_bias):
    lower = jnp.cumsum(jax.nn.softmax(hgrn_lower_bounds.astype(jnp.float32), axis=0), axis=0)
    cos, sin = _rope_tables(positions)
    for l in range(DEPTH):
        lam_init = 0.8 - 0.6 * math.exp(-0.3 * l)
        mix = _token_mixer(x, cos, sin, mem, w_in[l], w_gates[l], lower[l], hgrn_norm_gain[l],
                           diff_lambda_q1[l], diff_lambda_k1[l], diff_lambda_q2[l], diff_lambda_k2[l],
                           lam_init, diff_subln_gain[l], w_mem_kv[l], w_branch_hgrn[l],
                           w_branch_diff[l], w_branch_mem[l], w_out[l])
        x = _layer_norm(DEEPNORM_ALPHA * x + mix, ln1_gain[l], ln1_bias[l])
        ffn = _hier_moe(x, w_group_router[l], w_expert_router[l], w_expert_gate[l],
                        w_expert_up[l], w_expert_down[l])
        x = _layer_norm(DEEPNORM_ALPHA * x + ffn, ln2_gain[l], ln2_bias[l])
    return x
```

```python
import math
from contextlib import ExitStack

import numpy as np
import concourse.bass as bass
import concourse.mybir as mybir
from concourse.bass_utils import run_bass_kernel_spmd

F32 = mybir.dt.float32
BF16 = mybir.dt.bfloat16
I32 = mybir.dt.int32
AF = mybir.ActivationFunctionType
ALU = mybir.AluOpType
AX = mybir.AxisListType

NCORES = 8
D = 1024
SEQ = 4096
NSEQ = 2
NTOK = NSEQ * SEQ
NT = NTOK // 128
TPS = SEQ // 128
STT = 2
TW = STT * 128
NST = NT // STT
CH = 4608
NCHUNK = 20
NE = 32
NSLOT = NTOK * 2 + NE * 128
NBLK = NSLOT // 128
ALPHA = 2.0 ** 0.25
LN_EPS = 1e-5
RMS_EPS = 1e-6
TWO_PI = 2.0 * math.pi

SAME_ENGINE_SYNC = True


class Buf:
    __slots__ = ("name", "w", "r")

    def __init__(self, name=""):
        self.name = name
        self.w = None
        self.r = {}


class Sched:
    def __init__(self, nc, stack, n_dma_sems=32):
        self.nc = nc
        self.names = ["pe", "act", "dve", "pool", "sp"]
        self.sem = {}
        self.cnt = {}
        self.known = {}
        for k in self.names:
            self.sem[k] = stack.enter_context(nc.semaphore("s_" + k))
            self.cnt[k] = 0
            self.known[k] = {}
        self.dsem = [stack.enter_context(nc.semaphore("d%d" % i)) for i in range(n_dma_sems)]
        self.dcnt = [0] * n_dma_sems
        self.dnext = 0
        self.n_wait = 0
        self.n_ins = 0
        self.prog = {k: [] for k in self.names}

    def _need(self, e, deps, sem, val, who):
        if who == e and not SAME_ENGINE_SYNC:
            return
        if who == "pe" and e == "pe":
            return
        key = id(sem)
        if deps.get(key, (None, 0))[1] < val:
            deps[key] = (sem, val)

    def _collect(self, e, reads, writes):
        deps = {}
        for b in reads:
            if b.w is not None:
                self._need(e, deps, b.w[0], b.w[1], b.w[2])
        for b in writes:
            if b.w is not None:
                self._need(e, deps, b.w[0], b.w[1], b.w[2])
            for (sem, (val, who)) in b.r.values():
                self._need(e, deps, sem, val, who)
        return deps

    def _emit_waits(self, e, deps):
        kn = self.known[e]
        for key, (sem, val) in deps.items():
            if kn.get(key, 0) >= val:
                continue
            self.prog[e].append((0, sem, val))
            kn[key] = val
            self.n_wait += 1

    def _mark(self, reads, writes, sem, val, who):
        for b in reads:
            b.r[id(sem)] = (sem, (val, who))
        for b in writes:
            b.w = (sem, val, who)
            b.r = {}

    def op(self, e, fn, reads=(), writes=()):
        deps = self._collect(e, reads, writes)
        self._emit_waits(e, deps)
        self.cnt[e] += 1
        self.prog[e].append((1, fn, self.sem[e], 1))
        self._mark(reads, writes, self.sem[e], self.cnt[e], e)
        self.n_ins += 1

    def dma_fn(self, q, fn, reads=(), writes=()):
        i = self.dnext
        self.dnext = (self.dnext + 1) % len(self.dsem)
        sem = self.dsem[i]
        deps = self._collect(q, reads, writes)
        if self.dcnt[i] > 0:
            key = id(sem)
            if deps.get(key, (None, 0))[1] < self.dcnt[i]:
                deps[key] = (sem, self.dcnt[i])
        self._emit_waits(q, deps)
        self.prog[q].append((1, fn, sem, 16))
        self.dcnt[i] += 16
        self._mark(reads, writes, sem, self.dcnt[i], "dma")
        self.n_ins += 1

    def dma(self, q, out, in_, reads=(), writes=()):
        self.dma_fn(q, (lambda eng, out=out, in_=in_: eng.dma_start(out=out, in_=in_)), reads, writes)

    def barrier(self):
        for e in self.names:
            deps = {}
            for o in self.names:
                if o != e and self.cnt[o] > 0:
                    deps[id(self.sem[o])] = (self.sem[o], self.cnt[o])
            for i, s in enumerate(self.dsem):
                if self.dcnt[i] > 0:
                    deps[id(s)] = (s, self.dcnt[i])
            self._emit_waits(e, deps)

    def emit(self):
        def replay(e):
            def body(eng):
                for item in self.prog[e]:
                    if item[0] == 0:
                        eng.wait_ge(item[1], item[2])
                    else:
                        item[1](eng).then_inc(item[2], item[3])
            return body

        with self.nc.Block() as block:
            block.sync(replay("sp"))
            block.tensor(replay("pe"))
            block.scalar(replay("act"))
            block.vector(replay("dve"))
            block.gpsimd(replay("pool"))


def build_program(debug=False, phases=3):
    nc = bass.Bass("TRN2", target_bir_lowering=False)
    dk = "ExternalOutput" if debug else "Internal"
    x_d = nc.dram_tensor("x", [NTOK, D], F32, kind="ExternalInput")
    mem_d = nc.dram_tensor("mem", [NSEQ * 256, D], F32, kind="ExternalInput")
    pos_d = nc.dram_tensor("pos", [128, NT], I32, kind="ExternalInput")
    wf_d = nc.dram_tensor("wf", [NCHUNK, 128, CH], F32, kind="ExternalInput")
    wr_d = nc.dram_tensor("wr", [128, 8, 36], F32, kind="ExternalInput")
    v1_d = nc.dram_tensor("vec1", [128, 3328], F32, kind="ExternalInput")
    lbr_d = nc.dram_tensor("lbr", [128, 1024], F32, kind="ExternalInput")
    v2_d = nc.dram_tensor("vec2", [128, 2048], F32, kind="ExternalInput")
    wg_d = [nc.dram_tensor("wg%d" % i, [NE * 128, 2048], F32, kind="ExternalInput") for i in range(2)]
    wu_d = [nc.dram_tensor("wu%d" % i, [NE * 128, 2048], F32, kind="ExternalInput") for i in range(2)]
    wd_d = [nc.dram_tensor("wd%d" % i, [NE * 128, 2048], F32, kind="ExternalInput") for i in range(2)]
    out_d = nc.dram_tensor("out", [NTOK, D], F32, kind="ExternalOutput")
    wb_d = nc.dram_tensor("wb", [NCHUNK, 128, CH], BF16, kind="Internal")
    x1_d = nc.dram_tensor("x1s", [NTOK, D], F32, kind=dk)
    x1b_d = nc.dram_tensor("x1b", [NTOK, D], BF16, kind="Internal")
    xs_d = nc.dram_tensor("xsl", [NSLOT, D], BF16, kind="Internal")
    ys_d = nc.dram_tensor("ysl", [NSLOT, D], F32, kind="Internal")
    if debug:
        dbg_hg = nc.dram_tensor("dbg_hg", [NTOK, 512], F32, kind="ExternalOutput")
        dbg_df = nc.dram_tensor("dbg_df", [NTOK, 512], F32, kind="ExternalOutput")
        dbg_mem = nc.dram_tensor("dbg_mem", [NTOK, 512], F32, kind="ExternalOutput")
        dbg_rt = nc.dram_tensor("dbg_rt", [128, NT, 8], F32, kind="ExternalOutput")
        dbg_kc = nc.dram_tensor("dbg_kc", [128, 4 * SEQ], BF16, kind="ExternalOutput")
        dbg_vc = nc.dram_tensor("dbg_vc", [128, TPS * 4 * 130], BF16, kind="ExternalOutput")
        dbg_v1 = nc.dram_tensor("dbg_v1", [128, 3328], F32, kind="ExternalOutput")

    with ExitStack() as top:
        S = Sched(nc, top)

        def sbt(st, name, shape, dt):
            return st.enter_context(nc.sbuf_tensor(name, shape, dt))

        def MM(out, lhsT, rhs, st_, sp_, rd, wr):
            S.op("pe", lambda e: e.matmul(out, lhsT, rhs, start=st_, stop=sp_), rd, wr)

        def TR(out, in_, idn, rd, wr):
            S.op("pe", lambda e: e.transpose(out, in_, idn), rd, wr)

        def ACT(out, in_, func, rd, wr, bias=None, scale=None, accum=None):
            kw = {}
            if bias is not None:
                kw["bias"] = bias
            if scale is not None:
                kw["scale"] = scale
            if accum is not None:
                kw["accum_out"] = accum
            S.op("act", lambda e: e.activation(out, in_, func, **kw), rd, wr)

        def TT(out, a, b, op, rd, wr, eng="dve"):
            S.op(eng, lambda e: e.tensor_tensor(out, a, b, op), rd, wr)

        def TS(out, a, s1, s2, op0, op1, rd, wr, eng="dve"):
            if s2 is None:
                S.op(eng, lambda e: e.tensor_scalar(out, a, s1, None, op0), rd, wr)
            else:
                S.op(eng, lambda e: e.tensor_scalar(out, a, s1, s2, op0, op1), rd, wr)

        def SCTT(out, in0, scalar, in1, op0, op1, rd, wr):
            S.op("dve", lambda e: e.scalar_tensor_tensor(out, in0, scalar, in1, op0, op1), rd, wr)

        def CP(eng, out, in_, rd, wr):
            if eng == "act":
                S.op("act", lambda e: e.copy(out, in_), rd, wr)
            else:
                S.op(eng, lambda e: e.tensor_copy(out, in_), rd, wr)

        def RSUM(out, in_, rd, wr):
            S.op("dve", lambda e: e.reduce_sum(out, in_, AX.X), rd, wr)

        def RMAX(out, in_, rd, wr):
            S.op("dve", lambda e: e.reduce_max(out, in_, AX.X), rd, wr)

        def RECIP(out, in_, rd, wr):
            S.op("dve", lambda e: e.reciprocal(out, in_), rd, wr)

        def MEMSET(eng, ap, val, wr):
            S.op(eng, lambda e: e.memset(ap, val), (), wr)

        def ASEL(out, in_, pattern, cmp, fill, base, cm, rd, wr):
            S.op("pool", lambda e: e.affine_select(out, in_, pattern=pattern, compare_op=cmp, fill=fill,
                                                   base=base, channel_multiplier=cm), rd, wr)

        psum = [top.enter_context(nc.psum_tensor("ps%d" % i, [128, 512], F32)) for i in range(8)]
        psb = [Buf("ps%d" % i) for i in range(8)]
        rot = {"n": 0}

        def PS():
            i = rot["n"] % 5
            rot["n"] += 1
            return psum[i], psb[i]

        ACC = [(psum[5], psb[5]), (psum[6], psb[6]), (psum[7], psb[7])]

        cst = top
        ident_bf = sbt(cst, "ident_bf", [128, 128], BF16); b_ident_bf = Buf()
        ident_f = sbt(cst, "ident_f", [128, 128], F32); b_ident_f = Buf()
        maskT = sbt(cst, "maskT", [128, 128], F32); b_maskT = Buf()
        lstrict = sbt(cst, "lstrict", [128, 128], BF16); b_lstrict = Buf()
        ones_bf = sbt(cst, "ones_bf", [128, 128], BF16); b_ones = Buf()
        e_iota = sbt(cst, "e_iota", [128, 32], F32); b_eiota = Buf()
        rt = sbt(cst, "rt", [128, NT, 8], F32)
        b_rt = [Buf() for _ in range(NT)]
        tot = sbt(cst, "tot", [128, 32], F32); b_tot = Buf()
        wr_sb = sbt(cst, "wr_sb", [128, 8, 36], F32); b_wr = Buf()

        S.barrier()
        MEMSET("pool", ident_bf[:], 1.0, [b_ident_bf])
        ASEL(ident_bf[:], ident_bf[:], [[-1, 128]], ALU.is_equal, 0.0, 0, 1, [b_ident_bf], [b_ident_bf])
        MEMSET("pool", ident_f[:], 1.0, [b_ident_f])
        ASEL(ident_f[:], ident_f[:], [[-1, 128]], ALU.is_equal, 0.0, 0, 1, [b_ident_f], [b_ident_f])
        MEMSET("pool", maskT[:], 1.0, [b_maskT])
        ASEL(maskT[:], maskT[:], [[1, 128]], ALU.is_ge, 0.0, 0, -1, [b_maskT], [b_maskT])
        MEMSET("pool", lstrict[:], 1.0, [b_lstrict])
        ASEL(lstrict[:], lstrict[:], [[1, 128]], ALU.is_gt, 0.0, 0, -1, [b_lstrict], [b_lstrict])
        MEMSET("pool", ones_bf[:], 1.0, [b_ones])
        ei_i = sbt(cst, "ei_i", [128, 32], I32); b_eii = Buf()
        S.barrier()
        MEMSET("dve", tot[:], 0.0, [b_tot])
        S.op("pool", lambda e: e.iota(ei_i[:], pattern=[[1, 32]], base=0, channel_multiplier=0), (), [b_eii])
        CP("dve", e_iota[:], ei_i[:], [b_eii], [b_eiota])
        S.dma("sp", wr_sb[:], wr_d[:, :, :], (), [b_wr])

        b_wb = [Buf() for _ in range(NCHUNK)]
        with ExitStack() as pro:
            cb = [sbt(pro, "castbuf%d" % i, [128, CH], BF16) for i in range(3)]
            b_cb = [Buf() for _ in range(3)]
            zt = sbt(pro, "zt", [128, 4096], BF16); b_zt = Buf()
            S.barrier()
            for c in range(NCHUNK):
                k = c % 3
                for part in range(3):
                    S.dma("pool", cb[k][:, part * 1536:(part + 1) * 1536], wf_d[c, :, part * 1536:(part + 1) * 1536],
                          (), [b_cb[k]])
                S.dma("sp", wb_d[c, :, :], cb[k][:], [b_cb[k]], [b_wb[c]])
            MEMSET("dve", zt[:], 0.0, [b_zt])
            b_xs_zero = []
            xs_v = xs_d[:, :].rearrange("(n p r) d -> n p (r d)", p=128, r=4)
            for n in range(NSLOT // 512):
                bz = Buf()
                S.dma("sp", xs_v[n], zt[:], [b_zt], [bz])
                b_xs_zero.append(bz)
        S.barrier()

        b_x1 = [Buf() for _ in range(NT)]
        b_x1b = [Buf() for _ in range(NT)]
        with ExitStack() as p1:
            vec1 = sbt(p1, "vec1_sb", [128, 3328], F32); b_vec1 = Buf()
            S.dma("sp", vec1[:], v1_d[:, :], (), [b_vec1])
            hg_gain = vec1[:, 0:512]
            lamv = vec1[:, 512:768]
            df_gain = vec1[:, 768:1280]
            ln1g = vec1[:, 1280:2304]
            ln1b = vec1[:, 2304:3328]
            lbv = sbt(p1, "lbv", [128, 512], F32); b_lbv = Buf()
            oml = sbt(p1, "oml", [128, 512], F32); b_oml = Buf()
            lam_t = sbt(p1, "lam_t", [128, 4], F32); b_lam = Buf()
            lscr = sbt(p1, "lscr", [128, 128], F32); b_lscr = Buf()
            TT(lscr[:, 0:64], lamv[:, 0:64], lamv[:, 64:128], ALU.mult, [b_vec1], [b_lscr])
            TT(lscr[:, 64:128], lamv[:, 128:192], lamv[:, 192:256], ALU.mult, [b_vec1], [b_lscr])
            RSUM(lam_t[:, 0:2], lscr[:].rearrange("p (a b) -> p a b", a=2), [b_lscr], [b_lam])
            ACT(lam_t[:, 0:2], lam_t[:, 0:2], AF.Exp, [b_lam], [b_lam])
            TT(lam_t[:, 2:3], lam_t[:, 0:1], lam_t[:, 1:2], ALU.subtract, [b_lam], [b_lam])
            TS(lam_t[:, 3:4], lam_t[:, 2:3], 0.2, None, ALU.add, None, [b_lam], [b_lam])
            lam_ap = lam_t[:, 3:4]
            dmat = sbt(p1, "dmat", [128, 128], F32); b_dmat = Buf()
            dtmp = sbt(p1, "dtmp", [128, 128], F32); b_dtmp = Buf()
            cols2 = sbt(p1, "cols2", [128, 2], F32); b_cols2 = Buf()
            MEMSET("pool", dtmp[:], 1.0, [b_dtmp])
            ASEL(dtmp[:], dtmp[:], [[0, 128]], ALU.is_ge, 0.0, 63, -1, [b_dtmp], [b_dtmp])
            TT(dmat[:], dtmp[:], maskT[:], ALU.subtract, [b_dtmp, b_maskT], [b_dmat], eng="pool")
            MEMSET("pool", cols2[:], 1.0, [b_cols2])
            CP("pool", cols2[:, 1:2], dtmp[:, 0:1], [b_dtmp, b_cols2], [b_cols2])
            trig = sbt(p1, "trig", [128, 2, NT, 8], F32); b_trig = Buf()
            with ExitStack() as tr:
                lbr = sbt(tr, "lbr_sb", [128, 1024], F32); b_lbr = Buf()
                S.dma("sp", lbr[:], lbr_d[:, :], (), [b_lbr])
                TT(lbv[:], lbr[:, 0:512], lbr[:, 512:1024], ALU.subtract, [b_lbr], [b_lbv])
                ACT(lbv[:], lbv[:], AF.Sigmoid, [b_lbv], [b_lbv])
                TS(oml[:], lbv[:], -1.0, 1.0, ALU.mult, ALU.add, [b_lbv], [b_oml])
                posi = sbt(tr, "posi", [128, NT], I32); b_posi = Buf()
                posf = sbt(tr, "posf", [128, NT], F32); b_posf = Buf()
                u = sbt(tr, "u", [128, 2, NT, 8], F32); b_u = Buf()
                kf = sbt(tr, "kf", [128, 2 * NT * 8], F32); b_kf = Buf()
                ki = sbt(tr, "ki", [128, 2 * NT * 8], I32); b_ki = Buf()
                S.dma("sp", posi[:], pos_d[:, :], (), [b_posi])
                CP("dve", posf[:], posi[:], [b_posi], [b_posf])
                for j in range(8):
                    invf = 500000.0 ** (-(2.0 * j) / 16.0)
                    TS(u[:, 0, :, j], posf[:], float(np.float32(invf)), None, ALU.mult, None, [b_posf], [b_u])
                TS(u[:, 1, :, :], u[:, 0, :, :], math.pi / 2.0, None, ALU.add, None, [b_u], [b_u])
                uf = u[:].rearrange("p a n j -> p (a n j)")
                TS(kf[:], uf, 1.0 / TWO_PI, None, ALU.mult, None, [b_u], [b_kf])
                CP("dve", ki[:], kf[:], [b_kf], [b_ki])
                CP("dve", kf[:], ki[:], [b_ki], [b_kf])
                C1 = 6.28125
                C2 = TWO_PI - C1
                SCTT(uf, kf[:], -C1, uf, ALU.mult, ALU.add, [b_kf, b_u], [b_u])
                SCTT(uf, kf[:], -C2, uf, ALU.mult, ALU.add, [b_kf, b_u], [b_u])
                TS(kf[:], uf, math.pi, -TWO_PI, ALU.is_gt, ALU.mult, [b_u], [b_kf])
                TT(uf, uf, kf[:], ALU.add, [b_u, b_kf], [b_u])
                TS(kf[:], uf, -math.pi, TWO_PI, ALU.is_lt, ALU.mult, [b_u], [b_kf])
                TT(uf, uf, kf[:], ALU.add, [b_u, b_kf], [b_u])
                TS(uf, uf, -3.1415925, 3.1415925, ALU.max, ALU.min, [b_u], [b_u])
                ACT(trig[:].rearrange("p a n j -> p (a n j)"), uf, AF.Sin, [b_u], [b_trig])
                S.barrier()

            kc = sbt(p1, "kc", [128, 4, SEQ], BF16)
            b_kc = [Buf() for _ in range(TPS)]
            vc = sbt(p1, "vc", [128, TPS, 4, 130], BF16)
            b_vc = [Buf() for _ in range(TPS)]
            b_vc1 = Buf()
            MEMSET("pool", vc[:, :, :, 128:130], 1.0, [b_vc1])
            mkT = sbt(p1, "mkT", [128, 4, 256], BF16); b_mkT = Buf()
            mvc = sbt(p1, "mvc", [128, 2, 4, 130], BF16); b_mvc = Buf()
            b_mvc1 = Buf()
            MEMSET("pool", mvc[:, :, :, 128:130], 1.0, [b_mvc1])
            Sst = sbt(p1, "Sst", [128, 4, 128], F32); b_S = Buf()
            Smm = sbt(p1, "Smm", [128, 4, 128], BF16); b_Smm = Buf()
            wbuf = [sbt(p1, "wbuf%d" % i, [128, CH], BF16) for i in range(2)]
            b_wbuf = [Buf() for _ in range(2)]
            worder = []
            for _sq in range(NSEQ):
                worder += [18, 19]
                for _st in range(TPS // STT):
                    worder += list(range(18))
            wst = {"i": 0, "issued": {}}

            def _issue_chunk(i):
                c = worder[i]
                k = i % 2
                S.dma("sp", wbuf[k][:], wb_d[c, :, :], [b_wb[c]], [b_wbuf[k]])
                return wbuf[k], b_wbuf[k]

            def load_chunk(c):
                i = wst["i"]
                assert worder[i] == c, (i, worder[i], c)
                if i not in wst["issued"]:
                    wst["issued"][i] = _issue_chunk(i)
                cur = wst["issued"].pop(i)
                if i + 1 < len(worder) and (i + 1) not in wst["issued"]:
                    wst["issued"][i + 1] = _issue_chunk(i + 1)
                wst["i"] += 1
                return cur

            xs = sbt(p1, "xs", [128, STT, D], F32); b_xs = [Buf() for _ in range(STT)]
            xbf = sbt(p1, "xbf", [128, D], BF16); b_xbf = Buf()
            xT = sbt(p1, "xT", [128, 8, TW], BF16); b_xT = Buf()
            sq = sbt(p1, "sq", [128, STT, 512], F32); b_sq = [Buf() for _ in range(STT)]
            sg = sbt(p1, "sg", [128, STT, 512], F32); b_sg = [Buf() for _ in range(STT)]
            vhg = sbt(p1, "vhg", [128, STT, 512], BF16); b_vhg = [Buf() for _ in range(STT)]
            gsl = sbt(p1, "gsl", [128, STT, 512], BF16); b_gsl = [Buf() for _ in range(STT)]
            qT = sbt(p1, "qT", [128, 4, TW], BF16); b_qT = [Buf() for _ in range(STT)]
            mqT = sbt(p1, "mqT", [128, 4, TW], BF16); b_mqT = Buf()
            yT = [sbt(p1, "yT%d" % i, [128, 4, TW], BF16) for i in range(3)]
            b_yT = [[Buf() for _ in range(STT)] for _ in range(3)]
            mergedT = sbt(p1, "mergedT", [128, 8, TW], BF16); b_mg = [Buf() for _ in range(8)]
            wk = [sbt(p1, "wk%d" % i, [128, 512], F32) for i in range(8)]
            b_wk = [Buf() for _ in range(8)]
            wkb = [sbt(p1, "wkb%d" % i, [128, 512], BF16) for i in range(6)]
            b_wkb = [Buf() for _ in range(6)]
            NPT = 4
            LOOKAHEAD = 2
            pT = [sbt(p1, "pT%d" % i, [128, 512], BF16) for i in range(NPT)]
            b_pT = [Buf() for _ in range(NPT)]
            psel = {"n": 0}
            sm = sbt(p1, "sm", [128, 64], F32); b_sm = Buf()
            z = sbt(p1, "z", [128, D], F32); b_z = Buf()
            z_b = sbt(p1, "z_b", [128, D], F32); b_zB = Buf()
            zb = sbt(p1, "zb", [128, D], BF16); b_zb = Buf()
            x1T = sbt(p1, "x1T", [128, 8, 128], F32); b_x1T = Buf()
            stats = sbt(p1, "stats", [128, 16], F32); b_stats = Buf()
            rl = sbt(p1, "rl", [128, 128], F32); b_rl = Buf()
            oh = sbt(p1, "oh", [128, 96], F32); b_oh = Buf()
            ohb = sbt(p1, "ohb", [128, 32], BF16); b_ohb = Buf()

            def layer_norm(zt, bz, gain, bias, bgb, out_ap, bout):
                for hlf in range(2):
                    S.op("dve", lambda e, hlf=hlf: e.bn_stats(stats[:, hlf * 6:(hlf + 1) * 6], zt[:, hlf * 512:(hlf + 1) * 512]),
                         [bz], [b_stats])
                S.op("dve", lambda e: e.bn_aggr(stats[:, 12:14], stats[:, 0:12]), [b_stats], [b_stats])
                TS(stats[:, 14:15], stats[:, 13:14], LN_EPS, None, ALU.add, None, [b_stats], [b_stats])
                ACT(stats[:, 14:15], stats[:, 14:15], AF.Ln, [b_stats], [b_stats])
                ACT(stats[:, 15:16], stats[:, 14:15], AF.Exp, [b_stats], [b_stats], scale=-0.5)
                TS(zt, zt, stats[:, 12:13], stats[:, 15:16], ALU.subtract, ALU.mult, [bz, b_stats], [bz])
                TT(zt, zt, gain, ALU.mult, [bz] + bgb, [bz])
                TT(out_ap, zt, bias, ALU.add, [bz] + bgb, [bout])

            def rms_gate(o_sb, bo, extra_scale, gain_ap, bgain, gate_ap, bgate, out_bf, bout):
                o3 = o_sb.rearrange("p (h v) -> p h v", h=4)
                w0, bw0 = wk[7], b_wk[7]
                TT(w0[:], o_sb, o_sb, ALU.mult, [bo], [bw0])
                RSUM(sm[:, 0:4], w0[:].rearrange("p (h v) -> p h v", h=4), [bw0], [b_sm])
                TS(sm[:, 0:4], sm[:, 0:4], 1.0 / 128.0, RMS_EPS, ALU.mult, ALU.add, [b_sm], [b_sm])
                ACT(sm[:, 0:4], sm[:, 0:4], AF.Ln, [b_sm], [b_sm])
                ACT(sm[:, 4:8], sm[:, 0:4], AF.Exp, [b_sm], [b_sm], scale=-0.5)
                if extra_scale != 1.0:
                    TS(sm[:, 4:8], sm[:, 4:8], float(extra_scale), None, ALU.mult, None, [b_sm], [b_sm])
                TT(w0[:].rearrange("p (h v) -> p h v", h=4), o3, sm[:, 4:8].unsqueeze(2).to_broadcast([128, 4, 128]),
                   ALU.mult, [bo, b_sm], [bw0])
                if gate_ap is not None:
                    TT(w0[:], w0[:], gain_ap, ALU.mult, [bw0] + bgain, [bw0])
                    TT(out_bf, w0[:], gate_ap, ALU.mult, [bw0] + bgate, [bout])
                else:
                    TT(out_bf, w0[:], gain_ap, ALU.mult, [bw0] + bgain, [bout])

            def transpose_to(y_bf, by, dstT, bdst, col0):
                pt, bpt = PS()
                ptb = pt[:, :].bitcast(BF16)
                for c4 in range(4):
                    TR(ptb[:, c4 * 128:(c4 + 1) * 128], y_bf[:, c4 * 128:(c4 + 1) * 128], ident_bf[:],
                       [by, b_ident_bf], [bpt])
                CP("act", dstT[:, :, col0:col0 + 128], ptb[:, 0:512].rearrange("p (c t) -> p c t", c=4), [bpt], [bdst])

            for seq in range(NSEQ):
                MEMSET("dve", Sst[:], 0.0, [b_S])
                with ExitStack() as ms:
                    mems, b_mems = z, b_z
                    memb, b_memb = zb, b_zb
                    memTt = sbt(ms, "memTt%d" % seq, [128, 8, 256], BF16); b_memT = Buf()
                    for nb in range(2):
                        S.dma("sp", mems[:], mem_d[seq * 256 + nb * 128: seq * 256 + (nb + 1) * 128, :], (), [b_mems])
                        CP("dve", memb[:], mems[:], [b_mems], [b_memb])
                        pt, bpt = PS()
                        ptb = pt[:, :].bitcast(BF16)
                        for k in range(8):
                            TR(ptb[:, k * 128:(k + 1) * 128], memb[:, k * 128:(k + 1) * 128], ident_bf[:],
                               [b_memb, b_ident_bf], [bpt])
                        CP("act", memTt[:, :, nb * 128:(nb + 1) * 128], ptb[:, :].rearrange("p (k t) -> p k t", k=8),
                           [bpt], [b_memT])
                    wt, bw = load_chunk(18)
                    w3 = wt[:, 0:4096].rearrange("p (k c) -> p k c", k=8)
                    pt, bpt = PS()
                    pt2, bpt2 = PS()
                    for h in range(4):
                        po = (pt if h < 2 else pt2)
                        bpo = (bpt if h < 2 else bpt2)
                        for k in range(8):
                            MM(po[:, (h % 2) * 256:(h % 2) * 256 + 256], w3[:, k, h * 128:(h + 1) * 128], memTt[:, k, :],
                               k == 0, k == 7, [bw, b_memT], [bpo])
                    CP("act", mkT[:, 0:2, :], pt[:, :].rearrange("p (h n) -> p h n", h=2), [bpt], [b_mkT])
                    CP("act", mkT[:, 2:4, :], pt2[:, :].rearrange("p (h n) -> p h n", h=2), [bpt2], [b_mkT])
                    wt, bw = load_chunk(19)
                    w3 = wt[:, 0:4096].rearrange("p (k c) -> p k c", k=8)
                    for nb in range(2):
                        pt, bpt = PS()
                        for k in range(8):
                            MM(pt[:, :], memTt[:, k, nb * 128:(nb + 1) * 128], w3[:, k, :], k == 0, k == 7,
                               [bw, b_memT], [bpt])
                        CP("act", mvc[:, nb, :, 0:128], pt[:, :].rearrange("p (h v) -> p h v", h=4), [bpt], [b_mvc])
                    S.barrier()

                for st_i in range(TPS // STT):
                    tiles = [st_i * STT + tt for tt in range(STT)]
                    gt = [seq * TPS + t for t in tiles]
                    for tt in range(STT):
                        r0 = gt[tt] * 128
                        S.dma("sp", xs[:, tt, :], x_d[r0:r0 + 128, :], (), [b_xs[tt]])
                        CP("dve", xbf[:], xs[:, tt, :], [b_xs[tt]], [b_xbf])
                        pt, bpt = PS()
                        ptb = pt[:, :].bitcast(BF16)
                        for k in range(8):
                            TR(ptb[:, k * 128:(k + 1) * 128], xbf[:, k * 128:(k + 1) * 128], ident_bf[:],
                               [b_xbf, b_ident_bf], [bpt])
                        CP("act", xT[:, :, tt * 128:(tt + 1) * 128], ptb[:, :].rearrange("p (k t) -> p k t", k=8),
                           [bpt], [b_xT])
                    for c in range(8):
                        wt, bw = load_chunk(c)
                        w3 = wt[:, 0:4096].rearrange("p (k c) -> p k c", k=8)
                        if c == 7:
                            for h in range(4):
                                pt, bpt = PS()
                                for k in range(8):
                                    MM(pt[:, 0:TW], w3[:, k, h * 128:(h + 1) * 128], xT[:, k, :], k == 0, k == 7,
                                       [bw, b_xT], [bpt])
                                CP("act", mqT[:, h, :], pt[:, 0:TW], [bpt], [b_mqT])
                            continue
                        for tt in range(STT):
                            pt, bpt = PS()
                            for k in range(8):
                                MM(pt[:, :], xT[:, k, tt * 128:(tt + 1) * 128], w3[:, k, :], k == 0, k == 7,
                                   [bw, b_xT], [bpt])
                            if c == 0:
                                ACT(sq[:, tt, :], pt[:, :], AF.Silu, [bpt], [b_sq[tt]])
                            elif c == 1:
                                ACT(sg[:, tt, :], pt[:, :], AF.Sigmoid, [bpt], [b_sg[tt]])
                            elif c == 2:
                                CP("act", vhg[:, tt, :], pt[:, :], [bpt], [b_vhg[tt]])
                            elif c == 3:
                                ACT(gsl[:, tt, :], pt[:, :], AF.Silu, [bpt], [b_gsl[tt]])
                            elif c in (4, 5):
                                ti = gt[tt]
                                sc = 0.125 if c == 4 else 1.0
                                r_f, br_f = wk[0], b_wk[0]
                                r_b, br_b = wkb[0], b_wkb[0]
                                ACT(r_f[:], pt[:, :], AF.Copy, [bpt], [br_f], scale=sc)
                                CP("dve", r_b[:], r_f[:], [br_f], [br_b])
                                f3 = r_f[:].rearrange("p (g d) -> p g d", g=8)
                                o3 = r_b[:].rearrange("p (g d) -> p g d", g=8)
                                sn = trig[:, 0, ti, :].unsqueeze(1).to_broadcast([128, 8, 8])
                                cs = trig[:, 1, ti, :].unsqueeze(1).to_broadcast([128, 8, 8])
                                t_a, bt_a = wk[1], b_wk[1]
                                a3 = t_a[:, 0:64].rearrange("p (g d) -> p g d", g=8)
                                b3 = t_a[:, 64:128].rearrange("p (g d) -> p g d", g=8)
                                TT(a3, f3[:, :, 0:8], cs, ALU.mult, [br_f, b_trig], [bt_a])
                                TT(b3, f3[:, :, 8:16], sn, ALU.mult, [br_f, b_trig], [bt_a])
                                TT(o3[:, :, 0:8], a3, b3, ALU.subtract, [bt_a, br_b], [br_b])
                                TT(a3, f3[:, :, 8:16], cs, ALU.mult, [br_f, b_trig, bt_a], [bt_a])
                                TT(b3, f3[:, :, 0:8], sn, ALU.mult, [br_f, b_trig, bt_a], [bt_a])
                                TT(o3[:, :, 8:16], a3, b3, ALU.add, [bt_a, br_b], [br_b])
                                if c == 4:
                                    transpose_to(r_b, br_b, qT, b_qT[tt], tt * 128)
                                else:
                                    transpose_to(r_b, br_b, kc, b_kc[tiles[tt]], tiles[tt] * 128)
                            elif c == 6:
                                CP("act", vc[:, tiles[tt], :, 0:128], pt[:, :].rearrange("p (h v) -> p h v", h=4),
                                   [bpt, b_vc1], [b_vc[tiles[tt]]])

                    for tt in range(STT):
                        ti = tiles[tt]
                        c0 = tt * 128
                        fg, bfg = wk[0], b_wk[0]
                        lf, blf = wk[1], b_wk[1]
                        TT(fg[:], sg[:, tt, :], oml[:], ALU.mult, [b_sg[tt], b_oml], [bfg])
                        TT(fg[:], fg[:], lbv[:], ALU.add, [bfg, b_lbv], [bfg])
                        ACT(lf[:], fg[:], AF.Ln, [bfg], [blf])
                        pD, bpD = PS()
                        MM(pD[:, :], dmat[:], lf[:], True, True, [b_dmat, blf], [bpD])
                        eD, beD = wk[2], b_wk[2]
                        eDn, beDn = wk[3], b_wk[3]
                        ACT(eD[:], pD[:, :], AF.Exp, [bpD], [beD])
                        ACT(eDn[:], pD[:, :], AF.Exp, [bpD], [beDn], scale=-1.0)
                        TS(fg[:], fg[:], -1.0, 1.0, ALU.mult, ALU.add, [bfg], [bfg])
                        kt, bkt = wkb[0], b_wkb[0]
                        qt, bqt = wkb[1], b_wkb[1]
                        TT(kt[:], fg[:], eD[:], ALU.mult, [bfg, beD], [bkt])
                        TT(qt[:], sq[:, tt, :], eDn[:], ALU.mult, [b_sq[tt], beDn], [bqt])
                        kqT, bkqT = wkb[2], b_wkb[2]
                        qqT, bqqT = wkb[3], b_wkb[3]
                        for (src, bsrc, dst, bdst) in ((kt, bkt, kqT, bkqT), (qt, bqt, qqT, bqqT)):
                            pt, bpt = PS()
                            ptb = pt[:, :].bitcast(BF16)
                            for h in range(4):
                                TR(ptb[:, h * 128:(h + 1) * 128], src[:, h * 128:(h + 1) * 128], ident_bf[:],
                                   [bsrc, b_ident_bf], [bpt])
                            CP("act", dst[:], ptb[:, 0:512], [bpt], [bdst])
                        pc, bpc = PS()
                        for h in range(4):
                            MM(pc[:, h * 2:h * 2 + 2], lf[:, h * 128:(h + 1) * 128], cols2[:], True, True,
                               [blf, b_cols2], [bpc])
                        ex = sm[:, 8:20].rearrange("p (h c) -> p h c", h=4)
                        pc3 = pc[:, 0:8].rearrange("p (h c) -> p h c", h=4)
                        CP("dve", ex[:, :, 0:2], pc3, [bpc], [b_sm])
                        TT(ex[:, :, 2:3], ex[:, :, 0:1], ex[:, :, 1:2], ALU.subtract, [b_sm], [b_sm])
                        ACT(sm[:, 8:20], sm[:, 8:20], AF.Exp, [b_sm], [b_sm])
                        po, bpo = ACC[0]
                        pU, bpU = ACC[1]
                        hsc = []
                        for h in range(4):
                            hs = slice(h * 128, (h + 1) * 128)
                            psc, bpsc = PS()
                            MM(psc[:, 0:128], kqT[:, hs], qqT[:, hs], True, True, [bkqT, bqqT], [bpsc])
                            hsc.append((psc, bpsc))
                        hat = []
                        for h in range(4):
                            psc, bpsc = hsc[h]
                            AT, bAT = pT[psel["n"] % NPT], b_pT[psel["n"] % NPT]
                            psel["n"] += 1
                            TT(AT[:, 0:128], psc[:, 0:128], maskT[:], ALU.mult, [bpsc, b_maskT], [bAT])
                            hat.append((AT, bAT))
                        for h in range(4):
                            TS(Smm[:, h, :], Sst[:, h, :], ex[:, h, 1:2], None, ALU.mult, None, [b_S, b_sm], [b_Smm])
                        for h in range(4):
                            hs = slice(h * 128, (h + 1) * 128)
                            AT, bAT = hat[h]
                            MM(pU[:, hs], kt[:, hs], vhg[:, tt, hs], True, True, [bkt, b_vhg[tt]], [bpU])
                            MM(po[:, hs], AT[:, 0:128], vhg[:, tt, hs], True, False, [bAT, b_vhg[tt]], [bpo])
                            MM(po[:, hs], qqT[:, hs], Smm[:, h, :], False, True, [bqqT, b_Smm], [bpo])
                        for h in range(4):
                            hs = slice(h * 128, (h + 1) * 128)
                            TS(Sst[:, h, :], Sst[:, h, :], ex[:, h, 0:1], None, ALU.mult, None, [b_S, b_sm], [b_S])
                            SCTT(Sst[:, h, :], pU[:, hs], ex[:, h, 2:3], Sst[:, h, :], ALU.mult, ALU.add,
                                [bpU, b_sm, b_S], [b_S])
                        o_sb, bo_sb = wk[4], b_wk[4]
                        CP("act", o_sb[:], po[:, :], [bpo], [bo_sb])
                        y_bf, by_bf = wkb[4], b_wkb[4]
                        rms_gate(o_sb[:], bo_sb, 1.0, hg_gain, [b_vec1], gsl[:, tt, :], [b_gsl[tt]], y_bf[:], by_bf)
                        if debug:
                            CP("dve", wk[5][:], y_bf[:], [by_bf], [b_wk[5]])
                            S.dma("sp", dbg_hg[gt[tt] * 128:(gt[tt] + 1) * 128, :], wk[5][:], [b_wk[5]], [Buf()])
                        transpose_to(y_bf, by_bf, yT[0], b_yT[0][tt], c0)

                        od, bod = wk[4], b_wk[4]
                        nkb = ti + 1
                        tasks = []
                        for h in range(4):
                            for m in range(2):
                                for g0 in range(0, nkb, 4):
                                    tasks.append((h, m, g0, min(4, nkb - g0)))

                        def att_a(task):
                            h, m, g0, ng = task
                            ps_ = slice(m * 64, (m + 1) * 64)
                            psc, bpsc = PS()
                            for jj in range(ng):
                                j = g0 + jj
                                MM(psc[:, jj * 128:(jj + 1) * 128], kc[ps_, h, j * 128:(j + 1) * 128],
                                   qT[ps_, h, c0:c0 + 128], True, True, [b_kc[j], b_qT[tt]], [bpsc])
                            P, bP = pT[psel["n"] % NPT], b_pT[psel["n"] % NPT]
                            psel["n"] += 1
                            ACT(P[:, 0:ng * 128], psc[:, 0:ng * 128], AF.Exp, [bpsc], [bP])
                            if g0 + ng == nkb:
                                dsl = slice((ng - 1) * 128, ng * 128)
                                TT(P[:, dsl], P[:, dsl], maskT[:], ALU.mult, [bP, b_maskT], [bP])
                            return P, bP

                        def att_b(task, P, bP):
                            h, m, g0, ng = task
                            pacc, bpacc = ACC[2] if h % 2 == 0 else ACC[0]
                            for jj in range(ng):
                                j = g0 + jj
                                MM(pacc[:, m * 130:(m + 1) * 130], P[:, jj * 128:(jj + 1) * 128], vc[:, j, h, :],
                                   j == 0, j == nkb - 1, [bP, b_vc[j], b_vc1], [bpacc])
                            if m == 1 and g0 + ng == nkb:
                                rr = sm[:, 24:28]
                                RECIP(rr[:, 0:1], pacc[:, 128:129], [bpacc], [b_sm])
                                RECIP(rr[:, 1:2], pacc[:, 258:259], [bpacc], [b_sm])
                                TT(rr[:, 2:3], rr[:, 1:2], lam_ap, ALU.mult, [b_sm, b_lam], [b_sm])
                                t2, bt2 = wk[5], b_wk[5]
                                TS(t2[:, 0:128], pacc[:, 130:258], rr[:, 2:3], None, ALU.mult, None, [bpacc, b_sm], [bt2])
                                SCTT(od[:, h * 128:(h + 1) * 128], pacc[:, 0:128], rr[:, 0:1], t2[:, 0:128], ALU.mult,
                                     ALU.subtract, [bpacc, b_sm, bt2], [bod])

                        inflight = []
                        for task in tasks:
                            inflight.append((task,) + att_a(task))
                            if len(inflight) > LOOKAHEAD:
                                t0_, P0_, bP0_ = inflight.pop(0)
                                att_b(t0_, P0_, bP0_)
                        for (t0_, P0_, bP0_) in inflight:
                            att_b(t0_, P0_, bP0_)
                        y_bf, by_bf = wkb[4], b_wkb[4]
                        rms_gate(od[:], bod, 0.8, df_gain, [b_vec1], None, None, y_bf[:], by_bf)
                        if debug:
                            CP("dve", wk[5][:], y_bf[:], [by_bf], [b_wk[5]])
                            S.dma("sp", dbg_df[gt[tt] * 128:(gt[tt] + 1) * 128, :], wk[5][:], [b_wk[5]], [Buf()])
                        transpose_to(y_bf, by_bf, yT[1], b_yT[1][tt], c0)

                        ym, bym = wk[4], b_wk[4]
                        for hp in range(2):
                            psc, bpsc = PS()
                            for hh in range(2):
                                h = hp * 2 + hh
                                for nb in range(2):
                                    MM(psc[:, (hh * 2 + nb) * 128:(hh * 2 + nb + 1) * 128], mkT[:, h, nb * 128:(nb + 1) * 128],
                                       mqT[:, h, c0:c0 + 128], True, True, [b_mkT, b_mqT], [bpsc])
                            P, bP = pT[psel["n"] % NPT], b_pT[psel["n"] % NPT]
                            psel["n"] += 1
                            ACT(P[:, :], psc[:, :], AF.Exp, [bpsc], [bP], scale=128.0 ** -0.5)
                            pacc, bpacc = ACC[1]
                            for hh in range(2):
                                h = hp * 2 + hh
                                for nb in range(2):
                                    MM(pacc[:, hh * 130:(hh + 1) * 130], P[:, (hh * 2 + nb) * 128:(hh * 2 + nb + 1) * 128],
                                       mvc[:, nb, h, :], nb == 0, nb == 1, [bP, b_mvc, b_mvc1], [bpacc])
                            rr = sm[:, 28:30]
                            RECIP(rr[:, 0:1], pacc[:, 128:129], [bpacc], [b_sm])
                            RECIP(rr[:, 1:2], pacc[:, 258:259], [bpacc], [b_sm])
                            for hh in range(2):
                                h = hp * 2 + hh
                                TS(ym[:, h * 128:(h + 1) * 128], pacc[:, hh * 130:hh * 130 + 128], rr[:, hh:hh + 1], None,
                                   ALU.mult, None, [bpacc, b_sm], [bym])
                        y_bf, by_bf = wkb[4], b_wkb[4]
                        CP("dve", y_bf[:], ym[:], [bym], [by_bf])
                        if debug:
                            S.dma("sp", dbg_mem[gt[tt] * 128:(gt[tt] + 1) * 128, :], ym[:], [bym], [Buf()])
                        transpose_to(y_bf, by_bf, yT[2], b_yT[2][tt], c0)

                    rd_y = [b for br in range(3) for b in b_yT[br]]
                    for cg in range(8):
                        wt, bw = load_chunk(8 + cg)
                        wg3 = wt[:, 0:3072].rearrange("p (k b c) -> p k b c", k=8, b=3)
                        wb3 = wt[:, 3072:4608].rearrange("p (k b c) -> p k b c", k=4, b=3)
                        acc, bacc = wk[6], b_wk[6]
                        for br in range(3):
                            pg, bpg = PS()
                            for k in range(8):
                                MM(pg[:, 0:TW], wg3[:, k, br, :], xT[:, k, :], k == 0, k == 7, [bw, b_xT], [bpg])
                            gt_, bgt_ = wk[br], b_wk[br]
                            ACT(gt_[:, 0:TW], pg[:, 0:TW], AF.Sigmoid, [bpg], [bgt_])
                            pb, bpb = PS()
                            for k in range(4):
                                MM(pb[:, 0:TW], wb3[:, k, br, :], yT[br][:, k, :], k == 0, k == 3, [bw] + rd_y, [bpb])
                            if br == 0:
                                TT(acc[:, 0:TW], pb[:, 0:TW], gt_[:, 0:TW], ALU.mult, [bpb, bgt_], [bacc])
                            else:
                                TT(gt_[:, 0:TW], pb[:, 0:TW], gt_[:, 0:TW], ALU.mult, [bpb, bgt_], [bgt_])
                                if br == 1:
                                    TT(acc[:, 0:TW], acc[:, 0:TW], gt_[:, 0:TW], ALU.add, [bacc, bgt_], [bacc])
                                else:
                                    TT(mergedT[:, cg, :], acc[:, 0:TW], gt_[:, 0:TW], ALU.add, [bacc, bgt_], [b_mg[cg]])

                    zts = [(z, b_z), (z_b, b_zB)]
                    for n in range(2):
                        wt, bw = load_chunk(16 + n)
                        w3 = wt[:, 0:4096].rearrange("p (k c) -> p k c", k=8)
                        for tt in range(STT):
                            zt_, bzt_ = zts[tt]
                            pt, bpt = PS()
                            for k in range(8):
                                MM(pt[:, :], mergedT[:, k, tt * 128:(tt + 1) * 128], w3[:, k, :], k == 0, k == 7,
                                   [bw] + b_mg, [bpt])
                            SCTT(zt_[:, n * 512:(n + 1) * 512], xs[:, tt, n * 512:(n + 1) * 512], ALPHA, pt[:, :],
                                 ALU.mult, ALU.add, [b_xs[tt], bpt], [bzt_])
                    for tt in range(STT):
                        g = gt[tt]
                        zc, b_zc = zts[tt]
                        layer_norm(zc[:], b_zc, ln1g, ln1b, [b_vec1], zc[:], b_zc)
                        S.dma("sp", x1_d[g * 128:(g + 1) * 128, :], zc[:], [b_zc], [b_x1[g]])
                        CP("act", zb[:], zc[:], [b_zc], [b_zb])
                        S.dma("sp", x1b_d[g * 128:(g + 1) * 128, :], zb[:], [b_zb], [b_x1b[g]])
                        for half in range(2):
                            pt, bpt = PS()
                            for k4 in range(4):
                                k = half * 4 + k4
                                TR(pt[:, k4 * 128:(k4 + 1) * 128], zc[:, k * 128:(k + 1) * 128], ident_f[:],
                                   [b_zc, b_ident_f], [bpt])
                            CP("act", x1T[:, half * 4:(half + 1) * 4, :], pt[:, :].rearrange("p (k t) -> p k t", k=4),
                               [bpt], [b_x1T])
                        pr, bpr = PS()
                        for k in range(8):
                            MM(pr[:, 0:36], x1T[:, k, :], wr_sb[:, k, :], k == 0, k == 7, [b_x1T, b_wr], [bpr])
                        CP("dve", rl[:, 0:36], pr[:, 0:36], [bpr], [b_rl])
                        gl = rl[:, 0:4]
                        el = rl[:, 4:36]
                        RMAX(rl[:, 40:41], gl, [b_rl], [b_rl])
                        TS(rl[:, 44:48], gl, rl[:, 40:41], None, ALU.subtract, None, [b_rl], [b_rl])
                        ACT(rl[:, 44:48], rl[:, 44:48], AF.Exp, [b_rl], [b_rl])
                        RSUM(rl[:, 41:42], rl[:, 44:48], [b_rl], [b_rl])
                        RECIP(rl[:, 42:43], rl[:, 41:42], [b_rl], [b_rl])
                        TS(rl[:, 48:52], gl, rl[:, 40:41], None, ALU.is_ge, None, [b_rl], [b_rl])
                        TS(rl[:, 48:52], rl[:, 48:52], -1.0, 1e30, ALU.add, ALU.mult, [b_rl], [b_rl])
                        TT(rl[:, 64:96].rearrange("p (g j) -> p g j", g=4), el.rearrange("p (g j) -> p g j", g=4),
                           rl[:, 48:52].unsqueeze(2).to_broadcast([128, 4, 8]), ALU.add, [b_rl], [b_rl])
                        S.op("dve", lambda e: e.max(rl[:, 96:104], rl[:, 64:96]), [b_rl], [b_rl])
                        TS(oh[:, 0:32], rl[:, 64:96], rl[:, 96:97], None, ALU.is_equal, None, [b_rl], [b_oh])
                        TS(oh[:, 32:64], rl[:, 64:96], rl[:, 97:98], None, ALU.is_equal, None, [b_rl], [b_oh])
                        TT(oh[:, 64:96], oh[:, 0:32], oh[:, 32:64], ALU.add, [b_oh], [b_oh])
                        CP("dve", ohb[:], oh[:, 64:96], [b_oh], [b_ohb])
                        rtt = rt[:, g, :]
                        w0, bw0 = wk[7], b_wk[7]
                        TT(w0[:, 0:32], oh[:, 0:32], e_iota[:], ALU.mult, [b_oh, b_eiota], [bw0])
                        RSUM(rtt[:, 0:1], w0[:, 0:32], [bw0], [b_rt[g]])
                        TT(w0[:, 32:64], oh[:, 32:64], e_iota[:], ALU.mult, [b_oh, b_eiota], [bw0])
                        RSUM(rtt[:, 1:2], w0[:, 32:64], [bw0], [b_rt[g]])
                        TT(rl[:, 43:44], rl[:, 97:98], rl[:, 96:97], ALU.subtract, [b_rl], [b_rl])
                        ACT(rl[:, 43:44], rl[:, 43:44], AF.Exp, [b_rl], [b_rl])
                        TS(rl[:, 43:44], rl[:, 43:44], 1.0, None, ALU.add, None, [b_rl], [b_rl])
                        RECIP(rl[:, 43:44], rl[:, 43:44], [b_rl], [b_rl])
                        TT(rtt[:, 2:3], rl[:, 43:44], rl[:, 42:43], ALU.mult, [b_rl], [b_rt[g]])
                        TT(rtt[:, 3:4], rl[:, 42:43], rtt[:, 2:3], ALU.subtract, [b_rl, b_rt[g]], [b_rt[g]])
                        pk, bpk = PS()
                        MM(pk[:, 0:32], lstrict[:], ohb[:], True, True, [b_lstrict, b_ohb], [bpk])
                        MM(pk[:, 32:64], ones_bf[:], ohb[:], True, True, [b_ones, b_ohb], [bpk])
                        TT(w0[:, 64:96], pk[:, 0:32], tot[:], ALU.add, [bpk, b_tot], [bw0])
                        TT(tot[:], tot[:], pk[:, 32:64], ALU.add, [bpk, b_tot], [b_tot])
                        TT(w0[:, 0:32], oh[:, 0:32], w0[:, 64:96], ALU.mult, [b_oh, bw0], [bw0])
                        RSUM(rtt[:, 4:5], w0[:, 0:32], [bw0], [b_rt[g]])
                        TT(w0[:, 32:64], oh[:, 32:64], w0[:, 64:96], ALU.mult, [b_oh, bw0], [bw0])
                        RSUM(rtt[:, 5:6], w0[:, 32:64], [bw0], [b_rt[g]])
            if debug:
                S.barrier()
                S.dma("sp", dbg_kc[:, :], kc[:].rearrange("p a b -> p (a b)"), (), [Buf()])
                S.dma("sp", dbg_vc[:, :], vc[:].rearrange("p a b c -> p (a b c)"), (), [Buf()])
                S.dma("sp", dbg_v1[:, :], vec1[:], (), [Buf()])
            S.barrier()

        if debug:
            fin = Buf()
            S.dma("sp", dbg_rt[:, :, :], rt[:], b_rt + [b_tot], [fin])

        if phases >= 2:
            with ExitStack() as p2:
                vec2 = sbt(p2, "vec2_sb", [128, 2048], F32); b_vec2 = Buf()
                S.dma("sp", vec2[:], v2_d[:, :], (), [b_vec2])
                ln2g = vec2[:, 0:1024]
                ln2b = vec2[:, 1024:2048]
                big = sbt(p2, "big", [128, 32 * 160], F32); b_big = Buf()
                rows = sbt(p2, "rows", [128, 512], F32); b_rows = Buf()
                irow = sbt(p2, "irow", [128, 160], I32); b_irow = Buf()
                bexp = sbt(p2, "bexp", [128, 160], I32); b_bexp = Buf()
                dst_i = sbt(p2, "dst_i", [128, NT * 2], I32); b_dst = Buf()
                stats_2 = sbt(p2, "stats2", [128, 16], F32); b_stats2 = Buf()
                S.op("pool", lambda e: e.iota(irow[:], pattern=[[128, 160]], base=0, channel_multiplier=0), (), [b_irow])
                CP("dve", rows[:, 128:288], irow[:], [b_irow], [b_rows])
                TT(big[:, 0:2048].rearrange("p (e k) -> p e k", e=32),
                   tot[:].unsqueeze(2).to_broadcast([128, 32, 64]),
                   rows[:, 128:192].unsqueeze(1).to_broadcast([128, 32, 64]), ALU.is_gt, [b_tot, b_rows], [b_big])
                RSUM(rows[:, 32:64], big[:, 0:2048].rearrange("p (e k) -> p e k", e=32), [b_big], [b_rows])
                TS(rows[:, 32:64], rows[:, 32:64], 128.0, None, ALU.mult, None, [b_rows], [b_rows])
                MEMSET("dve", rows[:, 352:384], 1.0, [b_rows])
                S.op("dve", lambda e: e.tensor_tensor_scan(rows[:, 64:96], rows[:, 352:384], rows[:, 32:64], 0.0,
                                                           ALU.mult, ALU.add), [b_rows], [b_rows])
                TT(rows[:, 96:128], rows[:, 64:96], rows[:, 32:64], ALU.subtract, [b_rows], [b_rows])
                TT(big[:].rearrange("p (b e) -> p b e", b=160),
                   rows[:, 64:96].unsqueeze(1).to_broadcast([128, 160, 32]),
                   rows[:, 128:288].unsqueeze(2).to_broadcast([128, 160, 32]), ALU.is_le, [b_rows], [b_big])
                RSUM(rows[:, 288:448], big[:].rearrange("p (b e) -> p b e", b=160), [b_big], [b_rows])
                TS(rows[:, 288:448], rows[:, 288:448], 31.0, None, ALU.min, None, [b_rows], [b_rows])
                CP("dve", bexp[:], rows[:, 288:448], [b_rows], [b_bexp])
                pio_i = sbt(p2, "pio_i", [128, 1], I32); b_pio = Buf()
                pio_f = sbt(p2, "pio_f", [128, 1], F32)
                idxw_f = sbt(p2, "idxw_f", [128, 160], F32); b_idxwf = Buf()
                idxw = sbt(p2, "idxw", [128, 160], I32); b_idxw = Buf()
                S.op("pool", lambda e: e.iota(pio_i[:], pattern=[[0, 1]], base=0, channel_multiplier=1), (), [b_pio])
                CP("dve", pio_f[:], pio_i[:], [b_pio], [b_pio])
                same_f = sbt(p2, "same_f", [128, 160], F32); b_same = Buf()
                t1_f = sbt(p2, "t1_f", [128, 160], F32); b_t1 = Buf()
                MEMSET("dve", same_f[:], 0.0, [b_same])
                TT(same_f[:, 3:160], rows[:, 291:448], rows[:, 288:445], ALU.is_equal, [b_rows, b_same], [b_same])
                TS(t1_f[:], rows[:, 288:448], 128.0, None, ALU.mult, None, [b_rows], [b_t1])
                TS(idxw_f[:], t1_f[:], -1.0, 4096.0, ALU.mult, ALU.add, [b_t1], [b_idxwf])
                TT(idxw_f[:], idxw_f[:], same_f[:], ALU.mult, [b_idxwf, b_same], [b_idxwf])
                TT(idxw_f[:], idxw_f[:], t1_f[:], ALU.add, [b_idxwf, b_t1], [b_idxwf])
                TS(idxw_f[:], idxw_f[:], pio_f[:, 0:1], None, ALU.add, None, [b_idxwf, b_pio], [b_idxwf])
                CP("dve", idxw[:], idxw_f[:], [b_idxwf], [b_idxw])
                w0 = sbt(p2, "w0", [128, 64], F32); b_w0 = Buf()
                for g in range(NT):
                    for j in range(2):
                        TS(w0[:, 0:32], e_iota[:], rt[:, g, j:j + 1], None, ALU.is_equal, None, [b_eiota, b_rt[g]], [b_w0])
                        TT(w0[:, 0:32], w0[:, 0:32], rows[:, 96:128], ALU.mult, [b_w0, b_rows], [b_w0])
                        RSUM(rt[:, g, 6 + j:7 + j], w0[:, 0:32], [b_w0], [b_rt[g]])
                        TT(rt[:, g, 6 + j:7 + j], rt[:, g, 6 + j:7 + j], rt[:, g, 4 + j:5 + j], ALU.add, [b_rt[g]], [b_rt[g]])
                CP("dve", dst_i[:].rearrange("p (g j) -> p g j", j=2), rt[:, :, 6:8], b_rt, [b_dst])

                dyn = {}

                def bound(e):
                    if "bnd" not in dyn:
                        breg = e.alloc_register("bndreg")
                        e.reg_mov(breg, NSLOT - 1)
                        dyn["bnd"] = e.snap(breg)
                    return dyn["bnd"]

                def boundw(e):
                    if "bndw" not in dyn:
                        breg = e.alloc_register("bndwreg")
                        e.reg_mov(breg, NE * 128 - 1)
                        dyn["bndw"] = e.snap(breg)
                    return dyn["bndw"]

                b_xs_sc = []
                xg = [sbt(p2, "xg%d" % i, [128, D], BF16) for i in range(2)]
                b_xg = [Buf() for _ in range(2)]
                for g in range(NT):
                    k = g % 2
                    S.dma("sp", xg[k][:], x1b_d[g * 128:(g + 1) * 128, :], [b_x1b[g]], [b_xg[k]])
                    for j in range(2):
                        bsc = Buf()
                        S.dma_fn("pool", (lambda e, g=g, j=j, k=k: e.indirect_dma_start(
                            out=xs_d[:, :], out_offset=bass.IndirectOffsetOnAxis(ap=dst_i[:, g * 2 + j:g * 2 + j + 1], axis=0),
                            in_=xg[k][:, :], in_offset=None, bounds_check=bound(e), oob_is_err=False)),
                            [b_xg[k], b_dst] + b_xs_zero, [bsc])
                        b_xs_sc.append(bsc)

                NW = 3
                wgb = [sbt(p2, "wgb%d" % i, [128, 8, 512], BF16) for i in range(NW)]
                wub = [sbt(p2, "wub%d" % i, [128, 8, 512], BF16) for i in range(NW)]
                wdb = [sbt(p2, "wdb%d" % i, [128, 4, D], BF16) for i in range(NW)]
                b_wgb = [Buf() for _ in range(NW)]
                b_wub = [Buf() for _ in range(NW)]
                b_wdb = [Buf() for _ in range(NW)]
                xb = [sbt(p2, "xb%d" % i, [128, D], BF16) for i in range(2)]
                b_xb = [Buf() for _ in range(2)]
                xbT = [sbt(p2, "xbT%d" % i, [128, 8, 128], BF16) for i in range(2)]
                b_xbT = [Buf() for _ in range(2)]
                hs_ = [sbt(p2, "hs%d" % i, [128, 512], F32) for i in range(2)]
                b_hs = [Buf() for _ in range(2)]
                hact = [sbt(p2, "hact%d" % i, [128, 512], BF16) for i in range(2)]
                b_hact = [Buf() for _ in range(2)]
                hT = [sbt(p2, "hT%d" % i, [128, 4, 128], BF16) for i in range(2)]
                b_hT = [Buf() for _ in range(2)]
                yb = [sbt(p2, "yb%d" % i, [128, D], F32) for i in range(2)]
                b_yb = [Buf() for _ in range(2)]
                b_ys = [Buf() for _ in range(NBLK)]

                def moe_gather(b):
                    k = b % NW
                    for (dst, bdst, src) in ((wgb[k], b_wgb[k], wg_d), (wub[k], b_wub[k], wu_d), (wdb[k], b_wdb[k], wd_d)):
                        dflat = dst[:].rearrange("p a f -> p (a f)")
                        for hf in range(2):
                            S.dma_fn("pool", (lambda e, dflat=dflat, src=src, hf=hf, b=b: e.indirect_dma_start(
                                out=dflat[:, hf * 2048:(hf + 1) * 2048], out_offset=None,
                                in_=src[hf][:, :],
                                in_offset=bass.IndirectOffsetOnAxis(ap=idxw[:, b:b + 1], axis=0),
                                bounds_check=boundw(e), oob_is_err=False)), [b_idxw], [bdst])

                def moe_a(b):
                    k = b % NW
                    j = b % 2
                    S.dma("sp", xb[j][:], xs_d[b * 128:(b + 1) * 128, :], b_xs_sc if b == 0 else [], [b_xb[j]])
                    pt, bpt = PS()
                    ptb = pt[:, :].bitcast(BF16)
                    for kk in range(8):
                        TR(ptb[:, kk * 128:(kk + 1) * 128], xb[j][:, kk * 128:(kk + 1) * 128], ident_bf[:],
                           [b_xb[j], b_ident_bf], [bpt])
                    CP("act", xbT[j][:], ptb[:, :].rearrange("p (k t) -> p k t", k=8), [bpt], [b_xbT[j]])
                    pg, bpg = PS()
                    pu, bpu = PS()
                    for kk in range(8):
                        MM(pg[:, :], xbT[j][:, kk, :], wgb[k][:, kk, :], kk == 0, kk == 7, [b_xbT[j], b_wgb[k]], [bpg])
                    for kk in range(8):
                        MM(pu[:, :], xbT[j][:, kk, :], wub[k][:, kk, :], kk == 0, kk == 7, [b_xbT[j], b_wub[k]], [bpu])
                    ACT(hs_[j][:], pg[:, :], AF.Silu, [bpg], [b_hs[j]])
                    TT(hact[j][:], hs_[j][:], pu[:, :], ALU.mult, [b_hs[j], bpu], [b_hact[j]])

                def moe_b(b):
                    k = b % NW
                    j = b % 2
                    pt, bpt = PS()
                    ptb = pt[:, :].bitcast(BF16)
                    for kk in range(4):
                        TR(ptb[:, kk * 128:(kk + 1) * 128], hact[j][:, kk * 128:(kk + 1) * 128], ident_bf[:],
                           [b_hact[j], b_ident_bf], [bpt])
                    CP("act", hT[j][:], ptb[:, 0:512].rearrange("p (k t) -> p k t", k=4), [bpt], [b_hT[j]])
                    for n in range(2):
                        py, bpy = PS()
                        for kk in range(4):
                            MM(py[:, :], hT[j][:, kk, :], wdb[k][:, kk, n * 512:(n + 1) * 512], kk == 0, kk == 3,
                               [b_hT[j], b_wdb[k]], [bpy])
                        CP("act" if n == 0 else "dve", yb[j][:, n * 512:(n + 1) * 512], py[:, :], [bpy], [b_yb[j]])
                    S.dma("sp", ys_d[b * 128:(b + 1) * 128, :], yb[j][:], [b_yb[j]], [b_ys[b]])

                moe_gather(0)
                moe_gather(1)
                for b in range(NBLK):
                    moe_a(b)
                    if b >= 1:
                        moe_b(b - 1)
                    if b + 2 < NBLK:
                        moe_gather(b + 2)
                moe_b(NBLK - 1)

                y1 = [sbt(p2, "y1_%d" % i, [128, D], F32) for i in range(2)]
                y2 = [sbt(p2, "y2_%d" % i, [128, D], F32) for i in range(2)]
                xr = [sbt(p2, "xr%d" % i, [128, D], F32) for i in range(2)]
                b_y1 = [Buf() for _ in range(2)]
                b_y2 = [Buf() for _ in range(2)]
                b_xr = [Buf() for _ in range(2)]
                b_out = []

                def layer_norm2(zt, bz, out_ap, bout):
                    for hlf in range(2):
                        S.op("dve", lambda e, hlf=hlf: e.bn_stats(stats_2[:, hlf * 6:(hlf + 1) * 6], zt[:, hlf * 512:(hlf + 1) * 512]),
                             [bz], [b_stats2])
                    S.op("dve", lambda e: e.bn_aggr(stats_2[:, 12:14], stats_2[:, 0:12]), [b_stats2], [b_stats2])
                    TS(stats_2[:, 14:15], stats_2[:, 13:14], LN_EPS, None, ALU.add, None, [b_stats2], [b_stats2])
                    ACT(stats_2[:, 14:15], stats_2[:, 14:15], AF.Ln, [b_stats2], [b_stats2])
                    ACT(stats_2[:, 15:16], stats_2[:, 14:15], AF.Exp, [b_stats2], [b_stats2], scale=-0.5)
                    TS(zt, zt, stats_2[:, 12:13], stats_2[:, 15:16], ALU.subtract, ALU.mult, [bz, b_stats2], [bz])
                    TT(zt, zt, ln2g, ALU.mult, [bz, b_vec2], [bz])
                    TT(out_ap, zt, ln2b, ALU.add, [bz, b_vec2], [bout])

                for g in range(NT):
                    k = g % 2
                    S.dma("sp", xr[k][:], x1_d[g * 128:(g + 1) * 128, :], [b_x1[g]], [b_xr[k]])
                    for j, (yt, byt) in enumerate(((y1[k], b_y1[k]), (y2[k], b_y2[k]))):
                        S.dma_fn("pool", (lambda e, g=g, j=j, yt=yt: e.indirect_dma_start(
                            out=yt[:, :], out_offset=None, in_=ys_d[:, :],
                            in_offset=bass.IndirectOffsetOnAxis(ap=dst_i[:, g * 2 + j:g * 2 + j + 1], axis=0),
                            bounds_check=bound(e), oob_is_err=False)), [b_dst] + (b_ys if g == 0 else []), [byt])
                    TS(xr[k][:], xr[k][:], ALPHA, None, ALU.mult, None, [b_xr[k]], [b_xr[k]])
                    SCTT(xr[k][:], y1[k][:], rt[:, g, 2:3], xr[k][:], ALU.mult, ALU.add, [b_y1[k], b_rt[g], b_xr[k]], [b_xr[k]])
                    SCTT(xr[k][:], y2[k][:], rt[:, g, 3:4], xr[k][:], ALU.mult, ALU.add, [b_y2[k], b_rt[g], b_xr[k]], [b_xr[k]])
                    layer_norm2(xr[k][:], b_xr[k], xr[k][:], b_xr[k])
                    bo = Buf()
                    S.dma("sp", out_d[g * 128:(g + 1) * 128, :], xr[k][:], [b_xr[k]], [bo])
                    b_out.append(bo)
                S.barrier()
        else:
            S.barrier()
        S.barrier()
        S.emit()
        print("program: %d instructions, %d waits" % (S.n_ins, S.n_wait))
    return nc


def _weight_chunks(w_in, w_gates, wb_hg, wb_df, wb_mem, w_out, w_mem_kv):
    wf = np.zeros((NCHUNK, 128, CH), np.float32)

    def kchunks(w):
        K, C = w.shape
        return w.reshape(K // 128, 128, C).transpose(1, 0, 2)

    cols = list(range(2048))
    for h in range(4):
        cols += list(range(2048 + h * 64, 2048 + (h + 1) * 64)) + list(range(2304 + h * 64, 2304 + (h + 1) * 64))
    for h in range(4):
        cols += list(range(2560 + h * 64, 2560 + (h + 1) * 64)) + list(range(2816 + h * 64, 2816 + (h + 1) * 64))
    cols += list(range(3072, 4096))
    w_in_p = w_in[:, cols]
    for c in range(8):
        wf[c, :, 0:4096] = kchunks(w_in_p[:, c * 512:(c + 1) * 512]).reshape(128, 4096)
    wbr = [wb_hg, wb_df, wb_mem]
    for cg in range(8):
        g = np.stack([kchunks(w_gates[:, br * 1024 + cg * 128: br * 1024 + (cg + 1) * 128]) for br in range(3)], axis=2)
        wf[8 + cg, :, 0:3072] = g.reshape(128, 3072)
        bb = np.stack([kchunks(wbr[br][:, cg * 128:(cg + 1) * 128]) for br in range(3)], axis=2)
        wf[8 + cg, :, 3072:4608] = bb.reshape(128, 1536)
    for n in range(2):
        wf[16 + n, :, 0:4096] = kchunks(w_out[:, n * 512:(n + 1) * 512]).reshape(128, 4096)
        wf[18 + n, :, 0:4096] = kchunks(w_mem_kv[:, n * 512:(n + 1) * 512]).reshape(128, 4096)
    return wf


_NC_CACHE = {}


def kernel(x, mem, positions, w_in, w_gates, hgrn_lower_bounds, hgrn_norm_gain,
           diff_lambda_q1, diff_lambda_k1, diff_lambda_q2, diff_lambda_k2, diff_subln_gain,
           w_mem_kv, w_branch_hgrn, w_branch_diff, w_branch_mem, w_out, ln1_gain, ln1_bias,
           w_group_router, w_expert_router, w_expert_gate, w_expert_up, w_expert_down,
           ln2_gain, ln2_bias, _debug=False, _phases=3):
    f = lambda a: np.ascontiguousarray(np.asarray(a))
    x = f(x); mem = f(mem); positions = f(positions)
    wf = _weight_chunks(f(w_in)[0], f(w_gates)[0], f(w_branch_hgrn)[0], f(w_branch_diff)[0], f(w_branch_mem)[0],
                        f(w_out)[0], f(w_mem_kv)[0])
    wr = np.concatenate([f(w_group_router)[0], f(w_expert_router)[0]], axis=1)
    wr = np.ascontiguousarray(wr.reshape(8, 128, 36).transpose(1, 0, 2))
    rep = lambda v: np.broadcast_to(np.asarray(v, np.float32).reshape(1, -1), (128, np.asarray(v).size))
    lbr = np.ascontiguousarray(np.concatenate([rep(f(hgrn_lower_bounds)[0]), rep(f(hgrn_lower_bounds)[1])], axis=1),
                               dtype=np.float32)
    vec1 = np.ascontiguousarray(np.concatenate([
        rep(f(hgrn_norm_gain)[0]),
        rep(f(diff_lambda_q1)[0]), rep(f(diff_lambda_k1)[0]), rep(f(diff_lambda_q2)[0]), rep(f(diff_lambda_k2)[0]),
        rep(np.tile(f(diff_subln_gain)[0], 4)), rep(f(ln1_gain)[0]), rep(f(ln1_bias)[0])], axis=1), dtype=np.float32)
    vec2 = np.ascontiguousarray(np.concatenate([rep(f(ln2_gain)[0]), rep(f(ln2_bias)[0])], axis=1), dtype=np.float32)
    def halves(w, kc):
        w2 = w.reshape(NE, kc, 128, -1).transpose(0, 2, 1, 3).reshape(NE * 128, 4096)
        return [np.ascontiguousarray(w2[:, 0:2048]), np.ascontiguousarray(w2[:, 2048:4096])]
    wg = halves(f(w_expert_gate)[0], 8)
    wu = halves(f(w_expert_up)[0], 8)
    wd = halves(f(w_expert_down)[0], 4)

    key = (bool(_debug), int(_phases))
    if key not in _NC_CACHE:
        _NC_CACHE[key] = build_program(debug=_debug, phases=_phases)
    nc = _NC_CACHE[key]
    in_maps = []
    for c in range(NCORES):
        xb = x[c * NSEQ:(c + 1) * NSEQ].reshape(NTOK, D)
        mb = mem[c * NSEQ:(c + 1) * NSEQ].reshape(NSEQ * 256, D)
        pb = positions[c * NSEQ:(c + 1) * NSEQ].reshape(NT, 128).T
        in_maps.append(dict(x=np.ascontiguousarray(xb), mem=np.ascontiguousarray(mb),
                            pos=np.ascontiguousarray(pb.astype(np.int32)), wf=wf, wr=wr, vec1=vec1, vec2=vec2, lbr=lbr,
                            wg0=wg[0], wg1=wg[1], wu0=wu[0], wu1=wu[1], wd0=wd[0], wd1=wd[1]))
    res = run_bass_kernel_spmd(nc, in_maps, core_ids=list(range(NCORES)))
    if _debug:
        return res.results
    out = np.concatenate([r["out"].reshape(NSEQ, SEQ, D) for r in res.results], axis=0)
    return out.astype(np.float32)
```

```python
import math
from contextlib import ExitStack

import numpy as np
import concourse.bass as bass
import concourse.mybir as mybir
from concourse.bass_utils import run_bass_kernel_spmd

F32 = mybir.dt.float32
BF16 = mybir.dt.bfloat16
I32 = mybir.dt.int32
AF = mybir.ActivationFunctionType
ALU = mybir.AluOpType
AX = mybir.AxisListType

NCORES = 8
D = 1024
SEQ = 4096
NSEQ = 2
NTOK = NSEQ * SEQ
NT = NTOK // 128
TPS = SEQ // 128
STT = 2
TW = STT * 128
NST = NT // STT
CH = 4608
NCHUNK = 20
NE = 32
NSLOT = NTOK * 2 + NE * 128
NBLK = NSLOT // 128
ALPHA = 2.0 ** 0.25
LN_EPS = 1e-5
RMS_EPS = 1e-6
TWO_PI = 2.0 * math.pi

SAME_ENGINE_SYNC = True


class Buf:
    __slots__ = ("name", "w", "r")

    def __init__(self, name=""):
        self.name = name
        self.w = None
        self.r = {}


class Sched:
    def __init__(self, nc, stack, n_dma_sems=32):
        self.nc = nc
        self.names = ["pe", "act", "dve", "pool", "sp"]
        self.sem = {}
        self.cnt = {}
        self.known = {}
        for k in self.names:
            self.sem[k] = stack.enter_context(nc.semaphore("s_" + k))
            self.cnt[k] = 0
            self.known[k] = {}
        self.dsem = [stack.enter_context(nc.semaphore("d%d" % i)) for i in range(n_dma_sems)]
        self.dcnt = [0] * n_dma_sems
        self.dnext = 0
        self.dnext_pool = 0
        self.n_wait = 0
        self.n_ins = 0
        self.prog = {k: [] for k in self.names}

    def _need(self, e, deps, sem, val, who):
        if who == e and not SAME_ENGINE_SYNC:
            return
        if who == "pe" and e == "pe":
            return
        key = id(sem)
        if deps.get(key, (None, 0))[1] < val:
            deps[key] = (sem, val)

    def _collect(self, e, reads, writes):
        deps = {}
        for b in reads:
            if b.w is not None:
                self._need(e, deps, b.w[0], b.w[1], b.w[2])
        for b in writes:
            if b.w is not None:
                self._need(e, deps, b.w[0], b.w[1], b.w[2])
            for (sem, (val, who)) in b.r.values():
                self._need(e, deps, sem, val, who)
        return deps

    def _emit_waits(self, e, deps):
        kn = self.known[e]
        for key, (sem, val) in deps.items():
            if kn.get(key, 0) >= val:
                continue
            self.prog[e].append((0, sem, val))
            kn[key] = val
            self.n_wait += 1

    def _mark(self, reads, writes, sem, val, who):
        for b in reads:
            b.r[id(sem)] = (sem, (val, who))
        for b in writes:
            b.w = (sem, val, who)
            b.r = {}

    def op(self, e, fn, reads=(), writes=()):
        deps = self._collect(e, reads, writes)
        self._emit_waits(e, deps)
        self.cnt[e] += 1
        self.prog[e].append((1, fn, self.sem[e], 1))
        self._mark(reads, writes, self.sem[e], self.cnt[e], e)
        self.n_ins += 1

    def dma_fn(self, q, fn, reads=(), writes=()):
        n = len(self.dsem)
        npool = 8
        if q == "pool":
            i = n - npool + (self.dnext_pool % npool)
            self.dnext_pool += 1
        else:
            i = self.dnext % (n - npool)
            self.dnext += 1
        sem = self.dsem[i]
        deps = self._collect(q, reads, writes)
        if self.dcnt[i] > 0:
            key = id(sem)
            if deps.get(key, (None, 0))[1] < self.dcnt[i]:
                deps[key] = (sem, self.dcnt[i])
        self._emit_waits(q, deps)
        self.prog[q].append((1, fn, sem, 16))
        self.dcnt[i] += 16
        self._mark(reads, writes, sem, self.dcnt[i], "dma")
        self.n_ins += 1

    def dma(self, q, out, in_, reads=(), writes=()):
        self.dma_fn(q, (lambda eng, out=out, in_=in_: eng.dma_start(out=out, in_=in_)), reads, writes)

    def barrier(self):
        for e in self.names:
            deps = {}
            for o in self.names:
                if o != e and self.cnt[o] > 0:
                    deps[id(self.sem[o])] = (self.sem[o], self.cnt[o])
            for i, s in enumerate(self.dsem):
                if self.dcnt[i] > 0:
                    deps[id(s)] = (s, self.dcnt[i])
            self._emit_waits(e, deps)

    def emit(self):
        def replay(e):
            def body(eng):
                for item in self.prog[e]:
                    if item[0] == 0:
                        eng.wait_ge(item[1], item[2])
                    else:
                        item[1](eng).then_inc(item[2], item[3])
            return body

        with self.nc.Block() as block:
            block.sync(replay("sp"))
            block.tensor(replay("pe"))
            block.scalar(replay("act"))
            block.vector(replay("dve"))
            block.gpsimd(replay("pool"))


def build_program(debug=False, phases=3):
    nc = bass.Bass("TRN2", target_bir_lowering=False)
    dk = "ExternalOutput" if debug else "Internal"
    x_d = nc.dram_tensor("x", [NTOK, D], F32, kind="ExternalInput")
    mem_d = nc.dram_tensor("mem", [NSEQ * 256, D], F32, kind="ExternalInput")
    pos_d = nc.dram_tensor("pos", [128, NT], I32, kind="ExternalInput")
    wf_d = nc.dram_tensor("wf", [NCHUNK, 128, CH], F32, kind="ExternalInput")
    wr_d = nc.dram_tensor("wr", [128, 8, 36], F32, kind="ExternalInput")
    v1_d = nc.dram_tensor("vec1", [128, 3328], F32, kind="ExternalInput")
    lbr_d = nc.dram_tensor("lbr", [128, 1024], F32, kind="ExternalInput")
    v2_d = nc.dram_tensor("vec2", [128, 2048], F32, kind="ExternalInput")
    wg_d = [nc.dram_tensor("wg%d" % i, [NE * 128, 2048], F32, kind="ExternalInput") for i in range(2)]
    wu_d = [nc.dram_tensor("wu%d" % i, [NE * 128, 2048], F32, kind="ExternalInput") for i in range(2)]
    wd_d = [nc.dram_tensor("wd%d" % i, [NE * 128, 2048], F32, kind="ExternalInput") for i in range(2)]
    out_d = nc.dram_tensor("out", [NTOK, D], F32, kind="ExternalOutput")
    wb_d = nc.dram_tensor("wb", [NCHUNK, 128, CH], BF16, kind="Internal")
    x1_d = nc.dram_tensor("x1s", [NTOK, D], F32, kind=dk)
    x1b_d = nc.dram_tensor("x1b", [NTOK, D], BF16, kind="Internal")
    xs_d = nc.dram_tensor("xsl", [NSLOT, D], BF16, kind="Internal")
    ys_d = nc.dram_tensor("ysl", [NSLOT, D], F32, kind="Internal")
    wall_d = nc.dram_tensor("wall", [NE * 128, 12288], BF16, kind="Internal")
    if debug:
        dbg_hg = nc.dram_tensor("dbg_hg", [NTOK, 512], F32, kind="ExternalOutput")
        dbg_df = nc.dram_tensor("dbg_df", [NTOK, 512], F32, kind="ExternalOutput")
        dbg_mem = nc.dram_tensor("dbg_mem", [NTOK, 512], F32, kind="ExternalOutput")
        dbg_rt = nc.dram_tensor("dbg_rt", [128, NT, 8], F32, kind="ExternalOutput")
        dbg_kc = nc.dram_tensor("dbg_kc", [128, 4 * SEQ], BF16, kind="ExternalOutput")
        dbg_vc = nc.dram_tensor("dbg_vc", [128, TPS * 4 * 130], BF16, kind="ExternalOutput")
        dbg_v1 = nc.dram_tensor("dbg_v1", [128, 3328], F32, kind="ExternalOutput")

    with ExitStack() as top:
        S = Sched(nc, top)

        def sbt(st, name, shape, dt):
            return st.enter_context(nc.sbuf_tensor(name, shape, dt))

        def MM(out, lhsT, rhs, st_, sp_, rd, wr):
            S.op("pe", lambda e: e.matmul(out, lhsT, rhs, start=st_, stop=sp_), rd, wr)

        def TR(out, in_, idn, rd, wr):
            S.op("pe", lambda e: e.transpose(out, in_, idn), rd, wr)

        def ACT(out, in_, func, rd, wr, bias=None, scale=None, accum=None):
            kw = {}
            if bias is not None:
                kw["bias"] = bias
            if scale is not None:
                kw["scale"] = scale
            if accum is not None:
                kw["accum_out"] = accum
            S.op("act", lambda e: e.activation(out, in_, func, **kw), rd, wr)

        def TT(out, a, b, op, rd, wr, eng="dve"):
            S.op(eng, lambda e: e.tensor_tensor(out, a, b, op), rd, wr)

        def TS(out, a, s1, s2, op0, op1, rd, wr, eng="dve"):
            if s2 is None:
                S.op(eng, lambda e: e.tensor_scalar(out, a, s1, None, op0), rd, wr)
            else:
                S.op(eng, lambda e: e.tensor_scalar(out, a, s1, s2, op0, op1), rd, wr)

        def SCTT(out, in0, scalar, in1, op0, op1, rd, wr):
            S.op("dve", lambda e: e.scalar_tensor_tensor(out, in0, scalar, in1, op0, op1), rd, wr)

        def CP(eng, out, in_, rd, wr):
            if eng == "act":
                S.op("act", lambda e: e.copy(out, in_), rd, wr)
            else:
                S.op(eng, lambda e: e.tensor_copy(out, in_), rd, wr)

        def RSUM(out, in_, rd, wr):
            S.op("dve", lambda e: e.reduce_sum(out, in_, AX.X), rd, wr)

        def RMAX(out, in_, rd, wr):
            S.op("dve", lambda e: e.reduce_max(out, in_, AX.X), rd, wr)

        def RECIP(out, in_, rd, wr):
            S.op("dve", lambda e: e.reciprocal(out, in_), rd, wr)

        def MEMSET(eng, ap, val, wr):
            S.op(eng, lambda e: e.memset(ap, val), (), wr)

        def ASEL(out, in_, pattern, cmp, fill, base, cm, rd, wr):
            S.op("pool", lambda e: e.affine_select(out, in_, pattern=pattern, compare_op=cmp, fill=fill,
                                                   base=base, channel_multiplier=cm), rd, wr)

        psum = [top.enter_context(nc.psum_tensor("ps%d" % i, [128, 512], F32)) for i in range(8)]
        psb = [Buf("ps%d" % i) for i in range(8)]
        rot = {"n": 0}

        def PS():
            i = rot["n"] % 5
            rot["n"] += 1
            return psum[i], psb[i]

        ACC = [(psum[5], psb[5]), (psum[6], psb[6]), (psum[7], psb[7])]

        cst = top
        ident_bf = sbt(cst, "ident_bf", [128, 128], BF16); b_ident_bf = Buf()
        ident_f = sbt(cst, "ident_f", [128, 128], F32); b_ident_f = Buf()
        maskT = sbt(cst, "maskT", [128, 128], F32); b_maskT = Buf()
        lstrict = sbt(cst, "lstrict", [128, 128], BF16); b_lstrict = Buf()
        ones_bf = sbt(cst, "ones_bf", [128, 128], BF16); b_ones = Buf()
        e_iota = sbt(cst, "e_iota", [128, 32], F32); b_eiota = Buf()
        rt = sbt(cst, "rt", [128, NT, 8], F32)
        b_rt = [Buf() for _ in range(NT)]
        tot = sbt(cst, "tot", [128, 32], F32); b_tot = Buf()
        wr_sb = sbt(cst, "wr_sb", [128, 8, 36], F32); b_wr = Buf()

        S.barrier()
        MEMSET("pool", ident_bf[:], 1.0, [b_ident_bf])
        ASEL(ident_bf[:], ident_bf[:], [[-1, 128]], ALU.is_equal, 0.0, 0, 1, [b_ident_bf], [b_ident_bf])
        MEMSET("pool", ident_f[:], 1.0, [b_ident_f])
        ASEL(ident_f[:], ident_f[:], [[-1, 128]], ALU.is_equal, 0.0, 0, 1, [b_ident_f], [b_ident_f])
        MEMSET("pool", maskT[:], 1.0, [b_maskT])
        ASEL(maskT[:], maskT[:], [[1, 128]], ALU.is_ge, 0.0, 0, -1, [b_maskT], [b_maskT])
        MEMSET("pool", lstrict[:], 1.0, [b_lstrict])
        ASEL(lstrict[:], lstrict[:], [[1, 128]], ALU.is_gt, 0.0, 0, -1, [b_lstrict], [b_lstrict])
        MEMSET("pool", ones_bf[:], 1.0, [b_ones])
        ei_i = sbt(cst, "ei_i", [128, 32], I32); b_eii = Buf()
        S.barrier()
        MEMSET("dve", tot[:], 0.0, [b_tot])
        S.op("pool", lambda e: e.iota(ei_i[:], pattern=[[1, 32]], base=0, channel_multiplier=0), (), [b_eii])
        CP("dve", e_iota[:], ei_i[:], [b_eii], [b_eiota])
        S.dma("sp", wr_sb[:], wr_d[:, :, :], (), [b_wr])

        b_wb = [Buf() for _ in range(NCHUNK)]
        with ExitStack() as pro:
            cb = [sbt(pro, "castbuf%d" % i, [128, CH], BF16) for i in range(3)]
            b_cb = [Buf() for _ in range(3)]
            zt = sbt(pro, "zt", [128, 4096], BF16); b_zt = Buf()
            S.barrier()
            for c in range(NCHUNK):
                k = c % 3
                for part in range(3):
                    S.dma("pool", cb[k][:, part * 1536:(part + 1) * 1536], wf_d[c, :, part * 1536:(part + 1) * 1536],
                          (), [b_cb[k]])
                S.dma("sp", wb_d[c, :, :], cb[k][:], [b_cb[k]], [b_wb[c]])
            MEMSET("dve", zt[:], 0.0, [b_zt])
            b_xs_zero = []
            xs_v = xs_d[:, :].rearrange("(n p r) d -> n p (r d)", p=128, r=4)
            for n in range(NSLOT // 512):
                bz = Buf()
                S.dma("sp", xs_v[n], zt[:], [b_zt], [bz])
                b_xs_zero.append(bz)
        S.barrier()

        b_x1 = [Buf() for _ in range(NT)]
        b_x1b = [Buf() for _ in range(NT)]
        b_wall = []
        with ExitStack() as p1:
            vec1 = sbt(p1, "vec1_sb", [128, 3328], F32); b_vec1 = Buf()
            S.dma("sp", vec1[:], v1_d[:, :], (), [b_vec1])
            hg_gain = vec1[:, 0:512]
            lamv = vec1[:, 512:768]
            df_gain = vec1[:, 768:1280]
            ln1g = vec1[:, 1280:2304]
            ln1b = vec1[:, 2304:3328]
            lbv = sbt(p1, "lbv", [128, 512], F32); b_lbv = Buf()
            oml = sbt(p1, "oml", [128, 512], F32); b_oml = Buf()
            lam_t = sbt(p1, "lam_t", [128, 4], F32); b_lam = Buf()
            lscr = sbt(p1, "lscr", [128, 128], F32); b_lscr = Buf()
            TT(lscr[:, 0:64], lamv[:, 0:64], lamv[:, 64:128], ALU.mult, [b_vec1], [b_lscr])
            TT(lscr[:, 64:128], lamv[:, 128:192], lamv[:, 192:256], ALU.mult, [b_vec1], [b_lscr])
            RSUM(lam_t[:, 0:2], lscr[:].rearrange("p (a b) -> p a b", a=2), [b_lscr], [b_lam])
            ACT(lam_t[:, 0:2], lam_t[:, 0:2], AF.Exp, [b_lam], [b_lam])
            TT(lam_t[:, 2:3], lam_t[:, 0:1], lam_t[:, 1:2], ALU.subtract, [b_lam], [b_lam])
            TS(lam_t[:, 3:4], lam_t[:, 2:3], 0.2, None, ALU.add, None, [b_lam], [b_lam])
            lam_ap = lam_t[:, 3:4]
            dmat = sbt(p1, "dmat", [128, 128], F32); b_dmat = Buf()
            dtmp = sbt(p1, "dtmp", [128, 128], F32); b_dtmp = Buf()
            cols2 = sbt(p1, "cols2", [128, 2], F32); b_cols2 = Buf()
            MEMSET("pool", dtmp[:], 1.0, [b_dtmp])
            ASEL(dtmp[:], dtmp[:], [[0, 128]], ALU.is_ge, 0.0, 63, -1, [b_dtmp], [b_dtmp])
            TT(dmat[:], dtmp[:], maskT[:], ALU.subtract, [b_dtmp, b_maskT], [b_dmat], eng="pool")
            MEMSET("pool", cols2[:], 1.0, [b_cols2])
            CP("pool", cols2[:, 1:2], dtmp[:, 0:1], [b_dtmp, b_cols2], [b_cols2])
            trig = sbt(p1, "trig", [128, 2, NT, 8], F32); b_trig = Buf()
            with ExitStack() as tr:
                lbr = sbt(tr, "lbr_sb", [128, 1024], F32); b_lbr = Buf()
                S.dma("sp", lbr[:], lbr_d[:, :], (), [b_lbr])
                TT(lbv[:], lbr[:, 0:512], lbr[:, 512:1024], ALU.subtract, [b_lbr], [b_lbv])
                ACT(lbv[:], lbv[:], AF.Sigmoid, [b_lbv], [b_lbv])
                TS(oml[:], lbv[:], -1.0, 1.0, ALU.mult, ALU.add, [b_lbv], [b_oml])
                posi = sbt(tr, "posi", [128, NT], I32); b_posi = Buf()
                posf = sbt(tr, "posf", [128, NT], F32); b_posf = Buf()
                u = sbt(tr, "u", [128, 2, NT, 8], F32); b_u = Buf()
                kf = sbt(tr, "kf", [128, 2 * NT * 8], F32); b_kf = Buf()
                ki = sbt(tr, "ki", [128, 2 * NT * 8], I32); b_ki = Buf()
                S.dma("sp", posi[:], pos_d[:, :], (), [b_posi])
                CP("dve", posf[:], posi[:], [b_posi], [b_posf])
                for j in range(8):
                    invf = 500000.0 ** (-(2.0 * j) / 16.0)
                    TS(u[:, 0, :, j], posf[:], float(np.float32(invf)), None, ALU.mult, None, [b_posf], [b_u])
                TS(u[:, 1, :, :], u[:, 0, :, :], math.pi / 2.0, None, ALU.add, None, [b_u], [b_u])
                uf = u[:].rearrange("p a n j -> p (a n j)")
                TS(kf[:], uf, 1.0 / TWO_PI, None, ALU.mult, None, [b_u], [b_kf])
                CP("dve", ki[:], kf[:], [b_kf], [b_ki])
                CP("dve", kf[:], ki[:], [b_ki], [b_kf])
                C1 = 6.28125
                C2 = TWO_PI - C1
                SCTT(uf, kf[:], -C1, uf, ALU.mult, ALU.add, [b_kf, b_u], [b_u])
                SCTT(uf, kf[:], -C2, uf, ALU.mult, ALU.add, [b_kf, b_u], [b_u])
                TS(kf[:], uf, math.pi, -TWO_PI, ALU.is_gt, ALU.mult, [b_u], [b_kf])
                TT(uf, uf, kf[:], ALU.add, [b_u, b_kf], [b_u])
                TS(kf[:], uf, -math.pi, TWO_PI, ALU.is_lt, ALU.mult, [b_u], [b_kf])
                TT(uf, uf, kf[:], ALU.add, [b_u, b_kf], [b_u])
                TS(uf, uf, -3.1415925, 3.1415925, ALU.max, ALU.min, [b_u], [b_u])
                ACT(trig[:].rearrange("p a n j -> p (a n j)"), uf, AF.Sin, [b_u], [b_trig])
                S.barrier()

            kc = sbt(p1, "kc", [128, 4, SEQ], BF16)
            b_kc = [Buf() for _ in range(TPS)]
            vc = sbt(p1, "vc", [128, TPS, 4, 130], BF16)
            b_vc = [Buf() for _ in range(TPS)]
            b_vc1 = Buf()
            MEMSET("pool", vc[:, :, :, 128:130], 1.0, [b_vc1])
            mkT = sbt(p1, "mkT", [128, 4, 256], BF16); b_mkT = Buf()
            mvc = sbt(p1, "mvc", [128, 2, 4, 130], BF16); b_mvc = Buf()
            b_mvc1 = Buf()
            MEMSET("pool", mvc[:, :, :, 128:130], 1.0, [b_mvc1])
            Sst = sbt(p1, "Sst", [128, 4, 128], F32); b_S = Buf()
            Smm = sbt(p1, "Smm", [128, 4, 128], BF16); b_Smm = Buf()
            wbuf = [sbt(p1, "wbuf%d" % i, [128, CH], BF16) for i in range(2)]
            b_wbuf = [Buf() for _ in range(2)]
            worder = []
            for _sq in range(NSEQ):
                worder += [18, 19]
                for _st in range(TPS // STT):
                    worder += list(range(18))
            wst = {"i": 0, "issued": {}}

            def _issue_chunk(i):
                c = worder[i]
                k = i % 2
                S.dma("sp", wbuf[k][:], wb_d[c, :, :], [b_wb[c]], [b_wbuf[k]])
                return wbuf[k], b_wbuf[k]

            def load_chunk(c):
                i = wst["i"]
                assert worder[i] == c, (i, worder[i], c)
                if i not in wst["issued"]:
                    wst["issued"][i] = _issue_chunk(i)
                cur = wst["issued"].pop(i)
                if i + 1 < len(worder) and (i + 1) not in wst["issued"]:
                    wst["issued"][i + 1] = _issue_chunk(i + 1)
                wst["i"] += 1
                return cur

            xs = sbt(p1, "xs", [128, STT, D], F32); b_xs = [Buf() for _ in range(STT)]
            xbf = sbt(p1, "xbf", [128, D], BF16); b_xbf = Buf()
            xT = sbt(p1, "xT", [128, 8, TW], BF16); b_xT = Buf()
            sq = sbt(p1, "sq", [128, STT, 512], F32); b_sq = [Buf() for _ in range(STT)]
            sg = sbt(p1, "sg", [128, STT, 512], F32); b_sg = [Buf() for _ in range(STT)]
            vhg = sbt(p1, "vhg", [128, STT, 512], BF16); b_vhg = [Buf() for _ in range(STT)]
            gsl = sbt(p1, "gsl", [128, STT, 512], BF16); b_gsl = [Buf() for _ in range(STT)]
            qT = sbt(p1, "qT", [128, 4, TW], BF16); b_qT = [Buf() for _ in range(STT)]
            mqT = sbt(p1, "mqT", [128, 4, TW], BF16); b_mqT = Buf()
            yT = [sbt(p1, "yT%d" % i, [128, 4, TW], BF16) for i in range(3)]
            b_yT = [[Buf() for _ in range(STT)] for _ in range(3)]
            mergedT = sbt(p1, "mergedT", [128, 8, TW], BF16); b_mg = [Buf() for _ in range(8)]
            wk = [sbt(p1, "wk%d" % i, [128, 512], F32) for i in range(8)]
            b_wk = [Buf() for _ in range(8)]
            wkb = [sbt(p1, "wkb%d" % i, [128, 512], BF16) for i in range(6)]
            b_wkb = [Buf() for _ in range(6)]
            NPT = 4
            LOOKAHEAD = 2
            pT = [sbt(p1, "pT%d" % i, [128, 512], BF16) for i in range(NPT)]
            b_pT = [Buf() for _ in range(NPT)]
            psel = {"n": 0}
            sm = sbt(p1, "sm", [128, 64], F32); b_sm = Buf()
            z = sbt(p1, "z", [128, D], F32); b_z = Buf()
            z_b = sbt(p1, "z_b", [128, D], F32); b_zB = Buf()
            zb = sbt(p1, "zb", [128, D], BF16); b_zb = Buf()
            x1T = sbt(p1, "x1T", [128, 8, 128], F32); b_x1T = Buf()
            stats = sbt(p1, "stats", [128, 16], F32); b_stats = Buf()
            rl = sbt(p1, "rl", [128, 128], F32); b_rl = Buf()
            oh = sbt(p1, "oh", [128, 96], F32); b_oh = Buf()
            ohb = sbt(p1, "ohb", [128, 32], BF16); b_ohb = Buf()

            def layer_norm(zt, bz, gain, bias, bgb, out_ap, bout):
                for hlf in range(2):
                    S.op("dve", lambda e, hlf=hlf: e.bn_stats(stats[:, hlf * 6:(hlf + 1) * 6], zt[:, hlf * 512:(hlf + 1) * 512]),
                         [bz], [b_stats])
                S.op("dve", lambda e: e.bn_aggr(stats[:, 12:14], stats[:, 0:12]), [b_stats], [b_stats])
                TS(stats[:, 14:15], stats[:, 13:14], LN_EPS, None, ALU.add, None, [b_stats], [b_stats])
                ACT(stats[:, 14:15], stats[:, 14:15], AF.Ln, [b_stats], [b_stats])
                ACT(stats[:, 15:16], stats[:, 14:15], AF.Exp, [b_stats], [b_stats], scale=-0.5)
                TS(zt, zt, stats[:, 12:13], stats[:, 15:16], ALU.subtract, ALU.mult, [bz, b_stats], [bz])
                TT(zt, zt, gain, ALU.mult, [bz] + bgb, [bz])
                TT(out_ap, zt, bias, ALU.add, [bz] + bgb, [bout])

            def rms_gate(o_sb, bo, extra_scale, gain_ap, bgain, gate_ap, bgate, out_bf, bout):
                o3 = o_sb.rearrange("p (h v) -> p h v", h=4)
                w0, bw0 = wk[7], b_wk[7]
                TT(w0[:], o_sb, o_sb, ALU.mult, [bo], [bw0])
                RSUM(sm[:, 0:4], w0[:].rearrange("p (h v) -> p h v", h=4), [bw0], [b_sm])
                TS(sm[:, 0:4], sm[:, 0:4], 1.0 / 128.0, RMS_EPS, ALU.mult, ALU.add, [b_sm], [b_sm])
                ACT(sm[:, 0:4], sm[:, 0:4], AF.Ln, [b_sm], [b_sm])
                ACT(sm[:, 4:8], sm[:, 0:4], AF.Exp, [b_sm], [b_sm], scale=-0.5)
                if extra_scale != 1.0:
                    TS(sm[:, 4:8], sm[:, 4:8], float(extra_scale), None, ALU.mult, None, [b_sm], [b_sm])
                TT(w0[:].rearrange("p (h v) -> p h v", h=4), o3, sm[:, 4:8].unsqueeze(2).to_broadcast([128, 4, 128]),
                   ALU.mult, [bo, b_sm], [bw0])
                if gate_ap is not None:
                    TT(w0[:], w0[:], gain_ap, ALU.mult, [bw0] + bgain, [bw0])
                    TT(out_bf, w0[:], gate_ap, ALU.mult, [bw0] + bgate, [bout])
                else:
                    TT(out_bf, w0[:], gain_ap, ALU.mult, [bw0] + bgain, [bout])

            def transpose_to(y_bf, by, dstT, bdst, col0):
                pt, bpt = PS()
                ptb = pt[:, :].bitcast(BF16)
                for c4 in range(4):
                    TR(ptb[:, c4 * 128:(c4 + 1) * 128], y_bf[:, c4 * 128:(c4 + 1) * 128], ident_bf[:],
                       [by, b_ident_bf], [bpt])
                CP("act", dstT[:, :, col0:col0 + 128], ptb[:, 0:512].rearrange("p (c t) -> p c t", c=4), [bpt], [bdst])

            for seq in range(NSEQ):
                MEMSET("dve", Sst[:], 0.0, [b_S])
                with ExitStack() as ms:
                    mems, b_mems = z, b_z
                    memb, b_memb = zb, b_zb
                    memTt = sbt(ms, "memTt%d" % seq, [128, 8, 256], BF16); b_memT = Buf()
                    for nb in range(2):
                        S.dma("sp", mems[:], mem_d[seq * 256 + nb * 128: seq * 256 + (nb + 1) * 128, :], (), [b_mems])
                        CP("dve", memb[:], mems[:], [b_mems], [b_memb])
                        pt, bpt = PS()
                        ptb = pt[:, :].bitcast(BF16)
                        for k in range(8):
                            TR(ptb[:, k * 128:(k + 1) * 128], memb[:, k * 128:(k + 1) * 128], ident_bf[:],
                               [b_memb, b_ident_bf], [bpt])
                        CP("act", memTt[:, :, nb * 128:(nb + 1) * 128], ptb[:, :].rearrange("p (k t) -> p k t", k=8),
                           [bpt], [b_memT])
                    wt, bw = load_chunk(18)
                    w3 = wt[:, 0:4096].rearrange("p (k c) -> p k c", k=8)
                    pt, bpt = PS()
                    pt2, bpt2 = PS()
                    for h in range(4):
                        po = (pt if h < 2 else pt2)
                        bpo = (bpt if h < 2 else bpt2)
                        for k in range(8):
                            MM(po[:, (h % 2) * 256:(h % 2) * 256 + 256], w3[:, k, h * 128:(h + 1) * 128], memTt[:, k, :],
                               k == 0, k == 7, [bw, b_memT], [bpo])
                    CP("act", mkT[:, 0:2, :], pt[:, :].rearrange("p (h n) -> p h n", h=2), [bpt], [b_mkT])
                    CP("act", mkT[:, 2:4, :], pt2[:, :].rearrange("p (h n) -> p h n", h=2), [bpt2], [b_mkT])
                    wt, bw = load_chunk(19)
                    w3 = wt[:, 0:4096].rearrange("p (k c) -> p k c", k=8)
                    for nb in range(2):
                        pt, bpt = PS()
                        for k in range(8):
                            MM(pt[:, :], memTt[:, k, nb * 128:(nb + 1) * 128], w3[:, k, :], k == 0, k == 7,
                               [bw, b_memT], [bpt])
                        CP("act", mvc[:, nb, :, 0:128], pt[:, :].rearrange("p (h v) -> p h v", h=4), [bpt], [b_mvc])
                    S.barrier()
                if seq == 0:
                    for ci, src in enumerate((wg_d[0], wg_d[1], wu_d[0], wu_d[1], wd_d[0], wd_d[1])):
                        for r0 in range(0, NE * 128, 512):
                            bw_ = Buf()
                            S.dma("pool", wall_d[r0:r0 + 512, ci * 2048:(ci + 1) * 2048], src[r0:r0 + 512, :], (), [bw_])
                            b_wall.append(bw_)

                for st_i in range(TPS // STT):
                    tiles = [st_i * STT + tt for tt in range(STT)]
                    gt = [seq * TPS + t for t in tiles]
                    for tt in range(STT):
                        r0 = gt[tt] * 128
                        S.dma("sp", xs[:, tt, :], x_d[r0:r0 + 128, :], (), [b_xs[tt]])
                        CP("dve", xbf[:], xs[:, tt, :], [b_xs[tt]], [b_xbf])
                        pt, bpt = PS()
                        ptb = pt[:, :].bitcast(BF16)
                        for k in range(8):
                            TR(ptb[:, k * 128:(k + 1) * 128], xbf[:, k * 128:(k + 1) * 128], ident_bf[:],
                               [b_xbf, b_ident_bf], [bpt])
                        CP("act", xT[:, :, tt * 128:(tt + 1) * 128], ptb[:, :].rearrange("p (k t) -> p k t", k=8),
                           [bpt], [b_xT])
                    for c in range(8):
                        wt, bw = load_chunk(c)
                        w3 = wt[:, 0:4096].rearrange("p (k c) -> p k c", k=8)
                        if c == 7:
                            for h in range(4):
                                pt, bpt = PS()
                                for k in range(8):
                                    MM(pt[:, 0:TW], w3[:, k, h * 128:(h + 1) * 128], xT[:, k, :], k == 0, k == 7,
                                       [bw, b_xT], [bpt])
                                CP("act", mqT[:, h, :], pt[:, 0:TW], [bpt], [b_mqT])
                            continue
                        for tt in range(STT):
                            pt, bpt = PS()
                            for k in range(8):
                                MM(pt[:, :], xT[:, k, tt * 128:(tt + 1) * 128], w3[:, k, :], k == 0, k == 7,
                                   [bw, b_xT], [bpt])
                            if c == 0:
                                ACT(sq[:, tt, :], pt[:, :], AF.Silu, [bpt], [b_sq[tt]])
                            elif c == 1:
                                ACT(sg[:, tt, :], pt[:, :], AF.Sigmoid, [bpt], [b_sg[tt]])
                            elif c == 2:
                                CP("act", vhg[:, tt, :], pt[:, :], [bpt], [b_vhg[tt]])
                            elif c == 3:
                                ACT(gsl[:, tt, :], pt[:, :], AF.Silu, [bpt], [b_gsl[tt]])
                            elif c in (4, 5):
                                ti = gt[tt]
                                sc = 0.125 if c == 4 else 1.0
                                r_f, br_f = wk[0], b_wk[0]
                                r_b, br_b = wkb[0], b_wkb[0]
                                ACT(r_f[:], pt[:, :], AF.Copy, [bpt], [br_f], scale=sc)
                                CP("dve", r_b[:], r_f[:], [br_f], [br_b])
                                f3 = r_f[:].rearrange("p (g d) -> p g d", g=8)
                                o3 = r_b[:].rearrange("p (g d) -> p g d", g=8)
                                sn = trig[:, 0, ti, :].unsqueeze(1).to_broadcast([128, 8, 8])
                                cs = trig[:, 1, ti, :].unsqueeze(1).to_broadcast([128, 8, 8])
                                t_a, bt_a = wk[1], b_wk[1]
                                a3 = t_a[:, 0:64].rearrange("p (g d) -> p g d", g=8)
                                b3 = t_a[:, 64:128].rearrange("p (g d) -> p g d", g=8)
                                TT(a3, f3[:, :, 0:8], cs, ALU.mult, [br_f, b_trig], [bt_a])
                                TT(b3, f3[:, :, 8:16], sn, ALU.mult, [br_f, b_trig], [bt_a])
                                TT(o3[:, :, 0:8], a3, b3, ALU.subtract, [bt_a, br_b], [br_b])
                                TT(a3, f3[:, :, 8:16], cs, ALU.mult, [br_f, b_trig, bt_a], [bt_a])
                                TT(b3, f3[:, :, 0:8], sn, ALU.mult, [br_f, b_trig, bt_a], [bt_a])
                                TT(o3[:, :, 8:16], a3, b3, ALU.add, [bt_a, br_b], [br_b])
                                if c == 4:
                                    transpose_to(r_b, br_b, qT, b_qT[tt], tt * 128)
                                else:
                                    transpose_to(r_b, br_b, kc, b_kc[tiles[tt]], tiles[tt] * 128)
                            elif c == 6:
                                CP("act", vc[:, tiles[tt], :, 0:128], pt[:, :].rearrange("p (h v) -> p h v", h=4),
                                   [bpt, b_vc1], [b_vc[tiles[tt]]])

                    for tt in range(STT):
                        ti = tiles[tt]
                        c0 = tt * 128
                        fg, bfg = wk[0], b_wk[0]
                        lf, blf = wk[1], b_wk[1]
                        TT(fg[:], sg[:, tt, :], oml[:], ALU.mult, [b_sg[tt], b_oml], [bfg])
                        TT(fg[:], fg[:], lbv[:], ALU.add, [bfg, b_lbv], [bfg])
                        ACT(lf[:], fg[:], AF.Ln, [bfg], [blf])
                        pD, bpD = PS()
                        MM(pD[:, :], dmat[:], lf[:], True, True, [b_dmat, blf], [bpD])
                        eD, beD = wk[2], b_wk[2]
                        eDn, beDn = wk[3], b_wk[3]
                        ACT(eD[:], pD[:, :], AF.Exp, [bpD], [beD])
                        ACT(eDn[:], pD[:, :], AF.Exp, [bpD], [beDn], scale=-1.0)
                        TS(fg[:], fg[:], -1.0, 1.0, ALU.mult, ALU.add, [bfg], [bfg])
                        kt, bkt = wkb[0], b_wkb[0]
                        qt, bqt = wkb[1], b_wkb[1]
                        TT(kt[:], fg[:], eD[:], ALU.mult, [bfg, beD], [bkt])
                        TT(qt[:], sq[:, tt, :], eDn[:], ALU.mult, [b_sq[tt], beDn], [bqt])
                        kqT, bkqT = wkb[2], b_wkb[2]
                        qqT, bqqT = wkb[3], b_wkb[3]
                        for (src, bsrc, dst, bdst) in ((kt, bkt, kqT, bkqT), (qt, bqt, qqT, bqqT)):
                            pt, bpt = PS()
                            ptb = pt[:, :].bitcast(BF16)
                            for h in range(4):
                                TR(ptb[:, h * 128:(h + 1) * 128], src[:, h * 128:(h + 1) * 128], ident_bf[:],
                                   [bsrc, b_ident_bf], [bpt])
                            CP("act", dst[:], ptb[:, 0:512], [bpt], [bdst])
                        pc, bpc = PS()
                        for h in range(4):
                            MM(pc[:, h * 2:h * 2 + 2], lf[:, h * 128:(h + 1) * 128], cols2[:], True, True,
                               [blf, b_cols2], [bpc])
                        ex = sm[:, 8:20].rearrange("p (h c) -> p h c", h=4)
                        pc3 = pc[:, 0:8].rearrange("p (h c) -> p h c", h=4)
                        CP("dve", ex[:, :, 0:2], pc3, [bpc], [b_sm])
                        TT(ex[:, :, 2:3], ex[:, :, 0:1], ex[:, :, 1:2], ALU.subtract, [b_sm], [b_sm])
                        ACT(sm[:, 8:20], sm[:, 8:20], AF.Exp, [b_sm], [b_sm])
                        po, bpo = ACC[0]
                        pU, bpU = ACC[1]
                        hsc = []
                        for h in range(4):
                            hs = slice(h * 128, (h + 1) * 128)
                            psc, bpsc = PS()
                            MM(psc[:, 0:128], kqT[:, hs], qqT[:, hs], True, True, [bkqT, bqqT], [bpsc])
                            hsc.append((psc, bpsc))
                        hat = []
                        for h in range(4):
                            psc, bpsc = hsc[h]
                            AT, bAT = pT[psel["n"] % NPT], b_pT[psel["n"] % NPT]
                            psel["n"] += 1
                            TT(AT[:, 0:128], psc[:, 0:128], maskT[:], ALU.mult, [bpsc, b_maskT], [bAT])
                            hat.append((AT, bAT))
                        for h in range(4):
                            TS(Smm[:, h, :], Sst[:, h, :], ex[:, h, 1:2], None, ALU.mult, None, [b_S, b_sm], [b_Smm])
                        for h in range(4):
                            hs = slice(h * 128, (h + 1) * 128)
                            AT, bAT = hat[h]
                            MM(pU[:, hs], kt[:, hs], vhg[:, tt, hs], True, True, [bkt, b_vhg[tt]], [bpU])
                            MM(po[:, hs], AT[:, 0:128], vhg[:, tt, hs], True, False, [bAT, b_vhg[tt]], [bpo])
                            MM(po[:, hs], qqT[:, hs], Smm[:, h, :], False, True, [bqqT, b_Smm], [bpo])
                        for h in range(4):
                            hs = slice(h * 128, (h + 1) * 128)
                            TS(Sst[:, h, :], Sst[:, h, :], ex[:, h, 0:1], None, ALU.mult, None, [b_S, b_sm], [b_S])
                            SCTT(Sst[:, h, :], pU[:, hs], ex[:, h, 2:3], Sst[:, h, :], ALU.mult, ALU.add,
                                [bpU, b_sm, b_S], [b_S])
                        o_sb, bo_sb = wk[4], b_wk[4]
                        CP("act", o_sb[:], po[:, :], [bpo], [bo_sb])
                        y_bf, by_bf = wkb[4], b_wkb[4]
                        rms_gate(o_sb[:], bo_sb, 1.0, hg_gain, [b_vec1], gsl[:, tt, :], [b_gsl[tt]], y_bf[:], by_bf)
                        if debug:
                            CP("dve", wk[5][:], y_bf[:], [by_bf], [b_wk[5]])
                            S.dma("sp", dbg_hg[gt[tt] * 128:(gt[tt] + 1) * 128, :], wk[5][:], [b_wk[5]], [Buf()])
                        transpose_to(y_bf, by_bf, yT[0], b_yT[0][tt], c0)

                        od, bod = wk[4], b_wk[4]
                        nkb = ti + 1
                        tasks = []
                        for h in range(4):
                            for m in range(2):
                                for g0 in range(0, nkb, 4):
                                    tasks.append((h, m, g0, min(4, nkb - g0)))

                        def att_a(task):
                            h, m, g0, ng = task
                            ps_ = slice(m * 64, (m + 1) * 64)
                            psc, bpsc = PS()
                            for jj in range(ng):
                                j = g0 + jj
                                MM(psc[:, jj * 128:(jj + 1) * 128], kc[ps_, h, j * 128:(j + 1) * 128],
                                   qT[ps_, h, c0:c0 + 128], True, True, [b_kc[j], b_qT[tt]], [bpsc])
                            P, bP = pT[psel["n"] % NPT], b_pT[psel["n"] % NPT]
                            psel["n"] += 1
                            ACT(P[:, 0:ng * 128], psc[:, 0:ng * 128], AF.Exp, [bpsc], [bP])
                            if g0 + ng == nkb:
                                dsl = slice((ng - 1) * 128, ng * 128)
                                TT(P[:, dsl], P[:, dsl], maskT[:], ALU.mult, [bP, b_maskT], [bP])
                            return P, bP

                        def att_b(task, P, bP):
                            h, m, g0, ng = task
                            pacc, bpacc = ACC[2] if h % 2 == 0 else ACC[0]
                            for jj in range(ng):
                                j = g0 + jj
                                MM(pacc[:, m * 130:(m + 1) * 130], P[:, jj * 128:(jj + 1) * 128], vc[:, j, h, :],
                                   j == 0, j == nkb - 1, [bP, b_vc[j], b_vc1], [bpacc])
                            if m == 1 and g0 + ng == nkb:
                                rr = sm[:, 24:28]
                                RECIP(rr[:, 0:1], pacc[:, 128:129], [bpacc], [b_sm])
                                RECIP(rr[:, 1:2], pacc[:, 258:259], [bpacc], [b_sm])
                                TT(rr[:, 2:3], rr[:, 1:2], lam_ap, ALU.mult, [b_sm, b_lam], [b_sm])
                                t2, bt2 = wk[5], b_wk[5]
                                TS(t2[:, 0:128], pacc[:, 130:258], rr[:, 2:3], None, ALU.mult, None, [bpacc, b_sm], [bt2])
                                SCTT(od[:, h * 128:(h + 1) * 128], pacc[:, 0:128], rr[:, 0:1], t2[:, 0:128], ALU.mult,
                                     ALU.subtract, [bpacc, b_sm, bt2], [bod])

                        inflight = []
                        for task in tasks:
                            inflight.append((task,) + att_a(task))
                            if len(inflight) > LOOKAHEAD:
                                t0_, P0_, bP0_ = inflight.pop(0)
                                att_b(t0_, P0_, bP0_)
                        for (t0_, P0_, bP0_) in inflight:
                            att_b(t0_, P0_, bP0_)
                        y_bf, by_bf = wkb[4], b_wkb[4]
                        rms_gate(od[:], bod, 0.8, df_gain, [b_vec1], None, None, y_bf[:], by_bf)
                        if debug:
                            CP("dve", wk[5][:], y_bf[:], [by_bf], [b_wk[5]])
                            S.dma("sp", dbg_df[gt[tt] * 128:(gt[tt] + 1) * 128, :], wk[5][:], [b_wk[5]], [Buf()])
                        transpose_to(y_bf, by_bf, yT[1], b_yT[1][tt], c0)

                        ym, bym = wk[4], b_wk[4]
                        for hp in range(2):
                            psc, bpsc = PS()
                            for hh in range(2):
                                h = hp * 2 + hh
                                for nb in range(2):
                                    MM(psc[:, (hh * 2 + nb) * 128:(hh * 2 + nb + 1) * 128], mkT[:, h, nb * 128:(nb + 1) * 128],
                                       mqT[:, h, c0:c0 + 128], True, True, [b_mkT, b_mqT], [bpsc])
                            P, bP = pT[psel["n"] % NPT], b_pT[psel["n"] % NPT]
                            psel["n"] += 1
                            ACT(P[:, :], psc[:, :], AF.Exp, [bpsc], [bP], scale=128.0 ** -0.5)
                            pacc, bpacc = ACC[1]
                            for hh in range(2):
                                h = hp * 2 + hh
                                for nb in range(2):
                                    MM(pacc[:, hh * 130:(hh + 1) * 130], P[:, (hh * 2 + nb) * 128:(hh * 2 + nb + 1) * 128],
                                       mvc[:, nb, h, :], nb == 0, nb == 1, [bP, b_mvc, b_mvc1], [bpacc])
                            rr = sm[:, 28:30]
                            RECIP(rr[:, 0:1], pacc[:, 128:129], [bpacc], [b_sm])
                            RECIP(rr[:, 1:2], pacc[:, 258:259], [bpacc], [b_sm])
                            for hh in range(2):
                                h = hp * 2 + hh
                                TS(ym[:, h * 128:(h + 1) * 128], pacc[:, hh * 130:hh * 130 + 128], rr[:, hh:hh + 1], None,
                                   ALU.mult, None, [bpacc, b_sm], [bym])
                        y_bf, by_bf = wkb[4], b_wkb[4]
                        CP("dve", y_bf[:], ym[:], [bym], [by_bf])
                        if debug:
                            S.dma("sp", dbg_mem[gt[tt] * 128:(gt[tt] + 1) * 128, :], ym[:], [bym], [Buf()])
                        transpose_to(y_bf, by_bf, yT[2], b_yT[2][tt], c0)

                    rd_y = [b for br in range(3) for b in b_yT[br]]
                    for cg in range(8):
                        wt, bw = load_chunk(8 + cg)
                        wg3 = wt[:, 0:3072].rearrange("p (k b c) -> p k b c", k=8, b=3)
                        wb3 = wt[:, 3072:4608].rearrange("p (k b c) -> p k b c", k=4, b=3)
                        acc, bacc = wk[6], b_wk[6]
                        for br in range(3):
                            pg, bpg = PS()
                            for k in range(8):
                                MM(pg[:, 0:TW], wg3[:, k, br, :], xT[:, k, :], k == 0, k == 7, [bw, b_xT], [bpg])
                            gt_, bgt_ = wk[br], b_wk[br]
                            ACT(gt_[:, 0:TW], pg[:, 0:TW], AF.Sigmoid, [bpg], [bgt_])
                            pb, bpb = PS()
                            for k in range(4):
                                MM(pb[:, 0:TW], wb3[:, k, br, :], yT[br][:, k, :], k == 0, k == 3, [bw] + rd_y, [bpb])
                            if br == 0:
                                TT(acc[:, 0:TW], pb[:, 0:TW], gt_[:, 0:TW], ALU.mult, [bpb, bgt_], [bacc])
                            else:
                                TT(gt_[:, 0:TW], pb[:, 0:TW], gt_[:, 0:TW], ALU.mult, [bpb, bgt_], [bgt_])
                                if br == 1:
                                    TT(acc[:, 0:TW], acc[:, 0:TW], gt_[:, 0:TW], ALU.add, [bacc, bgt_], [bacc])
                                else:
                                    TT(mergedT[:, cg, :], acc[:, 0:TW], gt_[:, 0:TW], ALU.add, [bacc, bgt_], [b_mg[cg]])

                    zts = [(z, b_z), (z_b, b_zB)]
                    for n in range(2):
                        wt, bw = load_chunk(16 + n)
                        w3 = wt[:, 0:4096].rearrange("p (k c) -> p k c", k=8)
                        for tt in range(STT):
                            zt_, bzt_ = zts[tt]
                            pt, bpt = PS()
                            for k in range(8):
                                MM(pt[:, :], mergedT[:, k, tt * 128:(tt + 1) * 128], w3[:, k, :], k == 0, k == 7,
                                   [bw] + b_mg, [bpt])
                            SCTT(zt_[:, n * 512:(n + 1) * 512], xs[:, tt, n * 512:(n + 1) * 512], ALPHA, pt[:, :],
                                 ALU.mult, ALU.add, [b_xs[tt], bpt], [bzt_])
                    for tt in range(STT):
                        g = gt[tt]
                        zc, b_zc = zts[tt]
                        layer_norm(zc[:], b_zc, ln1g, ln1b, [b_vec1], zc[:], b_zc)
                        S.dma("sp", x1_d[g * 128:(g + 1) * 128, :], zc[:], [b_zc], [b_x1[g]])
                        CP("act", zb[:], zc[:], [b_zc], [b_zb])
                        S.dma("sp", x1b_d[g * 128:(g + 1) * 128, :], zb[:], [b_zb], [b_x1b[g]])
                        for half in range(2):
                            pt, bpt = PS()
                            for k4 in range(4):
                                k = half * 4 + k4
                                TR(pt[:, k4 * 128:(k4 + 1) * 128], zc[:, k * 128:(k + 1) * 128], ident_f[:],
                                   [b_zc, b_ident_f], [bpt])
                            CP("act", x1T[:, half * 4:(half + 1) * 4, :], pt[:, :].rearrange("p (k t) -> p k t", k=4),
                               [bpt], [b_x1T])
                        pr, bpr = PS()
                        for k in range(8):
                            MM(pr[:, 0:36], x1T[:, k, :], wr_sb[:, k, :], k == 0, k == 7, [b_x1T, b_wr], [bpr])
                        CP("dve", rl[:, 0:36], pr[:, 0:36], [bpr], [b_rl])
                        gl = rl[:, 0:4]
                        el = rl[:, 4:36]
                        RMAX(rl[:, 40:41], gl, [b_rl], [b_rl])
                        TS(rl[:, 44:48], gl, rl[:, 40:41], None, ALU.subtract, None, [b_rl], [b_rl])
                        ACT(rl[:, 44:48], rl[:, 44:48], AF.Exp, [b_rl], [b_rl])
                        RSUM(rl[:, 41:42], rl[:, 44:48], [b_rl], [b_rl])
                        RECIP(rl[:, 42:43], rl[:, 41:42], [b_rl], [b_rl])
                        TS(rl[:, 48:52], gl, rl[:, 40:41], None, ALU.is_ge, None, [b_rl], [b_rl])
                        TS(rl[:, 48:52], rl[:, 48:52], -1.0, 1e30, ALU.add, ALU.mult, [b_rl], [b_rl])
                        TT(rl[:, 64:96].rearrange("p (g j) -> p g j", g=4), el.rearrange("p (g j) -> p g j", g=4),
                           rl[:, 48:52].unsqueeze(2).to_broadcast([128, 4, 8]), ALU.add, [b_rl], [b_rl])
                        S.op("dve", lambda e: e.max(rl[:, 96:104], rl[:, 64:96]), [b_rl], [b_rl])
                        TS(oh[:, 0:32], rl[:, 64:96], rl[:, 96:97], None, ALU.is_equal, None, [b_rl], [b_oh])
                        TS(oh[:, 32:64], rl[:, 64:96], rl[:, 97:98], None, ALU.is_equal, None, [b_rl], [b_oh])
                        TT(oh[:, 64:96], oh[:, 0:32], oh[:, 32:64], ALU.add, [b_oh], [b_oh])
                        CP("dve", ohb[:], oh[:, 64:96], [b_oh], [b_ohb])
                        rtt = rt[:, g, :]
                        w0, bw0 = wk[7], b_wk[7]
                        TT(w0[:, 0:32], oh[:, 0:32], e_iota[:], ALU.mult, [b_oh, b_eiota], [bw0])
                        RSUM(rtt[:, 0:1], w0[:, 0:32], [bw0], [b_rt[g]])
                        TT(w0[:, 32:64], oh[:, 32:64], e_iota[:], ALU.mult, [b_oh, b_eiota], [bw0])
                        RSUM(rtt[:, 1:2], w0[:, 32:64], [bw0], [b_rt[g]])
                        TT(rl[:, 43:44], rl[:, 97:98], rl[:, 96:97], ALU.subtract, [b_rl], [b_rl])
                        ACT(rl[:, 43:44], rl[:, 43:44], AF.Exp, [b_rl], [b_rl])
                        TS(rl[:, 43:44], rl[:, 43:44], 1.0, None, ALU.add, None, [b_rl], [b_rl])
                        RECIP(rl[:, 43:44], rl[:, 43:44], [b_rl], [b_rl])
                        TT(rtt[:, 2:3], rl[:, 43:44], rl[:, 42:43], ALU.mult, [b_rl], [b_rt[g]])
                        TT(rtt[:, 3:4], rl[:, 42:43], rtt[:, 2:3], ALU.subtract, [b_rl, b_rt[g]], [b_rt[g]])
                        pk, bpk = PS()
                        MM(pk[:, 0:32], lstrict[:], ohb[:], True, True, [b_lstrict, b_ohb], [bpk])
                        MM(pk[:, 32:64], ones_bf[:], ohb[:], True, True, [b_ones, b_ohb], [bpk])
                        TT(w0[:, 64:96], pk[:, 0:32], tot[:], ALU.add, [bpk, b_tot], [bw0])
                        TT(tot[:], tot[:], pk[:, 32:64], ALU.add, [bpk, b_tot], [b_tot])
                        TT(w0[:, 0:32], oh[:, 0:32], w0[:, 64:96], ALU.mult, [b_oh, bw0], [bw0])
                        RSUM(rtt[:, 4:5], w0[:, 0:32], [bw0], [b_rt[g]])
                        TT(w0[:, 32:64], oh[:, 32:64], w0[:, 64:96], ALU.mult, [b_oh, bw0], [bw0])
                        RSUM(rtt[:, 5:6], w0[:, 32:64], [bw0], [b_rt[g]])
            if debug:
                S.barrier()
                S.dma("sp", dbg_kc[:, :], kc[:].rearrange("p a b -> p (a b)"), (), [Buf()])
                S.dma("sp", dbg_vc[:, :], vc[:].rearrange("p a b c -> p (a b c)"), (), [Buf()])
                S.dma("sp", dbg_v1[:, :], vec1[:], (), [Buf()])
            S.barrier()

        if debug:
            fin = Buf()
            S.dma("sp", dbg_rt[:, :, :], rt[:], b_rt + [b_tot], [fin])

        if phases >= 2:
            with ExitStack() as p2:
                vec2 = sbt(p2, "vec2_sb", [128, 2048], F32); b_vec2 = Buf()
                S.dma("sp", vec2[:], v2_d[:, :], (), [b_vec2])
                ln2g = vec2[:, 0:1024]
                ln2b = vec2[:, 1024:2048]
                big = sbt(p2, "big", [128, 32 * 160], F32); b_big = Buf()
                rows = sbt(p2, "rows", [128, 512], F32); b_rows = Buf()
                irow = sbt(p2, "irow", [128, 160], I32); b_irow = Buf()
                bexp = sbt(p2, "bexp", [128, 160], I32); b_bexp = Buf()
                dst_i = sbt(p2, "dst_i", [128, NT * 2], I32); b_dst = Buf()
                stats_2 = sbt(p2, "stats2", [128, 16], F32); b_stats2 = Buf()
                S.op("pool", lambda e: e.iota(irow[:], pattern=[[128, 160]], base=0, channel_multiplier=0), (), [b_irow])
                CP("dve", rows[:, 128:288], irow[:], [b_irow], [b_rows])
                TT(big[:, 0:2048].rearrange("p (e k) -> p e k", e=32),
                   tot[:].unsqueeze(2).to_broadcast([128, 32, 64]),
                   rows[:, 128:192].unsqueeze(1).to_broadcast([128, 32, 64]), ALU.is_gt, [b_tot, b_rows], [b_big])
                RSUM(rows[:, 32:64], big[:, 0:2048].rearrange("p (e k) -> p e k", e=32), [b_big], [b_rows])
                TS(rows[:, 32:64], rows[:, 32:64], 128.0, None, ALU.mult, None, [b_rows], [b_rows])
                MEMSET("dve", rows[:, 352:384], 1.0, [b_rows])
                S.op("dve", lambda e: e.tensor_tensor_scan(rows[:, 64:96], rows[:, 352:384], rows[:, 32:64], 0.0,
                                                           ALU.mult, ALU.add), [b_rows], [b_rows])
                TT(rows[:, 96:128], rows[:, 64:96], rows[:, 32:64], ALU.subtract, [b_rows], [b_rows])
                TT(big[:].rearrange("p (b e) -> p b e", b=160),
                   rows[:, 64:96].unsqueeze(1).to_broadcast([128, 160, 32]),
                   rows[:, 128:288].unsqueeze(2).to_broadcast([128, 160, 32]), ALU.is_le, [b_rows], [b_big])
                RSUM(rows[:, 288:448], big[:].rearrange("p (b e) -> p b e", b=160), [b_big], [b_rows])
                TS(rows[:, 288:448], rows[:, 288:448], 31.0, None, ALU.min, None, [b_rows], [b_rows])
                CP("dve", bexp[:], rows[:, 288:448], [b_rows], [b_bexp])
                pio_i = sbt(p2, "pio_i", [128, 1], I32); b_pio = Buf()
                pio_f = sbt(p2, "pio_f", [128, 1], F32)
                idxw_f = sbt(p2, "idxw_f", [128, 160], F32); b_idxwf = Buf()
                idxw = sbt(p2, "idxw", [128, 160], I32); b_idxw = Buf()
                S.op("pool", lambda e: e.iota(pio_i[:], pattern=[[0, 1]], base=0, channel_multiplier=1), (), [b_pio])
                CP("dve", pio_f[:], pio_i[:], [b_pio], [b_pio])
                same_f = sbt(p2, "same_f", [128, 160], F32); b_same = Buf()
                t1_f = sbt(p2, "t1_f", [128, 160], F32); b_t1 = Buf()
                MEMSET("dve", same_f[:], 0.0, [b_same])
                TT(same_f[:, 3:160], rows[:, 291:448], rows[:, 288:445], ALU.is_equal, [b_rows, b_same], [b_same])
                TS(t1_f[:], rows[:, 288:448], 128.0, None, ALU.mult, None, [b_rows], [b_t1])
                TS(idxw_f[:], t1_f[:], -1.0, 4096.0, ALU.mult, ALU.add, [b_t1], [b_idxwf])
                TT(idxw_f[:], idxw_f[:], same_f[:], ALU.mult, [b_idxwf, b_same], [b_idxwf])
                TT(idxw_f[:], idxw_f[:], t1_f[:], ALU.add, [b_idxwf, b_t1], [b_idxwf])
                TS(idxw_f[:], idxw_f[:], pio_f[:, 0:1], None, ALU.add, None, [b_idxwf, b_pio], [b_idxwf])
                CP("dve", idxw[:], idxw_f[:], [b_idxwf], [b_idxw])
                w0 = sbt(p2, "w0", [128, 64], F32); b_w0 = Buf()
                for g in range(NT):
                    for j in range(2):
                        TS(w0[:, 0:32], e_iota[:], rt[:, g, j:j + 1], None, ALU.is_equal, None, [b_eiota, b_rt[g]], [b_w0])
                        TT(w0[:, 0:32], w0[:, 0:32], rows[:, 96:128], ALU.mult, [b_w0, b_rows], [b_w0])
                        RSUM(rt[:, g, 6 + j:7 + j], w0[:, 0:32], [b_w0], [b_rt[g]])
                        TT(rt[:, g, 6 + j:7 + j], rt[:, g, 6 + j:7 + j], rt[:, g, 4 + j:5 + j], ALU.add, [b_rt[g]], [b_rt[g]])
                CP("dve", dst_i[:].rearrange("p (g j) -> p g j", j=2), rt[:, :, 6:8], b_rt, [b_dst])

                dyn = {}

                def bound(e):
                    if "bnd" not in dyn:
                        breg = e.alloc_register("bndreg")
                        e.reg_mov(breg, NSLOT - 1)
                        dyn["bnd"] = e.snap(breg)
                    return dyn["bnd"]

                def boundw(e):
                    if "bndw" not in dyn:
                        breg = e.alloc_register("bndwreg")
                        e.reg_mov(breg, NE * 128 - 1)
                        dyn["bndw"] = e.snap(breg)
                    return dyn["bndw"]

                b_xs_sc = []
                xg = [sbt(p2, "xg%d" % i, [128, D], BF16) for i in range(2)]
                b_xg = [Buf() for _ in range(2)]
                for g in range(NT):
                    k = g % 2
                    S.dma("sp", xg[k][:], x1b_d[g * 128:(g + 1) * 128, :], [b_x1b[g]], [b_xg[k]])
                    for j in range(2):
                        bsc = Buf()
                        S.dma_fn("pool", (lambda e, g=g, j=j, k=k: e.indirect_dma_start(
                            out=xs_d[:, :], out_offset=bass.IndirectOffsetOnAxis(ap=dst_i[:, g * 2 + j:g * 2 + j + 1], axis=0),
                            in_=xg[k][:, :], in_offset=None, bounds_check=bound(e), oob_is_err=False)),
                            [b_xg[k], b_dst] + b_xs_zero, [bsc])
                        b_xs_sc.append(bsc)

                NW = 3
                wall_sb = [sbt(p2, "wall_sb%d" % i, [128, 12288], BF16) for i in range(NW)]
                wgb = [t[:, 0:4096].rearrange("p (k f) -> p k f", k=8) for t in wall_sb]
                wub = [t[:, 4096:8192].rearrange("p (k f) -> p k f", k=8) for t in wall_sb]
                wdb = [t[:, 8192:12288].rearrange("p (k f) -> p k f", k=4) for t in wall_sb]
                b_wgb = [Buf() for _ in range(NW)]
                b_wub = b_wgb
                b_wdb = b_wgb
                xb = [sbt(p2, "xb%d" % i, [128, D], BF16) for i in range(2)]
                b_xb = [Buf() for _ in range(2)]
                xbT = [sbt(p2, "xbT%d" % i, [128, 8, 128], BF16) for i in range(2)]
                b_xbT = [Buf() for _ in range(2)]
                hs_ = [sbt(p2, "hs%d" % i, [128, 512], F32) for i in range(2)]
                b_hs = [Buf() for _ in range(2)]
                hact = [sbt(p2, "hact%d" % i, [128, 512], BF16) for i in range(2)]
                b_hact = [Buf() for _ in range(2)]
                hT = [sbt(p2, "hT%d" % i, [128, 4, 128], BF16) for i in range(2)]
                b_hT = [Buf() for _ in range(2)]
                yb = [sbt(p2, "yb%d" % i, [128, D], F32) for i in range(2)]
                b_yb = [Buf() for _ in range(2)]
                b_ys = [Buf() for _ in range(NBLK)]

                def moe_gather(b):
                    k = b % NW
                    S.dma_fn("pool", (lambda e, k=k, b=b: e.indirect_dma_start(
                        out=wall_sb[k][:, :], out_offset=None, in_=wall_d[:, :],
                        in_offset=bass.IndirectOffsetOnAxis(ap=idxw[:, b:b + 1], axis=0),
                        bounds_check=boundw(e), oob_is_err=False)), [b_idxw] + (b_wall if b == 0 else []), [b_wgb[k]])

                def moe_a(b):
                    k = b % NW
                    j = b % 2
                    S.dma("sp", xb[j][:], xs_d[b * 128:(b + 1) * 128, :], b_xs_sc if b == 0 else [], [b_xb[j]])
                    pt, bpt = PS()
                    ptb = pt[:, :].bitcast(BF16)
                    for kk in range(8):
                        TR(ptb[:, kk * 128:(kk + 1) * 128], xb[j][:, kk * 128:(kk + 1) * 128], ident_bf[:],
                           [b_xb[j], b_ident_bf], [bpt])
                    CP("act", xbT[j][:], ptb[:, :].rearrange("p (k t) -> p k t", k=8), [bpt], [b_xbT[j]])
                    pg, bpg = PS()
                    pu, bpu = PS()
                    for kk in range(8):
                        MM(pg[:, :], xbT[j][:, kk, :], wgb[k][:, kk, :], kk == 0, kk == 7, [b_xbT[j], b_wgb[k]], [bpg])
                    for kk in range(8):
                        MM(pu[:, :], xbT[j][:, kk, :], wub[k][:, kk, :], kk == 0, kk == 7, [b_xbT[j], b_wub[k]], [bpu])
                    ACT(hs_[j][:], pg[:, :], AF.Silu, [bpg], [b_hs[j]])
                    TT(hact[j][:], hs_[j][:], pu[:, :], ALU.mult, [b_hs[j], bpu], [b_hact[j]])

                def moe_b(b):
                    k = b % NW
                    j = b % 2
                    pt, bpt = PS()
                    ptb = pt[:, :].bitcast(BF16)
                    for kk in range(4):
                        TR(ptb[:, kk * 128:(kk + 1) * 128], hact[j][:, kk * 128:(kk + 1) * 128], ident_bf[:],
                           [b_hact[j], b_ident_bf], [bpt])
                    CP("act", hT[j][:], ptb[:, 0:512].rearrange("p (k t) -> p k t", k=4), [bpt], [b_hT[j]])
                    for n in range(2):
                        py, bpy = PS()
                        for kk in range(4):
                            MM(py[:, :], hT[j][:, kk, :], wdb[k][:, kk, n * 512:(n + 1) * 512], kk == 0, kk == 3,
                               [b_hT[j], b_wdb[k]], [bpy])
                        CP("act" if n == 0 else "dve", yb[j][:, n * 512:(n + 1) * 512], py[:, :], [bpy], [b_yb[j]])
                    S.dma("sp", ys_d[b * 128:(b + 1) * 128, :], yb[j][:], [b_yb[j]], [b_ys[b]])

                moe_gather(0)
                moe_gather(1)
                for b in range(NBLK):
                    moe_a(b)
                    if b >= 1:
                        moe_b(b - 1)
                    if b + 2 < NBLK:
                        moe_gather(b + 2)
                moe_b(NBLK - 1)

                y1 = [sbt(p2, "y1_%d" % i, [128, D], F32) for i in range(2)]
                y2 = [sbt(p2, "y2_%d" % i, [128, D], F32) for i in range(2)]
                xr = [sbt(p2, "xr%d" % i, [128, D], F32) for i in range(2)]
                b_y1 = [Buf() for _ in range(2)]
                b_y2 = [Buf() for _ in range(2)]
                b_xr = [Buf() for _ in range(2)]
                b_out = []

                def layer_norm2(zt, bz, out_ap, bout):
                    for hlf in range(2):
                        S.op("dve", lambda e, hlf=hlf: e.bn_stats(stats_2[:, hlf * 6:(hlf + 1) * 6], zt[:, hlf * 512:(hlf + 1) * 512]),
                             [bz], [b_stats2])
                    S.op("dve", lambda e: e.bn_aggr(stats_2[:, 12:14], stats_2[:, 0:12]), [b_stats2], [b_stats2])
                    TS(stats_2[:, 14:15], stats_2[:, 13:14], LN_EPS, None, ALU.add, None, [b_stats2], [b_stats2])
                    ACT(stats_2[:, 14:15], stats_2[:, 14:15], AF.Ln, [b_stats2], [b_stats2])
                    ACT(stats_2[:, 15:16], stats_2[:, 14:15], AF.Exp, [b_stats2], [b_stats2], scale=-0.5)
                    TS(zt, zt, stats_2[:, 12:13], stats_2[:, 15:16], ALU.subtract, ALU.mult, [bz, b_stats2], [bz])
                    TT(zt, zt, ln2g, ALU.mult, [bz, b_vec2], [bz])
                    TT(out_ap, zt, ln2b, ALU.add, [bz, b_vec2], [bout])

                for g in range(NT):
                    k = g % 2
                    S.dma("sp", xr[k][:], x1_d[g * 128:(g + 1) * 128, :], [b_x1[g]], [b_xr[k]])
                    for j, (yt, byt) in enumerate(((y1[k], b_y1[k]), (y2[k], b_y2[k]))):
                        S.dma_fn("pool", (lambda e, g=g, j=j, yt=yt: e.indirect_dma_start(
                            out=yt[:, :], out_offset=None, in_=ys_d[:, :],
                            in_offset=bass.IndirectOffsetOnAxis(ap=dst_i[:, g * 2 + j:g * 2 + j + 1], axis=0),
                            bounds_check=bound(e), oob_is_err=False)), [b_dst] + (b_ys if g == 0 else []), [byt])
                    TS(xr[k][:], xr[k][:], ALPHA, None, ALU.mult, None, [b_xr[k]], [b_xr[k]])
                    SCTT(xr[k][:], y1[k][:], rt[:, g, 2:3], xr[k][:], ALU.mult, ALU.add, [b_y1[k], b_rt[g], b_xr[k]], [b_xr[k]])
                    SCTT(xr[k][:], y2[k][:], rt[:, g, 3:4], xr[k][:], ALU.mult, ALU.add, [b_y2[k], b_rt[g], b_xr[k]], [b_xr[k]])
                    layer_norm2(xr[k][:], b_xr[k], xr[k][:], b_xr[k])
                    bo = Buf()
                    S.dma("sp", out_d[g * 128:(g + 1) * 128, :], xr[k][:], [b_xr[k]], [bo])
                    b_out.append(bo)
                S.barrier()
        else:
            S.barrier()
        S.barrier()
        S.emit()
        print("program: %d instructions, %d waits" % (S.n_ins, S.n_wait))
    return nc


def _weight_chunks(w_in, w_gates, wb_hg, wb_df, wb_mem, w_out, w_mem_kv):
    wf = np.zeros((NCHUNK, 128, CH), np.float32)

    def kchunks(w):
        K, C = w.shape
        return w.reshape(K // 128, 128, C).transpose(1, 0, 2)

    cols = list(range(2048))
    for h in range(4):
        cols += list(range(2048 + h * 64, 2048 + (h + 1) * 64)) + list(range(2304 + h * 64, 2304 + (h + 1) * 64))
    for h in range(4):
        cols += list(range(2560 + h * 64, 2560 + (h + 1) * 64)) + list(range(2816 + h * 64, 2816 + (h + 1) * 64))
    cols += list(range(3072, 4096))
    w_in_p = w_in[:, cols]
    for c in range(8):
        wf[c, :, 0:4096] = kchunks(w_in_p[:, c * 512:(c + 1) * 512]).reshape(128, 4096)
    wbr = [wb_hg, wb_df, wb_mem]
    for cg in range(8):
        g = np.stack([kchunks(w_gates[:, br * 1024 + cg * 128: br * 1024 + (cg + 1) * 128]) for br in range(3)], axis=2)
        wf[8 + cg, :, 0:3072] = g.reshape(128, 3072)
        bb = np.stack([kchunks(wbr[br][:, cg * 128:(cg + 1) * 128]) for br in range(3)], axis=2)
        wf[8 + cg, :, 3072:4608] = bb.reshape(128, 1536)
    for n in range(2):
        wf[16 + n, :, 0:4096] = kchunks(w_out[:, n * 512:(n + 1) * 512]).reshape(128, 4096)
        wf[18 + n, :, 0:4096] = kchunks(w_mem_kv[:, n * 512:(n + 1) * 512]).reshape(128, 4096)
    return wf


_NC_CACHE = {}


def kernel(x, mem, positions, w_in, w_gates, hgrn_lower_bounds, hgrn_norm_gain,
           diff_lambda_q1, diff_lambda_k1, diff_lambda_q2, diff_lambda_k2, diff_subln_gain,
           w_mem_kv, w_branch_hgrn, w_branch_diff, w_branch_mem, w_out, ln1_gain, ln1_bias,
           w_group_router, w_expert_router, w_expert_gate, w_expert_up, w_expert_down,
           ln2_gain, ln2_bias, _debug=False, _phases=3):
    f = lambda a: np.ascontiguousarray(np.asarray(a))
    x = f(x); mem = f(mem); positions = f(positions)
    wf = _weight_chunks(f(w_in)[0], f(w_gates)[0], f(w_branch_hgrn)[0], f(w_branch_diff)[0], f(w_branch_mem)[0],
                        f(w_out)[0], f(w_mem_kv)[0])
    wr = np.concatenate([f(w_group_router)[0], f(w_expert_router)[0]], axis=1)
    wr = np.ascontiguousarray(wr.reshape(8, 128, 36).transpose(1, 0, 2))
    rep = lambda v: np.broadcast_to(np.asarray(v, np.float32).reshape(1, -1), (128, np.asarray(v).size))
    lbr = np.ascontiguousarray(np.concatenate([rep(f(hgrn_lower_bounds)[0]), rep(f(hgrn_lower_bounds)[1])], axis=1),
                               dtype=np.float32)
    vec1 = np.ascontiguousarray(np.concatenate([
        rep(f(hgrn_norm_gain)[0]),
        rep(f(diff_lambda_q1)[0]), rep(f(diff_lambda_k1)[0]), rep(f(diff_lambda_q2)[0]), rep(f(diff_lambda_k2)[0]),
        rep(np.tile(f(diff_subln_gain)[0], 4)), rep(f(ln1_gain)[0]), rep(f(ln1_bias)[0])], axis=1), dtype=np.float32)
    vec2 = np.ascontiguousarray(np.concatenate([rep(f(ln2_gain)[0]), rep(f(ln2_bias)[0])], axis=1), dtype=np.float32)
    def halves(w, kc):
        w2 = w.reshape(NE, kc, 128, -1).transpose(0, 2, 1, 3).reshape(NE * 128, 4096)
        return [np.ascontiguousarray(w2[:, 0:2048]), np.ascontiguousarray(w2[:, 2048:4096])]
    wg = halves(f(w_expert_gate)[0], 8)
    wu = halves(f(w_expert_up)[0], 8)
    wd = halves(f(w_expert_down)[0], 4)

    key = (bool(_debug), int(_phases))
    if key not in _NC_CACHE:
        _NC_CACHE[key] = build_program(debug=_debug, phases=_phases)
    nc = _NC_CACHE[key]
    in_maps = []
    for c in range(NCORES):
        xb = x[c * NSEQ:(c + 1) * NSEQ].reshape(NTOK, D)
        mb = mem[c * NSEQ:(c + 1) * NSEQ].reshape(NSEQ * 256, D)
        pb = positions[c * NSEQ:(c + 1) * NSEQ].reshape(NT, 128).T
        in_maps.append(dict(x=np.ascontiguousarray(xb), mem=np.ascontiguousarray(mb),
                            pos=np.ascontiguousarray(pb.astype(np.int32)), wf=wf, wr=wr, vec1=vec1, vec2=vec2, lbr=lbr,
                            wg0=wg[0], wg1=wg[1], wu0=wu[0], wu1=wu[1], wd0=wd[0], wd1=wd[1]))
    res = run_bass_kernel_spmd(nc, in_maps, core_ids=list(range(NCORES)))
    if _debug:
        return res.results
    out = np.concatenate([r["out"].reshape(NSEQ, SEQ, D) for r in res.results], axis=0)
    return out.astype(np.float32)
```

```python
import math
from contextlib import ExitStack

import numpy as np
import concourse.bass as bass
import concourse.mybir as mybir
from concourse.bass_utils import run_bass_kernel_spmd

F32 = mybir.dt.float32
BF16 = mybir.dt.bfloat16
I32 = mybir.dt.int32
AF = mybir.ActivationFunctionType
ALU = mybir.AluOpType
AX = mybir.AxisListType

NCORES = 8
D = 1024
SEQ = 4096
NSEQ = 2
NTOK = NSEQ * SEQ
NT = NTOK // 128
TPS = SEQ // 128
STT = 2
TW = STT * 128
NST = NT // STT
CH = 4608
NCHUNK = 20
NE = 32
NSLOT = NTOK * 2 + NE * 128
NBLK = NSLOT // 128
ALPHA = 2.0 ** 0.25
LN_EPS = 1e-5
RMS_EPS = 1e-6
TWO_PI = 2.0 * math.pi

SAME_ENGINE_SYNC = True


class Buf:
    __slots__ = ("name", "w", "r")

    def __init__(self, name=""):
        self.name = name
        self.w = None
        self.r = {}


class Sched:
    def __init__(self, nc, stack, n_dma_sems=32):
        self.nc = nc
        self.names = ["pe", "act", "dve", "pool", "sp"]
        self.sem = {}
        self.cnt = {}
        self.known = {}
        for k in self.names:
            self.sem[k] = stack.enter_context(nc.semaphore("s_" + k))
            self.cnt[k] = 0
            self.known[k] = {}
        self.dsem = [stack.enter_context(nc.semaphore("d%d" % i)) for i in range(n_dma_sems)]
        self.dcnt = [0] * n_dma_sems
        self.dnext = 0
        self.dnext_pool = 0
        self.n_wait = 0
        self.n_ins = 0
        self.prog = {k: [] for k in self.names}

    def _need(self, e, deps, sem, val, who):
        if who == e and not SAME_ENGINE_SYNC:
            return
        if who == "pe" and e == "pe":
            return
        key = id(sem)
        if deps.get(key, (None, 0))[1] < val:
            deps[key] = (sem, val)

    def _collect(self, e, reads, writes):
        deps = {}
        for b in reads:
            if b.w is not None:
                self._need(e, deps, b.w[0], b.w[1], b.w[2])
        for b in writes:
            if b.w is not None:
                self._need(e, deps, b.w[0], b.w[1], b.w[2])
            for (sem, (val, who)) in b.r.values():
                self._need(e, deps, sem, val, who)
        return deps

    def _emit_waits(self, e, deps):
        kn = self.known[e]
        for key, (sem, val) in deps.items():
            if kn.get(key, 0) >= val:
                continue
            self.prog[e].append((0, sem, val))
            kn[key] = val
            self.n_wait += 1

    def _mark(self, reads, writes, sem, val, who):
        for b in reads:
            b.r[id(sem)] = (sem, (val, who))
        for b in writes:
            b.w = (sem, val, who)
            b.r = {}

    def op(self, e, fn, reads=(), writes=()):
        deps = self._collect(e, reads, writes)
        self._emit_waits(e, deps)
        self.cnt[e] += 1
        self.prog[e].append((1, fn, self.sem[e], 1))
        self._mark(reads, writes, self.sem[e], self.cnt[e], e)
        self.n_ins += 1

    def dma_fn(self, q, fn, reads=(), writes=()):
        n = len(self.dsem)
        npool = 8
        if q == "pool":
            i = n - npool + (self.dnext_pool % npool)
            self.dnext_pool += 1
        else:
            i = self.dnext % (n - npool)
            self.dnext += 1
        sem = self.dsem[i]
        deps = self._collect(q, reads, writes)
        if self.dcnt[i] > 0:
            key = id(sem)
            if deps.get(key, (None, 0))[1] < self.dcnt[i]:
                deps[key] = (sem, self.dcnt[i])
        self._emit_waits(q, deps)
        self.prog[q].append((1, fn, sem, 16))
        self.dcnt[i] += 16
        self._mark(reads, writes, sem, self.dcnt[i], "dma")
        self.n_ins += 1

    def dma(self, q, out, in_, reads=(), writes=()):
        self.dma_fn(q, (lambda eng, out=out, in_=in_: eng.dma_start(out=out, in_=in_)), reads, writes)

    def barrier(self):
        for e in self.names:
            deps = {}
            for o in self.names:
                if o != e and self.cnt[o] > 0:
                    deps[id(self.sem[o])] = (self.sem[o], self.cnt[o])
            for i, s in enumerate(self.dsem):
                if self.dcnt[i] > 0:
                    deps[id(s)] = (s, self.dcnt[i])
            self._emit_waits(e, deps)

    def emit(self):
        def replay(e):
            def body(eng):
                for item in self.prog[e]:
                    if item[0] == 0:
                        eng.wait_ge(item[1], item[2])
                    else:
                        item[1](eng).then_inc(item[2], item[3])
            return body

        with self.nc.Block() as block:
            block.sync(replay("sp"))
            block.tensor(replay("pe"))
            block.scalar(replay("act"))
            block.vector(replay("dve"))
            block.gpsimd(replay("pool"))


def build_program(debug=False, phases=3):
    nc = bass.Bass("TRN2", target_bir_lowering=False)
    dk = "ExternalOutput" if debug else "Internal"
    x_d = nc.dram_tensor("x", [NTOK, D], F32, kind="ExternalInput")
    mem_d = nc.dram_tensor("mem", [NSEQ * 256, D], F32, kind="ExternalInput")
    pos_d = nc.dram_tensor("pos", [128, NT], I32, kind="ExternalInput")
    wf_d = nc.dram_tensor("wf", [NCHUNK, 128, CH], F32, kind="ExternalInput")
    wr_d = nc.dram_tensor("wr", [128, 8, 36], F32, kind="ExternalInput")
    v1_d = nc.dram_tensor("vec1", [128, 3328], F32, kind="ExternalInput")
    lbr_d = nc.dram_tensor("lbr", [128, 1024], F32, kind="ExternalInput")
    v2_d = nc.dram_tensor("vec2", [128, 2048], F32, kind="ExternalInput")
    wg_d = [nc.dram_tensor("wg%d" % i, [NE * 128, 2048], F32, kind="ExternalInput") for i in range(2)]
    wu_d = [nc.dram_tensor("wu%d" % i, [NE * 128, 2048], F32, kind="ExternalInput") for i in range(2)]
    wd_d = [nc.dram_tensor("wd%d" % i, [NE * 128, 2048], F32, kind="ExternalInput") for i in range(2)]
    out_d = nc.dram_tensor("out", [NTOK, D], F32, kind="ExternalOutput")
    wb_d = nc.dram_tensor("wb", [NCHUNK, 128, CH], BF16, kind="Internal")
    x1_d = nc.dram_tensor("x1s", [NTOK, D], F32, kind=dk)
    x1b_d = nc.dram_tensor("x1b", [NTOK, D], BF16, kind="Internal")
    xs_d = nc.dram_tensor("xsl", [NSLOT, D], BF16, kind="Internal")
    ys_d = nc.dram_tensor("ysl", [NSLOT, D], F32, kind="Internal")
    wall_d = nc.dram_tensor("wall", [NE * 128, 12288], BF16, kind="Internal")
    if debug:
        dbg_hg = nc.dram_tensor("dbg_hg", [NTOK, 512], F32, kind="ExternalOutput")
        dbg_df = nc.dram_tensor("dbg_df", [NTOK, 512], F32, kind="ExternalOutput")
        dbg_mem = nc.dram_tensor("dbg_mem", [NTOK, 512], F32, kind="ExternalOutput")
        dbg_rt = nc.dram_tensor("dbg_rt", [128, NT, 8], F32, kind="ExternalOutput")
        dbg_kc = nc.dram_tensor("dbg_kc", [128, 4 * SEQ], BF16, kind="ExternalOutput")
        dbg_vc = nc.dram_tensor("dbg_vc", [128, TPS * 4 * 130], BF16, kind="ExternalOutput")
        dbg_v1 = nc.dram_tensor("dbg_v1", [128, 3328], F32, kind="ExternalOutput")

    with ExitStack() as top:
        S = Sched(nc, top)

        def sbt(st, name, shape, dt):
            return st.enter_context(nc.sbuf_tensor(name, shape, dt))

        def MM(out, lhsT, rhs, st_, sp_, rd, wr):
            S.op("pe", lambda e: e.matmul(out, lhsT, rhs, start=st_, stop=sp_), rd, wr)

        def TR(out, in_, idn, rd, wr):
            S.op("pe", lambda e: e.transpose(out, in_, idn), rd, wr)

        def ACT(out, in_, func, rd, wr, bias=None, scale=None, accum=None):
            kw = {}
            if bias is not None:
                kw["bias"] = bias
            if scale is not None:
                kw["scale"] = scale
            if accum is not None:
                kw["accum_out"] = accum
            S.op("act", lambda e: e.activation(out, in_, func, **kw), rd, wr)

        def TT(out, a, b, op, rd, wr, eng="dve"):
            S.op(eng, lambda e: e.tensor_tensor(out, a, b, op), rd, wr)

        def TS(out, a, s1, s2, op0, op1, rd, wr, eng="dve"):
            if s2 is None:
                S.op(eng, lambda e: e.tensor_scalar(out, a, s1, None, op0), rd, wr)
            else:
                S.op(eng, lambda e: e.tensor_scalar(out, a, s1, s2, op0, op1), rd, wr)

        def SCTT(out, in0, scalar, in1, op0, op1, rd, wr):
            S.op("dve", lambda e: e.scalar_tensor_tensor(out, in0, scalar, in1, op0, op1), rd, wr)

        def CP(eng, out, in_, rd, wr):
            if eng == "act":
                S.op("act", lambda e: e.copy(out, in_), rd, wr)
            else:
                S.op(eng, lambda e: e.tensor_copy(out, in_), rd, wr)

        def RSUM(out, in_, rd, wr):
            S.op("dve", lambda e: e.reduce_sum(out, in_, AX.X), rd, wr)

        def RMAX(out, in_, rd, wr):
            S.op("dve", lambda e: e.reduce_max(out, in_, AX.X), rd, wr)

        def RECIP(out, in_, rd, wr):
            S.op("dve", lambda e: e.reciprocal(out, in_), rd, wr)

        def MEMSET(eng, ap, val, wr):
            S.op(eng, lambda e: e.memset(ap, val), (), wr)

        def ASEL(out, in_, pattern, cmp, fill, base, cm, rd, wr):
            S.op("pool", lambda e: e.affine_select(out, in_, pattern=pattern, compare_op=cmp, fill=fill,
                                                   base=base, channel_multiplier=cm), rd, wr)

        psum = [top.enter_context(nc.psum_tensor("ps%d" % i, [128, 512], F32)) for i in range(8)]
        psb = [Buf("ps%d" % i) for i in range(8)]
        rot = {"n": 0}

        def PS():
            i = rot["n"] % 5
            rot["n"] += 1
            return psum[i], psb[i]

        ACC = [(psum[5], psb[5]), (psum[6], psb[6]), (psum[7], psb[7])]

        cst = top
        ident_bf = sbt(cst, "ident_bf", [128, 128], BF16); b_ident_bf = Buf()
        ident_f = sbt(cst, "ident_f", [128, 128], F32); b_ident_f = Buf()
        maskT = sbt(cst, "maskT", [128, 128], F32); b_maskT = Buf()
        lstrict = sbt(cst, "lstrict", [128, 128], BF16); b_lstrict = Buf()
        ones_bf = sbt(cst, "ones_bf", [128, 128], BF16); b_ones = Buf()
        e_iota = sbt(cst, "e_iota", [128, 32], F32); b_eiota = Buf()
        rt = sbt(cst, "rt", [128, NT, 8], F32)
        b_rt = [Buf() for _ in range(NT)]
        tot = sbt(cst, "tot", [128, 32], F32); b_tot = Buf()
        wr_sb = sbt(cst, "wr_sb", [128, 8, 36], F32); b_wr = Buf()

        S.barrier()
        MEMSET("pool", ident_bf[:], 1.0, [b_ident_bf])
        ASEL(ident_bf[:], ident_bf[:], [[-1, 128]], ALU.is_equal, 0.0, 0, 1, [b_ident_bf], [b_ident_bf])
        MEMSET("pool", ident_f[:], 1.0, [b_ident_f])
        ASEL(ident_f[:], ident_f[:], [[-1, 128]], ALU.is_equal, 0.0, 0, 1, [b_ident_f], [b_ident_f])
        MEMSET("pool", maskT[:], 1.0, [b_maskT])
        ASEL(maskT[:], maskT[:], [[1, 128]], ALU.is_ge, 0.0, 0, -1, [b_maskT], [b_maskT])
        MEMSET("pool", lstrict[:], 1.0, [b_lstrict])
        ASEL(lstrict[:], lstrict[:], [[1, 128]], ALU.is_gt, 0.0, 0, -1, [b_lstrict], [b_lstrict])
        MEMSET("pool", ones_bf[:], 1.0, [b_ones])
        ei_i = sbt(cst, "ei_i", [128, 32], I32); b_eii = Buf()
        S.barrier()
        MEMSET("dve", tot[:], 0.0, [b_tot])
        S.op("pool", lambda e: e.iota(ei_i[:], pattern=[[1, 32]], base=0, channel_multiplier=0), (), [b_eii])
        CP("dve", e_iota[:], ei_i[:], [b_eii], [b_eiota])
        S.dma("sp", wr_sb[:], wr_d[:, :, :], (), [b_wr])

        b_wb = [Buf() for _ in range(NCHUNK)]
        with ExitStack() as pro:
            cb = [sbt(pro, "castbuf%d" % i, [128, CH], BF16) for i in range(3)]
            b_cb = [Buf() for _ in range(3)]
            zt = sbt(pro, "zt", [128, 4096], BF16); b_zt = Buf()
            S.barrier()
            for c in range(NCHUNK):
                k = c % 3
                for part in range(3):
                    S.dma("pool", cb[k][:, part * 1536:(part + 1) * 1536], wf_d[c, :, part * 1536:(part + 1) * 1536],
                          (), [b_cb[k]])
                S.dma("sp", wb_d[c, :, :], cb[k][:], [b_cb[k]], [b_wb[c]])
            MEMSET("dve", zt[:], 0.0, [b_zt])
            b_xs_zero = []
            xs_v = xs_d[:, :].rearrange("(n p r) d -> n p (r d)", p=128, r=4)
            for n in range(NSLOT // 512):
                bz = Buf()
                S.dma("sp", xs_v[n], zt[:], [b_zt], [bz])
                b_xs_zero.append(bz)
        S.barrier()

        b_x1 = [Buf() for _ in range(NT)]
        b_x1b = [Buf() for _ in range(NT)]
        b_wall = []
        with ExitStack() as p1:
            vec1 = sbt(p1, "vec1_sb", [128, 3328], F32); b_vec1 = Buf()
            S.dma("sp", vec1[:], v1_d[:, :], (), [b_vec1])
            hg_gain = vec1[:, 0:512]
            lamv = vec1[:, 512:768]
            df_gain = vec1[:, 768:1280]
            ln1g = vec1[:, 1280:2304]
            ln1b = vec1[:, 2304:3328]
            lbv = sbt(p1, "lbv", [128, 512], F32); b_lbv = Buf()
            oml = sbt(p1, "oml", [128, 512], F32); b_oml = Buf()
            lam_t = sbt(p1, "lam_t", [128, 4], F32); b_lam = Buf()
            lscr = sbt(p1, "lscr", [128, 128], F32); b_lscr = Buf()
            TT(lscr[:, 0:64], lamv[:, 0:64], lamv[:, 64:128], ALU.mult, [b_vec1], [b_lscr])
            TT(lscr[:, 64:128], lamv[:, 128:192], lamv[:, 192:256], ALU.mult, [b_vec1], [b_lscr])
            RSUM(lam_t[:, 0:2], lscr[:].rearrange("p (a b) -> p a b", a=2), [b_lscr], [b_lam])
            ACT(lam_t[:, 0:2], lam_t[:, 0:2], AF.Exp, [b_lam], [b_lam])
            TT(lam_t[:, 2:3], lam_t[:, 0:1], lam_t[:, 1:2], ALU.subtract, [b_lam], [b_lam])
            TS(lam_t[:, 3:4], lam_t[:, 2:3], 0.2, None, ALU.add, None, [b_lam], [b_lam])
            lam_ap = lam_t[:, 3:4]
            dmat = sbt(p1, "dmat", [128, 128], F32); b_dmat = Buf()
            dtmp = sbt(p1, "dtmp", [128, 128], F32); b_dtmp = Buf()
            cols2 = sbt(p1, "cols2", [128, 2], F32); b_cols2 = Buf()
            MEMSET("pool", dtmp[:], 1.0, [b_dtmp])
            ASEL(dtmp[:], dtmp[:], [[0, 128]], ALU.is_ge, 0.0, 63, -1, [b_dtmp], [b_dtmp])
            TT(dmat[:], dtmp[:], maskT[:], ALU.subtract, [b_dtmp, b_maskT], [b_dmat], eng="pool")
            MEMSET("pool", cols2[:], 1.0, [b_cols2])
            CP("pool", cols2[:, 1:2], dtmp[:, 0:1], [b_dtmp, b_cols2], [b_cols2])
            trig = sbt(p1, "trig", [128, 2, NT, 8], F32); b_trig = Buf()
            with ExitStack() as tr:
                lbr = sbt(tr, "lbr_sb", [128, 1024], F32); b_lbr = Buf()
                S.dma("sp", lbr[:], lbr_d[:, :], (), [b_lbr])
                TT(lbv[:], lbr[:, 0:512], lbr[:, 512:1024], ALU.subtract, [b_lbr], [b_lbv])
                ACT(lbv[:], lbv[:], AF.Sigmoid, [b_lbv], [b_lbv])
                TS(oml[:], lbv[:], -1.0, 1.0, ALU.mult, ALU.add, [b_lbv], [b_oml])
                posi = sbt(tr, "posi", [128, NT], I32); b_posi = Buf()
                posf = sbt(tr, "posf", [128, NT], F32); b_posf = Buf()
                u = sbt(tr, "u", [128, 2, NT, 8], F32); b_u = Buf()
                kf = sbt(tr, "kf", [128, 2 * NT * 8], F32); b_kf = Buf()
                ki = sbt(tr, "ki", [128, 2 * NT * 8], I32); b_ki = Buf()
                S.dma("sp", posi[:], pos_d[:, :], (), [b_posi])
                CP("dve", posf[:], posi[:], [b_posi], [b_posf])
                for j in range(8):
                    invf = 500000.0 ** (-(2.0 * j) / 16.0)
                    TS(u[:, 0, :, j], posf[:], float(np.float32(invf)), None, ALU.mult, None, [b_posf], [b_u])
                TS(u[:, 1, :, :], u[:, 0, :, :], math.pi / 2.0, None, ALU.add, None, [b_u], [b_u])
                uf = u[:].rearrange("p a n j -> p (a n j)")
                TS(kf[:], uf, 1.0 / TWO_PI, None, ALU.mult, None, [b_u], [b_kf])
                CP("dve", ki[:], kf[:], [b_kf], [b_ki])
                CP("dve", kf[:], ki[:], [b_ki], [b_kf])
                C1 = 6.28125
                C2 = TWO_PI - C1
                SCTT(uf, kf[:], -C1, uf, ALU.mult, ALU.add, [b_kf, b_u], [b_u])
                SCTT(uf, kf[:], -C2, uf, ALU.mult, ALU.add, [b_kf, b_u], [b_u])
                TS(kf[:], uf, math.pi, -TWO_PI, ALU.is_gt, ALU.mult, [b_u], [b_kf])
                TT(uf, uf, kf[:], ALU.add, [b_u, b_kf], [b_u])
                TS(kf[:], uf, -math.pi, TWO_PI, ALU.is_lt, ALU.mult, [b_u], [b_kf])
                TT(uf, uf, kf[:], ALU.add, [b_u, b_kf], [b_u])
                TS(uf, uf, -3.1415925, 3.1415925, ALU.max, ALU.min, [b_u], [b_u])
                ACT(trig[:].rearrange("p a n j -> p (a n j)"), uf, AF.Sin, [b_u], [b_trig])
                S.barrier()

            kc = sbt(p1, "kc", [128, 4, SEQ], BF16)
            b_kc = [Buf() for _ in range(TPS)]
            vc = sbt(p1, "vc", [128, TPS, 4, 130], BF16)
            b_vc = [Buf() for _ in range(TPS)]
            b_vc1 = Buf()
            MEMSET("pool", vc[:, :, :, 128:130], 1.0, [b_vc1])
            mkT = sbt(p1, "mkT", [128, 4, 256], BF16); b_mkT = Buf()
            mvc = sbt(p1, "mvc", [128, 2, 4, 130], BF16); b_mvc = Buf()
            b_mvc1 = Buf()
            MEMSET("pool", mvc[:, :, :, 128:130], 1.0, [b_mvc1])
            Sst = sbt(p1, "Sst", [128, 4, 128], F32); b_S = Buf()
            Smm = sbt(p1, "Smm", [128, 4, 128], BF16); b_Smm = Buf()
            wbuf = [sbt(p1, "wbuf%d" % i, [128, CH], BF16) for i in range(2)]
            b_wbuf = [Buf() for _ in range(2)]
            worder = []
            for _sq in range(NSEQ):
                worder += [18, 19]
                for _st in range(TPS // STT):
                    worder += list(range(18))
            wst = {"i": 0, "issued": {}}

            def _issue_chunk(i):
                c = worder[i]
                k = i % 2
                S.dma("sp", wbuf[k][:], wb_d[c, :, :], [b_wb[c]], [b_wbuf[k]])
                return wbuf[k], b_wbuf[k]

            def load_chunk(c):
                i = wst["i"]
                assert worder[i] == c, (i, worder[i], c)
                if i not in wst["issued"]:
                    wst["issued"][i] = _issue_chunk(i)
                cur = wst["issued"].pop(i)
                if i + 1 < len(worder) and (i + 1) not in wst["issued"]:
                    wst["issued"][i + 1] = _issue_chunk(i + 1)
                wst["i"] += 1
                return cur

            xs = sbt(p1, "xs", [128, STT, D], F32); b_xs = [Buf() for _ in range(STT)]
            xbf = sbt(p1, "xbf", [128, D], BF16); b_xbf = Buf()
            xT = sbt(p1, "xT", [128, 8, TW], BF16); b_xT = Buf()
            sq = sbt(p1, "sq", [128, STT, 512], F32); b_sq = [Buf() for _ in range(STT)]
            sg = sbt(p1, "sg", [128, STT, 512], F32); b_sg = [Buf() for _ in range(STT)]
            vhg = sbt(p1, "vhg", [128, STT, 512], BF16); b_vhg = [Buf() for _ in range(STT)]
            gsl = sbt(p1, "gsl", [128, STT, 512], BF16); b_gsl = [Buf() for _ in range(STT)]
            qT = sbt(p1, "qT", [128, 4, TW], BF16); b_qT = [Buf() for _ in range(STT)]
            mqT = sbt(p1, "mqT", [128, 4, TW], BF16); b_mqT = Buf()
            yT = [sbt(p1, "yT%d" % i, [128, 4, TW], BF16) for i in range(3)]
            b_yT = [[Buf() for _ in range(STT)] for _ in range(3)]
            mergedT = sbt(p1, "mergedT", [128, 8, TW], BF16); b_mg = [Buf() for _ in range(8)]
            wk = [sbt(p1, "wk%d" % i, [128, 512], F32) for i in range(8)]
            b_wk = [Buf() for _ in range(8)]
            wkb = [sbt(p1, "wkb%d" % i, [128, 512], BF16) for i in range(6)]
            b_wkb = [Buf() for _ in range(6)]
            NPT = 5
            LOOKAHEAD = 3
            pT = [sbt(p1, "pT%d" % i, [128, 512], BF16) for i in range(NPT)]
            b_pT = [Buf() for _ in range(NPT)]
            psel = {"n": 0}
            sm = sbt(p1, "sm", [128, 64], F32); b_sm = Buf()
            z = sbt(p1, "z", [128, D], F32); b_z = Buf()
            z_b = sbt(p1, "z_b", [128, D], F32); b_zB = Buf()
            zb = sbt(p1, "zb", [128, D], BF16); b_zb = Buf()
            x1T = sbt(p1, "x1T", [128, 8, 128], F32); b_x1T = Buf()
            stats = sbt(p1, "stats", [128, 16], F32); b_stats = Buf()
            rl = sbt(p1, "rl", [128, 128], F32); b_rl = Buf()
            oh = sbt(p1, "oh", [128, 96], F32); b_oh = Buf()
            ohb = sbt(p1, "ohb", [128, 32], BF16); b_ohb = Buf()

            def layer_norm(zt, bz, gain, bias, bgb, out_ap, bout):
                for hlf in range(2):
                    S.op("dve", lambda e, hlf=hlf: e.bn_stats(stats[:, hlf * 6:(hlf + 1) * 6], zt[:, hlf * 512:(hlf + 1) * 512]),
                         [bz], [b_stats])
                S.op("dve", lambda e: e.bn_aggr(stats[:, 12:14], stats[:, 0:12]), [b_stats], [b_stats])
                TS(stats[:, 14:15], stats[:, 13:14], LN_EPS, None, ALU.add, None, [b_stats], [b_stats])
                ACT(stats[:, 14:15], stats[:, 14:15], AF.Ln, [b_stats], [b_stats])
                ACT(stats[:, 15:16], stats[:, 14:15], AF.Exp, [b_stats], [b_stats], scale=-0.5)
                TS(zt, zt, stats[:, 12:13], stats[:, 15:16], ALU.subtract, ALU.mult, [bz, b_stats], [bz])
                TT(zt, zt, gain, ALU.mult, [bz] + bgb, [bz])
                TT(out_ap, zt, bias, ALU.add, [bz] + bgb, [bout])

            def rms_gate(o_sb, bo, extra_scale, gain_ap, bgain, gate_ap, bgate, out_bf, bout):
                o3 = o_sb.rearrange("p (h v) -> p h v", h=4)
                w0, bw0 = wk[7], b_wk[7]
                TT(w0[:], o_sb, o_sb, ALU.mult, [bo], [bw0])
                RSUM(sm[:, 0:4], w0[:].rearrange("p (h v) -> p h v", h=4), [bw0], [b_sm])
                TS(sm[:, 0:4], sm[:, 0:4], 1.0 / 128.0, RMS_EPS, ALU.mult, ALU.add, [b_sm], [b_sm])
                ACT(sm[:, 0:4], sm[:, 0:4], AF.Ln, [b_sm], [b_sm])
                ACT(sm[:, 4:8], sm[:, 0:4], AF.Exp, [b_sm], [b_sm], scale=-0.5)
                if extra_scale != 1.0:
                    TS(sm[:, 4:8], sm[:, 4:8], float(extra_scale), None, ALU.mult, None, [b_sm], [b_sm])
                TT(w0[:].rearrange("p (h v) -> p h v", h=4), o3, sm[:, 4:8].unsqueeze(2).to_broadcast([128, 4, 128]),
                   ALU.mult, [bo, b_sm], [bw0])
                if gate_ap is not None:
                    TT(w0[:], w0[:], gain_ap, ALU.mult, [bw0] + bgain, [bw0])
                    TT(out_bf, w0[:], gate_ap, ALU.mult, [bw0] + bgate, [bout])
                else:
                    TT(out_bf, w0[:], gain_ap, ALU.mult, [bw0] + bgain, [bout])

            def transpose_to(y_bf, by, dstT, bdst, col0):
                pt, bpt = PS()
                ptb = pt[:, :].bitcast(BF16)
                for c4 in range(4):
                    TR(ptb[:, c4 * 128:(c4 + 1) * 128], y_bf[:, c4 * 128:(c4 + 1) * 128], ident_bf[:],
                       [by, b_ident_bf], [bpt])
                CP("act", dstT[:, :, col0:col0 + 128], ptb[:, 0:512].rearrange("p (c t) -> p c t", c=4), [bpt], [bdst])

            def router(g, zc, b_zc):
                for half in range(2):
                    pt, bpt = PS()
                    for k4 in range(4):
                        k = half * 4 + k4
                        TR(pt[:, k4 * 128:(k4 + 1) * 128], zc[:, k * 128:(k + 1) * 128], ident_f[:],
                           [b_zc, b_ident_f], [bpt])
                    CP("act", x1T[:, half * 4:(half + 1) * 4, :], pt[:, :].rearrange("p (k t) -> p k t", k=4),
                       [bpt], [b_x1T])
                pr, bpr = PS()
                for k in range(8):
                    MM(pr[:, 0:36], x1T[:, k, :], wr_sb[:, k, :], k == 0, k == 7, [b_x1T, b_wr], [bpr])
                CP("dve", rl[:, 0:36], pr[:, 0:36], [bpr], [b_rl])
                gl = rl[:, 0:4]
                el = rl[:, 4:36]
                RMAX(rl[:, 40:41], gl, [b_rl], [b_rl])
                TS(rl[:, 44:48], gl, rl[:, 40:41], None, ALU.subtract, None, [b_rl], [b_rl])
                ACT(rl[:, 44:48], rl[:, 44:48], AF.Exp, [b_rl], [b_rl])
                RSUM(rl[:, 41:42], rl[:, 44:48], [b_rl], [b_rl])
                RECIP(rl[:, 42:43], rl[:, 41:42], [b_rl], [b_rl])
                TS(rl[:, 48:52], gl, rl[:, 40:41], None, ALU.is_ge, None, [b_rl], [b_rl])
                TS(rl[:, 48:52], rl[:, 48:52], -1.0, 1e30, ALU.add, ALU.mult, [b_rl], [b_rl])
                TT(rl[:, 64:96].rearrange("p (g j) -> p g j", g=4), el.rearrange("p (g j) -> p g j", g=4),
                   rl[:, 48:52].unsqueeze(2).to_broadcast([128, 4, 8]), ALU.add, [b_rl], [b_rl])
                S.op("dve", lambda e: e.max(rl[:, 96:104], rl[:, 64:96]), [b_rl], [b_rl])
                TS(oh[:, 0:32], rl[:, 64:96], rl[:, 96:97], None, ALU.is_equal, None, [b_rl], [b_oh])
                TS(oh[:, 32:64], rl[:, 64:96], rl[:, 97:98], None, ALU.is_equal, None, [b_rl], [b_oh])
                TT(oh[:, 64:96], oh[:, 0:32], oh[:, 32:64], ALU.add, [b_oh], [b_oh])
                CP("dve", ohb[:], oh[:, 64:96], [b_oh], [b_ohb])
                rtt = rt[:, g, :]
                w0, bw0 = wk[7], b_wk[7]
                TT(w0[:, 0:32], oh[:, 0:32], e_iota[:], ALU.mult, [b_oh, b_eiota], [bw0])
                RSUM(rtt[:, 0:1], w0[:, 0:32], [bw0], [b_rt[g]])
                TT(w0[:, 32:64], oh[:, 32:64], e_iota[:], ALU.mult, [b_oh, b_eiota], [bw0])
                RSUM(rtt[:, 1:2], w0[:, 32:64], [bw0], [b_rt[g]])
                TT(rl[:, 43:44], rl[:, 97:98], rl[:, 96:97], ALU.subtract, [b_rl], [b_rl])
                ACT(rl[:, 43:44], rl[:, 43:44], AF.Exp, [b_rl], [b_rl])
                TS(rl[:, 43:44], rl[:, 43:44], 1.0, None, ALU.add, None, [b_rl], [b_rl])
                RECIP(rl[:, 43:44], rl[:, 43:44], [b_rl], [b_rl])
                TT(rtt[:, 2:3], rl[:, 43:44], rl[:, 42:43], ALU.mult, [b_rl], [b_rt[g]])
                TT(rtt[:, 3:4], rl[:, 42:43], rtt[:, 2:3], ALU.subtract, [b_rl, b_rt[g]], [b_rt[g]])
                pk, bpk = PS()
                MM(pk[:, 0:32], lstrict[:], ohb[:], True, True, [b_lstrict, b_ohb], [bpk])
                MM(pk[:, 32:64], ones_bf[:], ohb[:], True, True, [b_ones, b_ohb], [bpk])
                TT(w0[:, 64:96], pk[:, 0:32], tot[:], ALU.add, [bpk, b_tot], [bw0])
                TT(tot[:], tot[:], pk[:, 32:64], ALU.add, [bpk, b_tot], [b_tot])
                TT(w0[:, 0:32], oh[:, 0:32], w0[:, 64:96], ALU.mult, [b_oh, bw0], [bw0])
                RSUM(rtt[:, 4:5], w0[:, 0:32], [bw0], [b_rt[g]])
                TT(w0[:, 32:64], oh[:, 32:64], w0[:, 64:96], ALU.mult, [b_oh, bw0], [bw0])
                RSUM(rtt[:, 5:6], w0[:, 32:64], [bw0], [b_rt[g]])


            pending_router = []

            def flush_router():
                while pending_router:
                    router(*pending_router.pop(0))

            for seq in range(NSEQ):
                flush_router()
                MEMSET("dve", Sst[:], 0.0, [b_S])
                with ExitStack() as ms:
                    mems, b_mems = z, b_z
                    memb, b_memb = zb, b_zb
                    memTt = sbt(ms, "memTt%d" % seq, [128, 8, 256], BF16); b_memT = Buf()
                    for nb in range(2):
                        S.dma("sp", mems[:], mem_d[seq * 256 + nb * 128: seq * 256 + (nb + 1) * 128, :], (), [b_mems])
                        CP("dve", memb[:], mems[:], [b_mems], [b_memb])
                        pt, bpt = PS()
                        ptb = pt[:, :].bitcast(BF16)
                        for k in range(8):
                            TR(ptb[:, k * 128:(k + 1) * 128], memb[:, k * 128:(k + 1) * 128], ident_bf[:],
                               [b_memb, b_ident_bf], [bpt])
                        CP("act", memTt[:, :, nb * 128:(nb + 1) * 128], ptb[:, :].rearrange("p (k t) -> p k t", k=8),
                           [bpt], [b_memT])
                    wt, bw = load_chunk(18)
                    w3 = wt[:, 0:4096].rearrange("p (k c) -> p k c", k=8)
                    pt, bpt = PS()
                    pt2, bpt2 = PS()
                    for h in range(4):
                        po = (pt if h < 2 else pt2)
                        bpo = (bpt if h < 2 else bpt2)
                        for k in range(8):
                            MM(po[:, (h % 2) * 256:(h % 2) * 256 + 256], w3[:, k, h * 128:(h + 1) * 128], memTt[:, k, :],
                               k == 0, k == 7, [bw, b_memT], [bpo])
                    CP("act", mkT[:, 0:2, :], pt[:, :].rearrange("p (h n) -> p h n", h=2), [bpt], [b_mkT])
                    CP("act", mkT[:, 2:4, :], pt2[:, :].rearrange("p (h n) -> p h n", h=2), [bpt2], [b_mkT])
                    wt, bw = load_chunk(19)
                    w3 = wt[:, 0:4096].rearrange("p (k c) -> p k c", k=8)
                    for nb in range(2):
                        pt, bpt = PS()
                        for k in range(8):
                            MM(pt[:, :], memTt[:, k, nb * 128:(nb + 1) * 128], w3[:, k, :], k == 0, k == 7,
                               [bw, b_memT], [bpt])
                        CP("act", mvc[:, nb, :, 0:128], pt[:, :].rearrange("p (h v) -> p h v", h=4), [bpt], [b_mvc])
                    S.barrier()
                if seq == 0:
                    for ci, src in enumerate((wg_d[0], wg_d[1], wu_d[0], wu_d[1], wd_d[0], wd_d[1])):
                        for r0 in range(0, NE * 128, 512):
                            bw_ = Buf()
                            S.dma("pool", wall_d[r0:r0 + 512, ci * 2048:(ci + 1) * 2048], src[r0:r0 + 512, :], (), [bw_])
                            b_wall.append(bw_)

                for st_i in range(TPS // STT):
                    tiles = [st_i * STT + tt for tt in range(STT)]
                    gt = [seq * TPS + t for t in tiles]
                    for tt in range(STT):
                        r0 = gt[tt] * 128
                        S.dma("sp", xs[:, tt, :], x_d[r0:r0 + 128, :], (), [b_xs[tt]])
                        CP("dve", xbf[:], xs[:, tt, :], [b_xs[tt]], [b_xbf])
                        pt, bpt = PS()
                        ptb = pt[:, :].bitcast(BF16)
                        for k in range(8):
                            TR(ptb[:, k * 128:(k + 1) * 128], xbf[:, k * 128:(k + 1) * 128], ident_bf[:],
                               [b_xbf, b_ident_bf], [bpt])
                        CP("act", xT[:, :, tt * 128:(tt + 1) * 128], ptb[:, :].rearrange("p (k t) -> p k t", k=8),
                           [bpt], [b_xT])
                    for c in range(8):
                        wt, bw = load_chunk(c)
                        w3 = wt[:, 0:4096].rearrange("p (k c) -> p k c", k=8)
                        if c == 7:
                            for h in range(4):
                                pt, bpt = PS()
                                for k in range(8):
                                    MM(pt[:, 0:TW], w3[:, k, h * 128:(h + 1) * 128], xT[:, k, :], k == 0, k == 7,
                                       [bw, b_xT], [bpt])
                                CP("act", mqT[:, h, :], pt[:, 0:TW], [bpt], [b_mqT])
                            continue
                        for tt in range(STT):
                            pt, bpt = PS()
                            for k in range(8):
                                MM(pt[:, :], xT[:, k, tt * 128:(tt + 1) * 128], w3[:, k, :], k == 0, k == 7,
                                   [bw, b_xT], [bpt])
                            if c == 0:
                                ACT(sq[:, tt, :], pt[:, :], AF.Silu, [bpt], [b_sq[tt]])
                            elif c == 1:
                                ACT(sg[:, tt, :], pt[:, :], AF.Sigmoid, [bpt], [b_sg[tt]])
                            elif c == 2:
                                CP("act", vhg[:, tt, :], pt[:, :], [bpt], [b_vhg[tt]])
                            elif c == 3:
                                ACT(gsl[:, tt, :], pt[:, :], AF.Silu, [bpt], [b_gsl[tt]])
                            elif c in (4, 5):
                                ti = gt[tt]
                                sc = 0.125 if c == 4 else 1.0
                                r_f, br_f = wk[0], b_wk[0]
                                r_b, br_b = wkb[0], b_wkb[0]
                                ACT(r_f[:], pt[:, :], AF.Copy, [bpt], [br_f], scale=sc)
                                CP("dve", r_b[:], r_f[:], [br_f], [br_b])
                                f3 = r_f[:].rearrange("p (g d) -> p g d", g=8)
                                o3 = r_b[:].rearrange("p (g d) -> p g d", g=8)
                                sn = trig[:, 0, ti, :].unsqueeze(1).to_broadcast([128, 8, 8])
                                cs = trig[:, 1, ti, :].unsqueeze(1).to_broadcast([128, 8, 8])
                                t_a, bt_a = wk[1], b_wk[1]
                                a3 = t_a[:, 0:64].rearrange("p (g d) -> p g d", g=8)
                                b3 = t_a[:, 64:128].rearrange("p (g d) -> p g d", g=8)
                                TT(a3, f3[:, :, 0:8], cs, ALU.mult, [br_f, b_trig], [bt_a])
                                TT(b3, f3[:, :, 8:16], sn, ALU.mult, [br_f, b_trig], [bt_a])
                                TT(o3[:, :, 0:8], a3, b3, ALU.subtract, [bt_a, br_b], [br_b])
                                TT(a3, f3[:, :, 8:16], cs, ALU.mult, [br_f, b_trig, bt_a], [bt_a])
                                TT(b3, f3[:, :, 0:8], sn, ALU.mult, [br_f, b_trig, bt_a], [bt_a])
                                TT(o3[:, :, 8:16], a3, b3, ALU.add, [bt_a, br_b], [br_b])
                                if c == 4:
                                    transpose_to(r_b, br_b, qT, b_qT[tt], tt * 128)
                                else:
                                    transpose_to(r_b, br_b, kc, b_kc[tiles[tt]], tiles[tt] * 128)
                            elif c == 6:
                                CP("act", vc[:, tiles[tt], :, 0:128], pt[:, :].rearrange("p (h v) -> p h v", h=4),
                                   [bpt, b_vc1], [b_vc[tiles[tt]]])

                    flush_router()
                    for tt in range(STT):
                        ti = tiles[tt]
                        c0 = tt * 128
                        fg, bfg = wk[0], b_wk[0]
                        lf, blf = wk[1], b_wk[1]
                        TT(fg[:], sg[:, tt, :], oml[:], ALU.mult, [b_sg[tt], b_oml], [bfg])
                        TT(fg[:], fg[:], lbv[:], ALU.add, [bfg, b_lbv], [bfg])
                        ACT(lf[:], fg[:], AF.Ln, [bfg], [blf])
                        pD, bpD = PS()
                        MM(pD[:, :], dmat[:], lf[:], True, True, [b_dmat, blf], [bpD])
                        eD, beD = wk[2], b_wk[2]
                        eDn, beDn = wk[3], b_wk[3]
                        ACT(eD[:], pD[:, :], AF.Exp, [bpD], [beD])
                        ACT(eDn[:], pD[:, :], AF.Exp, [bpD], [beDn], scale=-1.0)
                        TS(fg[:], fg[:], -1.0, 1.0, ALU.mult, ALU.add, [bfg], [bfg])
                        kt, bkt = wkb[0], b_wkb[0]
                        qt, bqt = wkb[1], b_wkb[1]
                        TT(kt[:], fg[:], eD[:], ALU.mult, [bfg, beD], [bkt])
                        TT(qt[:], sq[:, tt, :], eDn[:], ALU.mult, [b_sq[tt], beDn], [bqt])

                        od, bod = wk[4], b_wk[4]
                        nkb = ti + 1
                        tasks = []
                        for h in range(4):
                            for m in range(2):
                                for g0 in range(0, nkb, 4):
                                    tasks.append((h, m, g0, min(4, nkb - g0)))

                        def att_a(task):
                            h, m, g0, ng = task
                            ps_ = slice(m * 64, (m + 1) * 64)
                            psc, bpsc = PS()
                            for jj in range(ng):
                                j = g0 + jj
                                MM(psc[:, jj * 128:(jj + 1) * 128], kc[ps_, h, j * 128:(j + 1) * 128],
                                   qT[ps_, h, c0:c0 + 128], True, True, [b_kc[j], b_qT[tt]], [bpsc])
                            P, bP = pT[psel["n"] % NPT], b_pT[psel["n"] % NPT]
                            psel["n"] += 1
                            ACT(P[:, 0:ng * 128], psc[:, 0:ng * 128], AF.Exp, [bpsc], [bP])
                            if g0 + ng == nkb:
                                dsl = slice((ng - 1) * 128, ng * 128)
                                TT(P[:, dsl], P[:, dsl], maskT[:], ALU.mult, [bP, b_maskT], [bP])
                            return P, bP

                        def att_b(task, P, bP):
                            h, m, g0, ng = task
                            pacc, bpacc = ACC[2] if h % 2 == 0 else ACC[0]
                            for jj in range(ng):
                                j = g0 + jj
                                MM(pacc[:, m * 130:(m + 1) * 130], P[:, jj * 128:(jj + 1) * 128], vc[:, j, h, :],
                                   j == 0, j == nkb - 1, [bP, b_vc[j], b_vc1], [bpacc])
                            if m == 1 and g0 + ng == nkb:
                                rr = sm[:, 24:28]
                                RECIP(rr[:, 0:1], pacc[:, 128:129], [bpacc], [b_sm])
                                RECIP(rr[:, 1:2], pacc[:, 258:259], [bpacc], [b_sm])
                                TT(rr[:, 2:3], rr[:, 1:2], lam_ap, ALU.mult, [b_sm, b_lam], [b_sm])
                                t2, bt2 = wk[5], b_wk[5]
                                TS(t2[:, 0:128], pacc[:, 130:258], rr[:, 2:3], None, ALU.mult, None, [bpacc, b_sm], [bt2])
                                SCTT(od[:, h * 128:(h + 1) * 128], pacc[:, 0:128], rr[:, 0:1], t2[:, 0:128], ALU.mult,
                                     ALU.subtract, [bpacc, b_sm, bt2], [bod])

                        inflight = []
                        for task in tasks:
                            inflight.append((task,) + att_a(task))
                            if len(inflight) > LOOKAHEAD:
                                t0_, P0_, bP0_ = inflight.pop(0)
                                att_b(t0_, P0_, bP0_)
                        for (t0_, P0_, bP0_) in inflight:
                            att_b(t0_, P0_, bP0_)
                        y_bf, by_bf = wkb[4], b_wkb[4]
                        rms_gate(od[:], bod, 0.8, df_gain, [b_vec1], None, None, y_bf[:], by_bf)
                        if debug:
                            CP("dve", wk[5][:], y_bf[:], [by_bf], [b_wk[5]])
                            S.dma("sp", dbg_df[gt[tt] * 128:(gt[tt] + 1) * 128, :], wk[5][:], [b_wk[5]], [Buf()])
                        transpose_to(y_bf, by_bf, yT[1], b_yT[1][tt], c0)

                        kqT, bkqT = wkb[2], b_wkb[2]
                        qqT, bqqT = wkb[3], b_wkb[3]
                        for (src, bsrc, dst, bdst) in ((kt, bkt, kqT, bkqT), (qt, bqt, qqT, bqqT)):
                            pt, bpt = PS()
                            ptb = pt[:, :].bitcast(BF16)
                            for h in range(4):
                                TR(ptb[:, h * 128:(h + 1) * 128], src[:, h * 128:(h + 1) * 128], ident_bf[:],
                                   [bsrc, b_ident_bf], [bpt])
                            CP("act", dst[:], ptb[:, 0:512], [bpt], [bdst])
                        pc, bpc = PS()
                        for h in range(4):
                            MM(pc[:, h * 2:h * 2 + 2], lf[:, h * 128:(h + 1) * 128], cols2[:], True, True,
                               [blf, b_cols2], [bpc])
                        ex = sm[:, 8:20].rearrange("p (h c) -> p h c", h=4)
                        pc3 = pc[:, 0:8].rearrange("p (h c) -> p h c", h=4)
                        CP("dve", ex[:, :, 0:2], pc3, [bpc], [b_sm])
                        TT(ex[:, :, 2:3], ex[:, :, 0:1], ex[:, :, 1:2], ALU.subtract, [b_sm], [b_sm])
                        ACT(sm[:, 8:20], sm[:, 8:20], AF.Exp, [b_sm], [b_sm])
                        po, bpo = ACC[0]
                        pU, bpU = ACC[1]
                        hsc = []
                        for h in range(4):
                            hs = slice(h * 128, (h + 1) * 128)
                            psc, bpsc = PS()
                            MM(psc[:, 0:128], kqT[:, hs], qqT[:, hs], True, True, [bkqT, bqqT], [bpsc])
                            hsc.append((psc, bpsc))
                        hat = []
                        for h in range(4):
                            psc, bpsc = hsc[h]
                            AT, bAT = pT[psel["n"] % NPT], b_pT[psel["n"] % NPT]
                            psel["n"] += 1
                            TT(AT[:, 0:128], psc[:, 0:128], maskT[:], ALU.mult, [bpsc, b_maskT], [bAT])
                            hat.append((AT, bAT))
                        for h in range(4):
                            TS(Smm[:, h, :], Sst[:, h, :], ex[:, h, 1:2], None, ALU.mult, None, [b_S, b_sm], [b_Smm])
                        for h in range(4):
                            hs = slice(h * 128, (h + 1) * 128)
                            AT, bAT = hat[h]
                            MM(pU[:, hs], kt[:, hs], vhg[:, tt, hs], True, True, [bkt, b_vhg[tt]], [bpU])
                            MM(po[:, hs], AT[:, 0:128], vhg[:, tt, hs], True, False, [bAT, b_vhg[tt]], [bpo])
                            MM(po[:, hs], qqT[:, hs], Smm[:, h, :], False, True, [bqqT, b_Smm], [bpo])
                        for h in range(4):
                            hs = slice(h * 128, (h + 1) * 128)
                            TS(Sst[:, h, :], Sst[:, h, :], ex[:, h, 0:1], None, ALU.mult, None, [b_S, b_sm], [b_S])
                            SCTT(Sst[:, h, :], pU[:, hs], ex[:, h, 2:3], Sst[:, h, :], ALU.mult, ALU.add,
                                [bpU, b_sm, b_S], [b_S])
                        o_sb, bo_sb = wk[4], b_wk[4]
                        CP("act", o_sb[:], po[:, :], [bpo], [bo_sb])
                        y_bf, by_bf = wkb[4], b_wkb[4]
                        rms_gate(o_sb[:], bo_sb, 1.0, hg_gain, [b_vec1], gsl[:, tt, :], [b_gsl[tt]], y_bf[:], by_bf)
                        if debug:
                            CP("dve", wk[5][:], y_bf[:], [by_bf], [b_wk[5]])
                            S.dma("sp", dbg_hg[gt[tt] * 128:(gt[tt] + 1) * 128, :], wk[5][:], [b_wk[5]], [Buf()])
                        transpose_to(y_bf, by_bf, yT[0], b_yT[0][tt], c0)

                        ym, bym = wk[4], b_wk[4]
                        for hp in range(2):
                            psc, bpsc = PS()
                            for hh in range(2):
                                h = hp * 2 + hh
                                for nb in range(2):
                                    MM(psc[:, (hh * 2 + nb) * 128:(hh * 2 + nb + 1) * 128], mkT[:, h, nb * 128:(nb + 1) * 128],
                                       mqT[:, h, c0:c0 + 128], True, True, [b_mkT, b_mqT], [bpsc])
                            P, bP = pT[psel["n"] % NPT], b_pT[psel["n"] % NPT]
                            psel["n"] += 1
                            ACT(P[:, :], psc[:, :], AF.Exp, [bpsc], [bP], scale=128.0 ** -0.5)
                            pacc, bpacc = ACC[1]
                            for hh in range(2):
                                h = hp * 2 + hh
                                for nb in range(2):
                                    MM(pacc[:, hh * 130:(hh + 1) * 130], P[:, (hh * 2 + nb) * 128:(hh * 2 + nb + 1) * 128],
                                       mvc[:, nb, h, :], nb == 0, nb == 1, [bP, b_mvc, b_mvc1], [bpacc])
                            rr = sm[:, 28:30]
                            RECIP(rr[:, 0:1], pacc[:, 128:129], [bpacc], [b_sm])
                            RECIP(rr[:, 1:2], pacc[:, 258:259], [bpacc], [b_sm])
                            for hh in range(2):
                                h = hp * 2 + hh
                                TS(ym[:, h * 128:(h + 1) * 128], pacc[:, hh * 130:hh * 130 + 128], rr[:, hh:hh + 1], None,
                                   ALU.mult, None, [bpacc, b_sm], [bym])
                        y_bf, by_bf = wkb[4], b_wkb[4]
                        CP("dve", y_bf[:], ym[:], [bym], [by_bf])
                        if debug:
                            S.dma("sp", dbg_mem[gt[tt] * 128:(gt[tt] + 1) * 128, :], ym[:], [bym], [Buf()])
                        transpose_to(y_bf, by_bf, yT[2], b_yT[2][tt], c0)

                    rd_y = [b for br in range(3) for b in b_yT[br]]
                    for cg in range(8):
                        wt, bw = load_chunk(8 + cg)
                        wg3 = wt[:, 0:3072].rearrange("p (k b c) -> p k b c", k=8, b=3)
                        wb3 = wt[:, 3072:4608].rearrange("p (k b c) -> p k b c", k=4, b=3)
                        acc, bacc = wk[6], b_wk[6]
                        for br in range(3):
                            pg, bpg = PS()
                            for k in range(8):
                                MM(pg[:, 0:TW], wg3[:, k, br, :], xT[:, k, :], k == 0, k == 7, [bw, b_xT], [bpg])
                            gt_, bgt_ = wk[br], b_wk[br]
                            ACT(gt_[:, 0:TW], pg[:, 0:TW], AF.Sigmoid, [bpg], [bgt_])
                            pb, bpb = PS()
                            for k in range(4):
                                MM(pb[:, 0:TW], wb3[:, k, br, :], yT[br][:, k, :], k == 0, k == 3, [bw] + rd_y, [bpb])
                            if br == 0:
                                TT(acc[:, 0:TW], pb[:, 0:TW], gt_[:, 0:TW], ALU.mult, [bpb, bgt_], [bacc])
                            else:
                                TT(gt_[:, 0:TW], pb[:, 0:TW], gt_[:, 0:TW], ALU.mult, [bpb, bgt_], [bgt_])
                                if br == 1:
                                    TT(acc[:, 0:TW], acc[:, 0:TW], gt_[:, 0:TW], ALU.add, [bacc, bgt_], [bacc])
                                else:
                                    TT(mergedT[:, cg, :], acc[:, 0:TW], gt_[:, 0:TW], ALU.add, [bacc, bgt_], [b_mg[cg]])

                    zts = [(z, b_z), (z_b, b_zB)]
                    for n in range(2):
                        wt, bw = load_chunk(16 + n)
                        w3 = wt[:, 0:4096].rearrange("p (k c) -> p k c", k=8)
                        for tt in range(STT):
                            zt_, bzt_ = zts[tt]
                            pt, bpt = PS()
                            for k in range(8):
                                MM(pt[:, :], mergedT[:, k, tt * 128:(tt + 1) * 128], w3[:, k, :], k == 0, k == 7,
                                   [bw] + b_mg, [bpt])
                            SCTT(zt_[:, n * 512:(n + 1) * 512], xs[:, tt, n * 512:(n + 1) * 512], ALPHA, pt[:, :],
                                 ALU.mult, ALU.add, [b_xs[tt], bpt], [bzt_])
                    for tt in range(STT):
                        g = gt[tt]
                        zc, b_zc = zts[tt]
                        layer_norm(zc[:], b_zc, ln1g, ln1b, [b_vec1], zc[:], b_zc)
                        S.dma("sp", x1_d[g * 128:(g + 1) * 128, :], zc[:], [b_zc], [b_x1[g]])
                        CP("act", zb[:], zc[:], [b_zc], [b_zb])
                        S.dma("sp", x1b_d[g * 128:(g + 1) * 128, :], zb[:], [b_zb], [b_x1b[g]])
                        pending_router.append((g, zc, b_zc))
            flush_router()
            if debug:
                S.barrier()
                S.dma("sp", dbg_kc[:, :], kc[:].rearrange("p a b -> p (a b)"), (), [Buf()])
                S.dma("sp", dbg_vc[:, :], vc[:].rearrange("p a b c -> p (a b c)"), (), [Buf()])
                S.dma("sp", dbg_v1[:, :], vec1[:], (), [Buf()])
            S.barrier()

        if debug:
            fin = Buf()
            S.dma("sp", dbg_rt[:, :, :], rt[:], b_rt + [b_tot], [fin])

        if phases >= 2:
            with ExitStack() as p2:
                vec2 = sbt(p2, "vec2_sb", [128, 2048], F32); b_vec2 = Buf()
                S.dma("sp", vec2[:], v2_d[:, :], (), [b_vec2])
                ln2g = vec2[:, 0:1024]
                ln2b = vec2[:, 1024:2048]
                big = sbt(p2, "big", [128, 32 * 160], F32); b_big = Buf()
                rows = sbt(p2, "rows", [128, 512], F32); b_rows = Buf()
                irow = sbt(p2, "irow", [128, 160], I32); b_irow = Buf()
                bexp = sbt(p2, "bexp", [128, 160], I32); b_bexp = Buf()
                dst_i = sbt(p2, "dst_i", [128, NT * 2], I32); b_dst = Buf()
                stats_2 = sbt(p2, "stats2", [128, 16], F32); b_stats2 = Buf()
                S.op("pool", lambda e: e.iota(irow[:], pattern=[[128, 160]], base=0, channel_multiplier=0), (), [b_irow])
                CP("dve", rows[:, 128:288], irow[:], [b_irow], [b_rows])
                TT(big[:, 0:2048].rearrange("p (e k) -> p e k", e=32),
                   tot[:].unsqueeze(2).to_broadcast([128, 32, 64]),
                   rows[:, 128:192].unsqueeze(1).to_broadcast([128, 32, 64]), ALU.is_gt, [b_tot, b_rows], [b_big])
                RSUM(rows[:, 32:64], big[:, 0:2048].rearrange("p (e k) -> p e k", e=32), [b_big], [b_rows])
                TS(rows[:, 32:64], rows[:, 32:64], 128.0, None, ALU.mult, None, [b_rows], [b_rows])
                MEMSET("dve", rows[:, 352:384], 1.0, [b_rows])
                S.op("dve", lambda e: e.tensor_tensor_scan(rows[:, 64:96], rows[:, 352:384], rows[:, 32:64], 0.0,
                                                           ALU.mult, ALU.add), [b_rows], [b_rows])
                TT(rows[:, 96:128], rows[:, 64:96], rows[:, 32:64], ALU.subtract, [b_rows], [b_rows])
                TT(big[:].rearrange("p (b e) -> p b e", b=160),
                   rows[:, 64:96].unsqueeze(1).to_broadcast([128, 160, 32]),
                   rows[:, 128:288].unsqueeze(2).to_broadcast([128, 160, 32]), ALU.is_le, [b_rows], [b_big])
                RSUM(rows[:, 288:448], big[:].rearrange("p (b e) -> p b e", b=160), [b_big], [b_rows])
                TS(rows[:, 288:448], rows[:, 288:448], 31.0, None, ALU.min, None, [b_rows], [b_rows])
                CP("dve", bexp[:], rows[:, 288:448], [b_rows], [b_bexp])
                pio_i = sbt(p2, "pio_i", [128, 1], I32); b_pio = Buf()
                pio_f = sbt(p2, "pio_f", [128, 1], F32)
                idxw_f = sbt(p2, "idxw_f", [128, 160], F32); b_idxwf = Buf()
                idxw = sbt(p2, "idxw", [128, 160], I32); b_idxw = Buf()
                S.op("pool", lambda e: e.iota(pio_i[:], pattern=[[0, 1]], base=0, channel_multiplier=1), (), [b_pio])
                CP("dve", pio_f[:], pio_i[:], [b_pio], [b_pio])
                same_f = sbt(p2, "same_f", [128, 160], F32); b_same = Buf()
                t1_f = sbt(p2, "t1_f", [128, 160], F32); b_t1 = Buf()
                MEMSET("dve", same_f[:], 0.0, [b_same])
                TT(same_f[:, 3:160], rows[:, 291:448], rows[:, 288:445], ALU.is_equal, [b_rows, b_same], [b_same])
                TS(t1_f[:], rows[:, 288:448], 128.0, None, ALU.mult, None, [b_rows], [b_t1])
                TS(idxw_f[:], t1_f[:], -1.0, 4096.0, ALU.mult, ALU.add, [b_t1], [b_idxwf])
                TT(idxw_f[:], idxw_f[:], same_f[:], ALU.mult, [b_idxwf, b_same], [b_idxwf])
                TT(idxw_f[:], idxw_f[:], t1_f[:], ALU.add, [b_idxwf, b_t1], [b_idxwf])
                TS(idxw_f[:], idxw_f[:], pio_f[:, 0:1], None, ALU.add, None, [b_idxwf, b_pio], [b_idxwf])
                CP("dve", idxw[:], idxw_f[:], [b_idxwf], [b_idxw])
                w0 = sbt(p2, "w0", [128, 64], F32); b_w0 = Buf()
                for g in range(NT):
                    for j in range(2):
                        TS(w0[:, 0:32], e_iota[:], rt[:, g, j:j + 1], None, ALU.is_equal, None, [b_eiota, b_rt[g]], [b_w0])
                        TT(w0[:, 0:32], w0[:, 0:32], rows[:, 96:128], ALU.mult, [b_w0, b_rows], [b_w0])
                        RSUM(rt[:, g, 6 + j:7 + j], w0[:, 0:32], [b_w0], [b_rt[g]])
                        TT(rt[:, g, 6 + j:7 + j], rt[:, g, 6 + j:7 + j], rt[:, g, 4 + j:5 + j], ALU.add, [b_rt[g]], [b_rt[g]])
                CP("dve", dst_i[:].rearrange("p (g j) -> p g j", j=2), rt[:, :, 6:8], b_rt, [b_dst])

                dyn = {}

                def bound(e):
                    if "bnd" not in dyn:
                        breg = e.alloc_register("bndreg")
                        e.reg_mov(breg, NSLOT - 1)
                        dyn["bnd"] = e.snap(breg)
                    return dyn["bnd"]

                def boundw(e):
                    if "bndw" not in dyn:
                        breg = e.alloc_register("bndwreg")
                        e.reg_mov(breg, NE * 128 - 1)
                        dyn["bndw"] = e.snap(breg)
                    return dyn["bndw"]

                b_xs_sc = []
                xg = [sbt(p2, "xg%d" % i, [128, D], BF16) for i in range(2)]
                b_xg = [Buf() for _ in range(2)]
                for g in range(NT):
                    k = g % 2
                    S.dma("sp", xg[k][:], x1b_d[g * 128:(g + 1) * 128, :], [b_x1b[g]], [b_xg[k]])
                    for j in range(2):
                        bsc = Buf()
                        S.dma_fn("pool", (lambda e, g=g, j=j, k=k: e.indirect_dma_start(
                            out=xs_d[:, :], out_offset=bass.IndirectOffsetOnAxis(ap=dst_i[:, g * 2 + j:g * 2 + j + 1], axis=0),
                            in_=xg[k][:, :], in_offset=None, bounds_check=bound(e), oob_is_err=False)),
                            [b_xg[k], b_dst] + b_xs_zero, [bsc])
                        b_xs_sc.append(bsc)

                NW = 3
                wall_sb = [sbt(p2, "wall_sb%d" % i, [128, 12288], BF16) for i in range(NW)]
                wgb = [t[:, 0:4096].rearrange("p (k f) -> p k f", k=8) for t in wall_sb]
                wub = [t[:, 4096:8192].rearrange("p (k f) -> p k f", k=8) for t in wall_sb]
                wdb = [t[:, 8192:12288].rearrange("p (k f) -> p k f", k=4) for t in wall_sb]
                b_wgb = [Buf() for _ in range(NW)]
                b_wub = b_wgb
                b_wdb = b_wgb
                xb = [sbt(p2, "xb%d" % i, [128, D], BF16) for i in range(2)]
                b_xb = [Buf() for _ in range(2)]
                xbT = [sbt(p2, "xbT%d" % i, [128, 8, 128], BF16) for i in range(2)]
                b_xbT = [Buf() for _ in range(2)]
                hs_ = [sbt(p2, "hs%d" % i, [128, 512], F32) for i in range(2)]
                b_hs = [Buf() for _ in range(2)]
                hact = [sbt(p2, "hact%d" % i, [128, 512], BF16) for i in range(2)]
                b_hact = [Buf() for _ in range(2)]
                hT = [sbt(p2, "hT%d" % i, [128, 4, 128], BF16) for i in range(2)]
                b_hT = [Buf() for _ in range(2)]
                yb = [sbt(p2, "yb%d" % i, [128, D], F32) for i in range(2)]
                b_yb = [Buf() for _ in range(2)]
                b_ys = [Buf() for _ in range(NBLK)]

                def moe_gather(b):
                    k = b % NW
                    S.dma_fn("pool", (lambda e, k=k, b=b: e.indirect_dma_start(
                        out=wall_sb[k][:, :], out_offset=None, in_=wall_d[:, :],
                        in_offset=bass.IndirectOffsetOnAxis(ap=idxw[:, b:b + 1], axis=0),
                        bounds_check=boundw(e), oob_is_err=False)), [b_idxw] + (b_wall if b == 0 else []), [b_wgb[k]])

                def moe_a(b):
                    k = b % NW
                    j = b % 2
                    S.dma("sp", xb[j][:], xs_d[b * 128:(b + 1) * 128, :], b_xs_sc if b == 0 else [], [b_xb[j]])
                    pt, bpt = PS()
                    ptb = pt[:, :].bitcast(BF16)
                    for kk in range(8):
                        TR(ptb[:, kk * 128:(kk + 1) * 128], xb[j][:, kk * 128:(kk + 1) * 128], ident_bf[:],
                           [b_xb[j], b_ident_bf], [bpt])
                    CP("act", xbT[j][:], ptb[:, :].rearrange("p (k t) -> p k t", k=8), [bpt], [b_xbT[j]])

                def moe_a2(b):
                    k = b % NW
                    j = b % 2
                    pg, bpg = PS()
                    pu, bpu = PS()
                    for kk in range(8):
                        MM(pg[:, :], xbT[j][:, kk, :], wgb[k][:, kk, :], kk == 0, kk == 7, [b_xbT[j], b_wgb[k]], [bpg])
                    for kk in range(8):
                        MM(pu[:, :], xbT[j][:, kk, :], wub[k][:, kk, :], kk == 0, kk == 7, [b_xbT[j], b_wub[k]], [bpu])
                    ACT(hs_[j][:], pg[:, :], AF.Silu, [bpg], [b_hs[j]])
                    TT(hact[j][:], hs_[j][:], pu[:, :], ALU.mult, [b_hs[j], bpu], [b_hact[j]])

                def moe_b(b):
                    k = b % NW
                    j = b % 2
                    pt, bpt = PS()
                    ptb = pt[:, :].bitcast(BF16)
                    for kk in range(4):
                        TR(ptb[:, kk * 128:(kk + 1) * 128], hact[j][:, kk * 128:(kk + 1) * 128], ident_bf[:],
                           [b_hact[j], b_ident_bf], [bpt])
                    CP("act", hT[j][:], ptb[:, 0:512].rearrange("p (k t) -> p k t", k=4), [bpt], [b_hT[j]])

                def moe_b2(b):
                    k = b % NW
                    j = b % 2
                    for n in range(2):
                        py, bpy = PS()
                        for kk in range(4):
                            MM(py[:, :], hT[j][:, kk, :], wdb[k][:, kk, n * 512:(n + 1) * 512], kk == 0, kk == 3,
                               [b_hT[j], b_wdb[k]], [bpy])
                        CP("act" if n == 0 else "dve", yb[j][:, n * 512:(n + 1) * 512], py[:, :], [bpy], [b_yb[j]])
                    S.dma("sp", ys_d[b * 128:(b + 1) * 128, :], yb[j][:], [b_yb[j]], [b_ys[b]])

                moe_gather(0)
                moe_gather(1)
                for b in range(NBLK):
                    moe_a(b)
                    if b >= 1:
                        moe_b(b - 1)
                    moe_a2(b)
                    if b >= 1:
                        moe_b2(b - 1)
                    if b + 2 < NBLK:
                        moe_gather(b + 2)
                moe_b(NBLK - 1)
                moe_b2(NBLK - 1)

                y1 = [sbt(p2, "y1_%d" % i, [128, D], F32) for i in range(2)]
                y2 = [sbt(p2, "y2_%d" % i, [128, D], F32) for i in range(2)]
                xr = [sbt(p2, "xr%d" % i, [128, D], F32) for i in range(2)]
                b_y1 = [Buf() for _ in range(2)]
                b_y2 = [Buf() for _ in range(2)]
                b_xr = [Buf() for _ in range(2)]
                b_out = []

                def layer_norm2(zt, bz, out_ap, bout):
                    for hlf in range(2):
                        S.op("dve", lambda e, hlf=hlf: e.bn_stats(stats_2[:, hlf * 6:(hlf + 1) * 6], zt[:, hlf * 512:(hlf + 1) * 512]),
                             [bz], [b_stats2])
                    S.op("dve", lambda e: e.bn_aggr(stats_2[:, 12:14], stats_2[:, 0:12]), [b_stats2], [b_stats2])
                    TS(stats_2[:, 14:15], stats_2[:, 13:14], LN_EPS, None, ALU.add, None, [b_stats2], [b_stats2])
                    ACT(stats_2[:, 14:15], stats_2[:, 14:15], AF.Ln, [b_stats2], [b_stats2])
                    ACT(stats_2[:, 15:16], stats_2[:, 14:15], AF.Exp, [b_stats2], [b_stats2], scale=-0.5)
                    TS(zt, zt, stats_2[:, 12:13], stats_2[:, 15:16], ALU.subtract, ALU.mult, [bz, b_stats2], [bz])
                    TT(zt, zt, ln2g, ALU.mult, [bz, b_vec2], [bz])
                    TT(out_ap, zt, ln2b, ALU.add, [bz, b_vec2], [bout])

                for g in range(NT):
                    k = g % 2
                    S.dma("sp", xr[k][:], x1_d[g * 128:(g + 1) * 128, :], [b_x1[g]], [b_xr[k]])
                    for j, (yt, byt) in enumerate(((y1[k], b_y1[k]), (y2[k], b_y2[k]))):
                        S.dma_fn("pool", (lambda e, g=g, j=j, yt=yt: e.indirect_dma_start(
                            out=yt[:, :], out_offset=None, in_=ys_d[:, :],
                            in_offset=bass.IndirectOffsetOnAxis(ap=dst_i[:, g * 2 + j:g * 2 + j + 1], axis=0),
                            bounds_check=bound(e), oob_is_err=False)), [b_dst] + (b_ys if g == 0 else []), [byt])
                    TS(xr[k][:], xr[k][:], ALPHA, None, ALU.mult, None, [b_xr[k]], [b_xr[k]])
                    SCTT(xr[k][:], y1[k][:], rt[:, g, 2:3], xr[k][:], ALU.mult, ALU.add, [b_y1[k], b_rt[g], b_xr[k]], [b_xr[k]])
                    SCTT(xr[k][:], y2[k][:], rt[:, g, 3:4], xr[k][:], ALU.mult, ALU.add, [b_y2[k], b_rt[g], b_xr[k]], [b_xr[k]])
                    layer_norm2(xr[k][:], b_xr[k], xr[k][:], b_xr[k])
                    bo = Buf()
                    S.dma("sp", out_d[g * 128:(g + 1) * 128, :], xr[k][:], [b_xr[k]], [bo])
                    b_out.append(bo)
                S.barrier()
        else:
            S.barrier()
        S.barrier()
        S.emit()
        print("program: %d instructions, %d waits" % (S.n_ins, S.n_wait))
    return nc


def _weight_chunks(w_in, w_gates, wb_hg, wb_df, wb_mem, w_out, w_mem_kv):
    wf = np.zeros((NCHUNK, 128, CH), np.float32)

    def kchunks(w):
        K, C = w.shape
        return w.reshape(K // 128, 128, C).transpose(1, 0, 2)

    cols = list(range(2048))
    for h in range(4):
        cols += list(range(2048 + h * 64, 2048 + (h + 1) * 64)) + list(range(2304 + h * 64, 2304 + (h + 1) * 64))
    for h in range(4):
        cols += list(range(2560 + h * 64, 2560 + (h + 1) * 64)) + list(range(2816 + h * 64, 2816 + (h + 1) * 64))
    cols += list(range(3072, 4096))
    w_in_p = w_in[:, cols]
    for c in range(8):
        wf[c, :, 0:4096] = kchunks(w_in_p[:, c * 512:(c + 1) * 512]).reshape(128, 4096)
    wbr = [wb_hg, wb_df, wb_mem]
    for cg in range(8):
        g = np.stack([kchunks(w_gates[:, br * 1024 + cg * 128: br * 1024 + (cg + 1) * 128]) for br in range(3)], axis=2)
        wf[8 + cg, :, 0:3072] = g.reshape(128, 3072)
        bb = np.stack([kchunks(wbr[br][:, cg * 128:(cg + 1) * 128]) for br in range(3)], axis=2)
        wf[8 + cg, :, 3072:4608] = bb.reshape(128, 1536)
    for n in range(2):
        wf[16 + n, :, 0:4096] = kchunks(w_out[:, n * 512:(n + 1) * 512]).reshape(128, 4096)
        wf[18 + n, :, 0:4096] = kchunks(w_mem_kv[:, n * 512:(n + 1) * 512]).reshape(128, 4096)
    return wf


_NC_CACHE = {}


def kernel(x, mem, positions, w_in, w_gates, hgrn_lower_bounds, hgrn_norm_gain,
           diff_lambda_q1, diff_lambda_k1, diff_lambda_q2, diff_lambda_k2, diff_subln_gain,
           w_mem_kv, w_branch_hgrn, w_branch_diff, w_branch_mem, w_out, ln1_gain, ln1_bias,
           w_group_router, w_expert_router, w_expert_gate, w_expert_up, w_expert_down,
           ln2_gain, ln2_bias, _debug=False, _phases=3):
    f = lambda a: np.ascontiguousarray(np.asarray(a))
    x = f(x); mem = f(mem); positions = f(positions)
    wf = _weight_chunks(f(w_in)[0], f(w_gates)[0], f(w_branch_hgrn)[0], f(w_branch_diff)[0], f(w_branch_mem)[0],
                        f(w_out)[0], f(w_mem_kv)[0])
    wr = np.concatenate([f(w_group_router)[0], f(w_expert_router)[0]], axis=1)
    wr = np.ascontiguousarray(wr.reshape(8, 128, 36).transpose(1, 0, 2))
    rep = lambda v: np.broadcast_to(np.asarray(v, np.float32).reshape(1, -1), (128, np.asarray(v).size))
    lbr = np.ascontiguousarray(np.concatenate([rep(f(hgrn_lower_bounds)[0]), rep(f(hgrn_lower_bounds)[1])], axis=1),
                               dtype=np.float32)
    vec1 = np.ascontiguousarray(np.concatenate([
        rep(f(hgrn_norm_gain)[0]),
        rep(f(diff_lambda_q1)[0]), rep(f(diff_lambda_k1)[0]), rep(f(diff_lambda_q2)[0]), rep(f(diff_lambda_k2)[0]),
        rep(np.tile(f(diff_subln_gain)[0], 4)), rep(f(ln1_gain)[0]), rep(f(ln1_bias)[0])], axis=1), dtype=np.float32)
    vec2 = np.ascontiguousarray(np.concatenate([rep(f(ln2_gain)[0]), rep(f(ln2_bias)[0])], axis=1), dtype=np.float32)
    def halves(w, kc):
        w2 = w.reshape(NE, kc, 128, -1).transpose(0, 2, 1, 3).reshape(NE * 128, 4096)
        return [np.ascontiguousarray(w2[:, 0:2048]), np.ascontiguousarray(w2[:, 2048:4096])]
    wg = halves(f(w_expert_gate)[0], 8)
    wu = halves(f(w_expert_up)[0], 8)
    wd = halves(f(w_expert_down)[0], 4)

    key = (bool(_debug), int(_phases))
    if key not in _NC_CACHE:
        _NC_CACHE[key] = build_program(debug=_debug, phases=_phases)
    nc = _NC_CACHE[key]
    in_maps = []
    for c in range(NCORES):
        xb = x[c * NSEQ:(c + 1) * NSEQ].reshape(NTOK, D)
        mb = mem[c * NSEQ:(c + 1) * NSEQ].reshape(NSEQ * 256, D)
        pb = positions[c * NSEQ:(c + 1) * NSEQ].reshape(NT, 128).T
        in_maps.append(dict(x=np.ascontiguousarray(xb), mem=np.ascontiguousarray(mb),
                            pos=np.ascontiguousarray(pb.astype(np.int32)), wf=wf, wr=wr, vec1=vec1, vec2=vec2, lbr=lbr,
                            wg0=wg[0], wg1=wg[1], wu0=wu[0], wu1=wu[1], wd0=wd[0], wd1=wd[1]))
    res = run_bass_kernel_spmd(nc, in_maps, core_ids=list(range(NCORES)))
    if _debug:
        return res.results
    out = np.concatenate([r["out"].reshape(NSEQ, SEQ, D) for r in res.results], axis=0)
    return out.astype(np.float32)
```
